# Optimizing a Trainium2 kernel written in Bass

```python
import math
import jax, jax.numpy as jnp
from jax import lax
import numpy as np


D_MODEL = 1024
BATCH = 16
SEQ = 2048
DEPTH = 1

ATTN_HEADS = 16
ATTN_HEAD_DIM = 64
ATTN_WIDTH = ATTN_HEADS * ATTN_HEAD_DIM
ATTN_SCALE = ATTN_HEAD_DIM ** -0.5
MOBA_BLOCK = 256
MOBA_TOPK = 3
Q_BLOCK = 128
REL_BUCKETS = 32
REL_MAX_EXACT = 16
REL_MAX_DISTANCE = 1024
SSM_EXPAND = 2
SSM_INNER = SSM_EXPAND * D_MODEL
SSM_HEAD_DIM = 64
SSM_HEADS = SSM_INNER // SSM_HEAD_DIM
SSM_GROUPS = 4
SSM_HEADS_PER_GROUP = SSM_HEADS // SSM_GROUPS
SSM_STATE = 128
SSM_CONV = 4
SSM_CHUNK = 128
SSM_CONV_DIM = SSM_INNER + 2 * SSM_GROUPS * SSM_STATE
N_EXPERTS = 32
TOP_K = 4
EXPERT_FF = D_MODEL
SWIGLU_LIMIT = 7.0
SWIGLU_ALPHA = 1.702
MOE_BLOCK = 128
N_BRANCHES = 2
IN_PROJ_DIM = 3 * ATTN_WIDTH + SSM_INNER + SSM_CONV_DIM + SSM_HEADS + N_BRANCHES * D_MODEL
EPS = 1e-6

kernel_name = 'hybrid_moba_mamba2_moe_adaln'


def rms_norm(x, g):
    xf = x.astype(jnp.float32)
    y = xf * lax.rsqrt(jnp.mean(xf * xf, axis=-1, keepdims=True) + EPS)
    return (y * g.astype(jnp.float32)).astype(x.dtype)


def t5_bucket(dist):
    is_small = dist < REL_MAX_EXACT
    d = jnp.maximum(dist, 1).astype(jnp.float32)
    large = REL_MAX_EXACT + (jnp.log(d / REL_MAX_EXACT) / math.log(REL_MAX_DISTANCE / REL_MAX_EXACT)
                             * (REL_BUCKETS - REL_MAX_EXACT)).astype(jnp.int32)
    large = jnp.minimum(large, REL_BUCKETS - 1)
    return jnp.where(is_small, dist, large)


def moba_attention_single(q, k, v, rel_table):
    S = q.shape[0]
    s_pad = -(-S // MOBA_BLOCK) * MOBA_BLOCK
    pad = ((0, s_pad - S), (0, 0), (0, 0))
    q, k, v = (jnp.pad(t, pad).transpose(1, 0, 2) for t in (q, k, v))
    n_blocks = s_pad // MOBA_BLOCK
    n_sel = min(MOBA_TOPK, n_blocks)
    kb = k.reshape(ATTN_HEADS, n_blocks, MOBA_BLOCK, ATTN_HEAD_DIM)
    vb = v.reshape(ATTN_HEADS, n_blocks, MOBA_BLOCK, ATTN_HEAD_DIM)
    k_mean = jnp.mean(kb.astype(jnp.float32), axis=2)
    q_blk = jnp.arange(s_pad) // MOBA_BLOCK
    fully_past = jnp.arange(n_blocks)[None, :] < q_blk[:, None]
    gate = jnp.einsum('hsd,hnd->hsn', q.astype(jnp.float32), k_mean)
    _, sel = lax.top_k(jnp.where(fully_past[None], gate, -jnp.inf), n_sel)
    sel_ok = jnp.arange(n_sel)[None, :] < q_blk[:, None]
    table_t = rel_table.T.astype(jnp.float32)
    h_ix = jnp.arange(ATTN_HEADS)[:, None, None]
    key_off = jnp.arange(MOBA_BLOCK)

    def query_block(args):
        qb, idx, ok, q0 = args
        q_pos = q0 + jnp.arange(Q_BLOCK)
        k_sel = kb[h_ix, idx]
        v_sel = vb[h_ix, idx]
        s_past = jnp.einsum('hqd,hqntd->hqnt', qb, k_sel).astype(jnp.float32) * ATTN_SCALE
        dist_past = q_pos[None, :, None, None] - (idx[..., None] * MOBA_BLOCK + key_off)
        s_past = s_past + table_t[h_ix[..., None], t5_bucket(jnp.maximum(dist_past, 0))]
        own = q0 // MOBA_BLOCK
        k_own = lax.dynamic_index_in_dim(kb, own, axis=1, keepdims=False)
        v_own = lax.dynamic_index_in_dim(vb, own, axis=1, keepdims=False)
        dist_own = q_pos[:, None] - (own * MOBA_BLOCK + key_off)[None, :]
        s_own = (jnp.einsum('hqd,htd->hqt', qb, k_own).astype(jnp.float32) * ATTN_SCALE
                 + table_t[:, t5_bucket(jnp.maximum(dist_own, 0))])
        logits = jnp.concatenate([s_past.reshape(ATTN_HEADS, Q_BLOCK, n_sel * MOBA_BLOCK), s_own], axis=-1)
        mask = jnp.concatenate([
            jnp.broadcast_to(ok[None, :, :, None], s_past.shape).reshape(ATTN_HEADS, Q_BLOCK, n_sel * MOBA_BLOCK),
            jnp.broadcast_to((dist_own >= 0)[None], s_own.shape)], axis=-1)
        p = jax.nn.softmax(jnp.where(mask, logits, -jnp.inf), axis=-1).astype(v_own.dtype)
        p_past = p[..., :n_sel * MOBA_BLOCK].reshape(ATTN_HEADS, Q_BLOCK, n_sel, MOBA_BLOCK)
        p_own = p[..., n_sel * MOBA_BLOCK:]
        return (jnp.einsum('hqnt,hqntd->hqd', p_past, v_sel)
                + jnp.einsum('hqt,htd->hqd', p_own, v_own))

    n_qb = s_pad // Q_BLOCK
    xs = (q.reshape(ATTN_HEADS, n_qb, Q_BLOCK, ATTN_HEAD_DIM).transpose(1, 0, 2, 3),
          sel.reshape(ATTN_HEADS, n_qb, Q_BLOCK, n_sel).transpose(1, 0, 2, 3),
          sel_ok.reshape(n_qb, Q_BLOCK, n_sel),
          jnp.arange(n_qb, dtype=jnp.int32) * Q_BLOCK)
    out = lax.map(query_block, xs)
    return out.transpose(0, 2, 1, 3).reshape(s_pad, ATTN_HEADS, ATTN_HEAD_DIM)[:S]


def causal_depthwise_conv(x, w, b):
    K = w.shape[0]
    S = x.shape[1]
    xp = jnp.pad(x, ((0, 0), (K - 1, 0), (0, 0)))
    y = b
    for tap in range(K):
        y = y + xp[:, tap:tap + S] * w[tap]
    return y


def ssd_chunked(xh, dt, a, b_in, c_in):
    Bsz, S = xh.shape[:2]
    nc = S // SSM_CHUNK
    L, G, J = SSM_CHUNK, SSM_GROUPS, SSM_HEADS_PER_GROUP
    xdt = (xh * dt[..., None]).reshape(Bsz, nc, L, G, J, SSM_HEAD_DIM)
    a_dt = (dt * a).reshape(Bsz, nc, L, G, J).transpose(0, 3, 4, 1, 2)
    a_cs = jnp.cumsum(a_dt, axis=-1)
    bc = b_in.reshape(Bsz, nc, L, G, SSM_STATE)
    cc = c_in.reshape(Bsz, nc, L, G, SSM_STATE)
    pos = jnp.arange(L)
    causal = pos[:, None] >= pos[None, :]
    seg = a_cs[..., :, None] - a_cs[..., None, :]
    decay = jnp.where(causal, jnp.exp(jnp.where(causal, seg, 0.0)), 0.0)
    cb = jnp.einsum('bclgn,bcsgn->bcgls', cc, bc)
    y_diag = jnp.einsum('bcgls,bgjcls,bcsgjp->bclgjp', cb, decay, xdt)
    decay_to_end = jnp.exp(a_cs[..., -1:] - a_cs)
    chunk_states = jnp.einsum('bclgn,bgjcl,bclgjp->bcgjpn', bc, decay_to_end, xdt)
    chunk_decay = jnp.exp(a_cs[..., -1])

    def carry_state(h, inp):
        st, dec = inp
        return h * dec[..., None, None] + st, h

    h0 = jnp.zeros_like(chunk_states[:, 0])
    _, prev = lax.scan(carry_state, h0, (jnp.moveaxis(chunk_states, 1, 0), jnp.moveaxis(chunk_decay, -1, 0)))
    prev = jnp.moveaxis(prev, 0, 1)
    y_off = jnp.einsum('bclgn,bcgjpn,bgjcl->bclgjp', cc, prev, jnp.exp(a_cs))
    return (y_diag + y_off).reshape(Bsz, S, SSM_HEADS, SSM_HEAD_DIM)


def mamba2_branch(z, xbc, dt_raw, conv_w, conv_b, dt_bias, a_log, d_skip, ssm_norm_g):
    Bsz, S, _ = z.shape
    xbc = jax.nn.silu(causal_depthwise_conv(xbc, conv_w, conv_b))
    xs, b_in, c_in = jnp.split(xbc, [SSM_INNER, SSM_INNER + SSM_GROUPS * SSM_STATE], axis=-1)
    xs = xs.reshape(Bsz, S, SSM_HEADS, SSM_HEAD_DIM).astype(jnp.float32)
    dt = jax.nn.softplus((dt_raw + dt_bias).astype(jnp.float32))
    a = -jnp.exp(a_log.astype(jnp.float32))
    y = ssd_chunked(xs, dt, a,
                    b_in.reshape(Bsz, S, SSM_GROUPS, SSM_STATE).astype(jnp.float32),
                    c_in.reshape(Bsz, S, SSM_GROUPS, SSM_STATE).astype(jnp.float32))
    y = y + d_skip.astype(jnp.float32)[:, None] * xs
    y = y.reshape(Bsz, S, SSM_INNER) * jax.nn.silu(z.astype(jnp.float32))
    yg = y.reshape(Bsz, S, SSM_GROUPS, SSM_INNER // SSM_GROUPS)
    yg = yg * lax.rsqrt(jnp.mean(yg * yg, axis=-1, keepdims=True) + EPS)
    return (yg.reshape(Bsz, S, SSM_INNER) * ssm_norm_g.astype(jnp.float32)).astype(z.dtype)


def clamped_swiglu(h):
    glu, lin = jnp.split(h, 2, axis=-1)
    glu = jnp.minimum(glu, SWIGLU_LIMIT)
    lin = jnp.clip(lin, -SWIGLU_LIMIT, SWIGLU_LIMIT)
    return glu * jax.nn.sigmoid(SWIGLU_ALPHA * glu) * (lin + 1.0)


def moe_ffn(u, router_w, router_b, w1, b1, w2, b2):
    T, D = u.shape
    logits = (u @ router_w).astype(jnp.float32) + router_b.astype(jnp.float32)
    top_logits, top_e = lax.top_k(logits, TOP_K)
    top_w = jax.nn.softmax(top_logits, axis=-1).astype(u.dtype)
    n_pairs = T * TOP_K
    e_flat = top_e.reshape(-1)
    tok_flat = jnp.arange(n_pairs, dtype=jnp.int32) // TOP_K
    order = jnp.argsort(e_flat)
    e_sorted = e_flat[order]
    tok_sorted = tok_flat[order]
    w_sorted = top_w.reshape(-1)[order]
    counts = jax.ops.segment_sum(jnp.ones(n_pairs, jnp.int32), e_flat, num_segments=N_EXPERTS)
    padded = (counts + MOE_BLOCK - 1) // MOE_BLOCK * MOE_BLOCK
    start = jnp.cumsum(counts) - counts
    p_end = jnp.cumsum(padded)
    p_start = p_end - padded
    dest = p_start[e_sorted] + jnp.arange(n_pairs, dtype=jnp.int32) - start[e_sorted]
    cap = n_pairs + N_EXPERTS * MOE_BLOCK
    n_blocks = cap // MOE_BLOCK
    buf_tok = jnp.zeros(cap, jnp.int32).at[dest].set(tok_sorted)
    buf_w = jnp.zeros(cap, u.dtype).at[dest].set(w_sorted)
    block_e = jnp.minimum(jnp.searchsorted(p_end, jnp.arange(n_blocks) * MOE_BLOCK, side='right'), N_EXPERTS - 1)

    def expert_block(args):
        tok, wt, e = args
        xb = u[tok]
        h = xb @ w1[e] + b1[e]
        y = clamped_swiglu(h) @ w2[e] + b2[e]
        return y * wt[:, None]

    ys = lax.map(expert_block, (buf_tok.reshape(n_blocks, MOE_BLOCK), buf_w.reshape(n_blocks, MOE_BLOCK), block_e))
    return jnp.zeros_like(u).at[buf_tok].add(ys.reshape(cap, D))


def hybrid_layer(x, c_act, ada_w, ada_b, norm1_g, w_in, q_norm_g, k_norm_g, rel_bias_table,
                 conv_w, conv_b, dt_bias, a_log, d_skip, ssm_norm_g, w_attn_branch, w_ssm_branch,
                 gate_bias, w_out, norm2_g, router_w, router_b, expert_w1, expert_b1, expert_w2, expert_b2):
    Bsz, S, D = x.shape
    mod = c_act @ ada_w + ada_b
    shift1, scale1, gate1, shift2, scale2, gate2 = (m[:, None, :] for m in jnp.split(mod, 6, axis=-1))
    u = rms_norm(x, norm1_g) * (1.0 + scale1) + shift1
    proj = jnp.einsum('bsd,de->bse', u, w_in)
    c0 = 3 * ATTN_WIDTH
    cuts = [ATTN_WIDTH, 2 * ATTN_WIDTH, c0, c0 + SSM_INNER, c0 + SSM_INNER + SSM_CONV_DIM,
            c0 + SSM_INNER + SSM_CONV_DIM + SSM_HEADS]
    q, k, v, z, xbc, dt_raw, gate_logits = jnp.split(proj, cuts, axis=-1)
    heads = (Bsz, S, ATTN_HEADS, ATTN_HEAD_DIM)
    q = rms_norm(q.reshape(heads), q_norm_g)
    k = rms_norm(k.reshape(heads), k_norm_g)
    v = v.reshape(heads)
    attn = lax.map(lambda qkv: moba_attention_single(qkv[0], qkv[1], qkv[2], rel_bias_table), (q, k, v))
    attn = attn.reshape(Bsz, S, ATTN_WIDTH)
    ssm = mamba2_branch(z, xbc, dt_raw, conv_w, conv_b, dt_bias, a_log, d_skip, ssm_norm_g)
    gate_attn, gate_ssm = jnp.split(jax.nn.sigmoid(gate_logits + gate_bias), 2, axis=-1)
    merged = gate_attn * (attn @ w_attn_branch) + gate_ssm * (ssm @ w_ssm_branch)
    x = x + gate1 * (merged @ w_out)
    u2 = rms_norm(x, norm2_g) * (1.0 + scale2) + shift2
    ffn = moe_ffn(u2.reshape(Bsz * S, D), router_w, router_b, expert_w1, expert_b1, expert_w2, expert_b2)
    return x + gate2 * ffn.reshape(Bsz, S, D)


def setup_inputs(seed: int = 0) -> dict:
    key = jax.random.key(seed)
    ks = jax.random.split(key, 32)
    f32 = jnp.float32

    def nrm(k, shape, s):
        return jax.random.normal(k, shape, f32) * s

    L = DEPTH
    dt0 = jnp.exp(jax.random.uniform(ks[11], (L, SSM_HEADS), f32) * (math.log(0.1) - math.log(0.001)) + math.log(0.001))
    return {
        'x': nrm(ks[0], (BATCH, SEQ, D_MODEL), 1.0),
        'c': nrm(ks[1], (BATCH, D_MODEL), 1.0),
        'ada_w': nrm(ks[2], (L, D_MODEL, 6 * D_MODEL), D_MODEL ** -0.5),
        'ada_b': nrm(ks[3], (L, 6 * D_MODEL), 0.02),
        'norm1_g': 1.0 + nrm(ks[4], (L, D_MODEL), 0.02),
        'w_in': nrm(ks[5], (L, D_MODEL, IN_PROJ_DIM), D_MODEL ** -0.5),
        'q_norm_g': 1.0 + nrm(ks[6], (L, ATTN_HEAD_DIM), 0.02),
        'k_norm_g': 1.0 + nrm(ks[7], (L, ATTN_HEAD_DIM), 0.02),
        'rel_bias_table': nrm(ks[8], (REL_BUCKETS, ATTN_HEADS), 0.2),
        'conv_w': nrm(ks[9], (L, SSM_CONV, SSM_CONV_DIM), SSM_CONV ** -0.5),
        'conv_b': nrm(ks[10], (L, SSM_CONV_DIM), 0.02),
        'dt_bias': dt0 + jnp.log(-jnp.expm1(-dt0)),
        'a_log': jnp.log(jax.random.uniform(ks[12], (L, SSM_HEADS), f32, minval=1.0, maxval=16.0)),
        'd_skip': 1.0 + nrm(ks[13], (L, SSM_HEADS), 0.02),
        'ssm_norm_g': 1.0 + nrm(ks[14], (L, SSM_INNER), 0.02),
        'w_attn_branch': nrm(ks[15], (L, ATTN_WIDTH, D_MODEL), ATTN_WIDTH ** -0.5),
        'w_ssm_branch': nrm(ks[16], (L, SSM_INNER, D_MODEL), SSM_INNER ** -0.5),
        'gate_bias': nrm(ks[17], (L, N_BRANCHES * D_MODEL), 0.02),
        'w_out': nrm(ks[18], (L, D_MODEL, D_MODEL), D_MODEL ** -0.5),
        'norm2_g': 1.0 + nrm(ks[19], (L, D_MODEL), 0.02),
        'router_w': nrm(ks[20], (L, D_MODEL, N_EXPERTS), D_MODEL ** -0.5),
        'router_b': nrm(ks[21], (L, N_EXPERTS), 0.01),
        'expert_w1': nrm(ks[22], (L, N_EXPERTS, D_MODEL, 2 * EXPERT_FF), D_MODEL ** -0.5),
        'expert_b1': nrm(ks[23], (L, N_EXPERTS, 2 * EXPERT_FF), 0.01),
        'expert_w2': nrm(ks[24], (L, N_EXPERTS, EXPERT_FF, D_MODEL), EXPERT_FF ** -0.5),
        'expert_b2': nrm(ks[25], (L, N_EXPERTS, D_MODEL), 0.01),
    }


def reference(x, c, ada_w, ada_b, norm1_g, w_in, q_norm_g, k_norm_g, rel_bias_table, conv_w, conv_b,
              dt_bias, a_log, d_skip, ssm_norm_g, w_attn_branch, w_ssm_branch, gate_bias, w_out, norm2_g,
              router_w, router_b, expert_w1, expert_b1, expert_w2, expert_b2):
    c_act = jax.nn.silu(c)
    h = x
    for l in range(DEPTH):
        h = hybrid_layer(h, c_act, ada_w[l], ada_b[l], norm1_g[l], w_in[l], q_norm_g[l], k_norm_g[l],
                         rel_bias_table, conv_w[l], conv_b[l], dt_bias[l], a_log[l], d_skip[l],
                         ssm_norm_g[l], w_attn_branch[l], w_ssm_branch[l], gate_bias[l], w_out[l],
                         norm2_g[l], router_w[l], router_b[l], expert_w1[l], expert_b1[l],
                         expert_w2[l], expert_b2[l])
    return h
```

```python
import contextlib
import os
import math
import numpy as np
import concourse.bass as bass
import concourse.mybir as mybir
from concourse.bass_utils import run_bass_kernel_spmd

F32 = mybir.dt.float32
BF16 = mybir.dt.bfloat16
AF = mybir.ActivationFunctionType
ALU = mybir.AluOpType
AX = mybir.AxisListType

NCORES = 8
D = 1024
SEQ = 2048
NSEQ = 2
T = NSEQ * SEQ
NT = T // 128
H = 16
HD = 64
SSM_INNER = 2048
SSM_H = 32
SSM_G = 4
SSM_N = 128
CONV_DIM = 3072
NE = 32
FF = 1024
EPS = 1e-6
IN_PROJ = 10272
OFF_Q, OFF_K, OFF_V, OFF_Z, OFF_XBC, OFF_DT, OFF_GATE = 0, 1024, 2048, 3072, 5120, 8192, 8224
NEG = -30000.0


class Prog:
    def __init__(self, nc, es):
        self.nc, self.es = nc, es
        self.eng = {"pe": nc.tensor, "act": nc.scalar, "dve": nc.vector, "pool": nc.gpsimd, "sp": nc.sync}
        self.sems, self.val = {}, {}
        self.seen = {e: {} for e in self.eng}
        self.lastw, self.readers = {}, {}
        self.nins = 0
        self.free = []
        self.free_sw = []
        self.swkeys = set()
        self.nsem = 0

    def sem(self, key, sw=False):
        if key not in self.sems:
            fl = self.free_sw if sw else self.free
            if sw:
                self.swkeys.add(key)
            if fl:
                h, v = fl.pop()
                self.sems[key] = h
                self.val[key] = v
                for e in self.seen:
                    self.seen[e][key] = v
            else:
                self.nsem += 1
                self.sems[key] = self.es.enter_context(self.nc.semaphore("s%d" % self.nsem))
                self.val[key] = 0
        return self.sems[key]

    def sb(self, name, shape, dt=F32):
        return self.es.enter_context(self.nc.sbuf_tensor(name, list(shape), dt))

    def ps(self, name, shape, dt=F32):
        return self.es.enter_context(self.nc.psum_tensor(name, list(shape), dt))

    def op(self, eng, fn, reads=(), writes=(), dma=None):
        deps = {}
        for b in reads:
            for k, v in self.lastw.get(b, {}).items():
                deps[k] = max(deps.get(k, 0), v)
            if b.startswith("ps"):
                for k, v in self.readers.get(b, {}).items():
                    if k != eng:
                        deps[k] = max(deps.get(k, 0), v)
        for b in writes:
            for k, v in self.lastw.get(b, {}).items():
                deps[k] = max(deps.get(k, 0), v)
            for k, v in self.readers.get(b, {}).items():
                deps[k] = max(deps.get(k, 0), v)
        e = self.eng[eng]
        for k, v in deps.items():
            if dma is None and k == eng and eng == "pe":
                continue
            if self.seen[eng].get(k, 0) >= v:
                continue
            e.wait_ge(self.sems[k], v)
            self.seen[eng][k] = v
        ins = fn()
        if dma is None:
            key, inc = eng, 1
        else:
            key, inc = "dma:" + dma, 16
        s = self.sem(key, sw=(dma is not None and eng == "pool"))
        self.val[key] += inc
        ins.then_inc(s, inc)
        v = self.val[key]
        for b in writes:
            if dma is None:
                self.lastw[b] = {key: v}
            else:
                d_ = {k_: v_ for k_, v_ in self.lastw.get(b, {}).items() if k_.startswith("dma:")}
                d_[key] = v
                self.lastw[b] = d_
            self.readers[b] = {}
        for b in reads:
            self.readers.setdefault(b, {})[key] = v
        self.nins += 1
        return ins

    def barrier(self):
        for eng, e in self.eng.items():
            for k, v in self.val.items():
                if v > 0 and self.seen[eng].get(k, 0) < v:
                    e.wait_ge(self.sems[k], v)
                    self.seen[eng][k] = v
        for k in [k for k in self.sems if k.startswith("dma:")]:
            (self.free_sw if k in self.swkeys else self.free).append((self.sems.pop(k), self.val.pop(k)))
            self.swkeys.discard(k)
            for eng in self.seen:
                self.seen[eng].pop(k, None)
        self.lastw, self.readers = {}, {}

    def drain(self, eng, bufs):
        e = self.eng[eng]
        for b in bufs:
            for k, v in self.lastw.get(b, {}).items():
                if self.seen[eng].get(k, 0) < v:
                    e.wait_ge(self.sems[k], v)
                    self.seen[eng][k] = v


def bcast_rows(ap1d, n):
    return bass.AP(ap1d.tensor, ap1d.offset, [[0, 128], [1, n]])


def build_program(stages=("s0", "s1", "s3", "s4", "s5", "s6"), dbg=()):
    nc = bass.Bass("TRN2", target_bir_lowering=False)
    es = contextlib.ExitStack()

    def din(name, shape, dt=F32):
        return nc.dram_tensor(name, list(shape), dt, kind="ExternalInput").ap()

    def dscr(name, shape, dt=F32):
        if name in dbg:
            outs_to_drain.append(name)
            return nc.dram_tensor(name, list(shape), dt, kind="ExternalOutput").ap()
        return nc.dram_tensor(name, list(shape), dt, kind="Internal").ap()

    def dout(name, shape, dt=F32):
        return nc.dram_tensor(name, list(shape), dt, kind="ExternalOutput").ap()

    outs_to_drain = ["out"]
    x_d = din("x", [T, D])
    cT_d = din("cT", [D, NSEQ])
    ada_w = din("ada_w", [D, 6 * D])
    ada_b = din("ada_b", [6 * D])
    norm1_g = din("norm1_g", [D])
    w_in = din("w_in", [D, IN_PROJ])
    q_norm_g = din("q_norm_g", [HD])
    k_norm_g = din("k_norm_g", [HD])
    rel_tab = din("rel_tab", [33, H])
    conv_wT = din("conv_wT", [CONV_DIM, 4])
    conv_b = din("conv_b", [128, 24])
    dt_bias = din("dt_bias", [SSM_H])
    a_log = din("a_log", [SSM_H])
    d_skip = din("d_skip", [SSM_H])
    ssm_norm_g = din("ssm_norm_g", [SSM_INNER])
    w_attn = din("w_attn", [D, D])
    w_ssm = din("w_ssm", [SSM_INNER, D])
    gate_bias = din("gate_bias", [2 * D])
    w_out = din("w_out", [D, D])
    norm2_g = din("norm2_g", [D])
    router_w = din("router_w", [D, NE])
    router_b = din("router_b", [NE])
    w1 = din("w1", [NE, D, 2 * FF])
    b1T = din("b1T", [128, NE, 16])
    w2 = din("w2", [NE, FF, D])
    b2 = din("b2", [NE, D])
    c_ident = din("c_ident", [128, 128])
    c_rev = din("c_rev", [128, 128])
    c_tri = din("c_tri", [128, 128])
    c_negtri = din("c_negtri", [128, 128])
    c_bucket = din("c_bucket", [33, 2304])
    c_kind = din("c_kind", [8, SEQ])
    c_selmask = din("c_selmask", [128, 3, 16, 8])
    out_d = dout("out", [T, D])

    P = Prog(nc, es)
    with es:
        mod_d = dscr("mod_d", [NSEQ, 6 * D])
        qkv_d = dscr("qkv_d", [T, 3 * D])
        z_d = dscr("z_d", [T, SSM_INNER])
        dt_d = dscr("dt_d", [T, SSM_H])
        gl_d = dscr("gl_d", [T, 2 * D])
        xbcT_d = dscr("xbcT_d", [CONV_DIM, T])
        expf_d = dscr("expf_d", [H, 2304], BF16)
        wnat_d = dscr("wnat_d", [H, 128, SEQ], BF16)
        attnT_d = dscr("attnT_d", [D, T], BF16)
        ssmT_d = dscr("ssmT_d", [SSM_INNER, T], BF16)
        x1_d = dscr("x1_d", [T, D])
        u2T_d = dscr("u2T_d", [D, T], BF16)
        gw_d = dscr("gw_d", [NE, T])
        gwtm_d = dscr("gwtm_d", [T, NE])

        ident_f = P.sb("ident_f", [128, 128])
        rev_f = P.sb("rev_f", [128, 128])
        ident_b = P.sb("ident_b", [128, 128], BF16)
        rev_b = P.sb("rev_b", [128, 128], BF16)
        ones_b = P.sb("ones_b", [128, 128], BF16)
        ones_f = P.sb("ones_f", [128, 128])
        P.op("sp", lambda: nc.sync.dma_start(out=ident_f[:], in_=c_ident[:, :]), writes=["ident_f"], dma="ident_f")
        P.op("sp", lambda: nc.sync.dma_start(out=rev_f[:], in_=c_rev[:, :]), writes=["rev_f"], dma="rev_f")
        P.op("pool", lambda: nc.gpsimd.dma_start(out=ident_b[:], in_=c_ident[:, :]), writes=["ident_b"], dma="ident_b")
        P.op("pool", lambda: nc.gpsimd.dma_start(out=rev_b[:], in_=c_rev[:, :]), writes=["rev_b"], dma="rev_b")
        P.op("dve", lambda: nc.vector.memset(ones_b[:], 1.0), writes=["ones_b"])
        P.op("dve", lambda: nc.vector.memset(ones_f[:], 1.0), writes=["ones_f"])

        psb = [P.ps("psb%d" % i, [128, 512]) for i in range(8)]

        dbg_outs = {}

        def tap(name, shape, dt=F32):
            dbg_outs[name] = dout("dbg_" + name, shape, dt)
            outs_to_drain.append("dbg_" + name)
            return dbg_outs[name]

        if "s0" in stages:
            with contextlib.ExitStack() as st:
                sb = lambda n, s, d=F32: st.enter_context(nc.sbuf_tensor(n, list(s), d))
                cact = sb("cact", [128, 8, NSEQ])
                adab = sb("adab", [NSEQ, 6 * D])
                modsb = sb("modsb", [NSEQ, 6 * D])
                wb = [sb("s0w%d" % i, [128, 8, 512]) for i in range(2)]
                P.op("sp", lambda: nc.sync.dma_start(out=cact[:], in_=cT_d.rearrange("(k p) b -> p k b", p=128)),
                     writes=["cact"], dma="cact")
                P.op("sp", lambda: nc.sync.dma_start(out=adab[:], in_=bass.AP(ada_b.tensor, 0, [[0, NSEQ], [1, 6 * D]])),
                     writes=["adab"], dma="adab")
                P.op("act", lambda: nc.scalar.activation(out=cact[:], in_=cact[:], func=AF.Silu), reads=["cact"], writes=["cact"])
                for j in range(12):
                    w = wb[j % 2]
                    wk = "s0w%d" % (j % 2)
                    P.op("sp", lambda: nc.sync.dma_start(out=w[:], in_=ada_w[:, j * 512:(j + 1) * 512].rearrange("(k p) n -> p k n", p=128)),
                         writes=[wk], dma=wk)
                    pk = "ps%d" % (j % 2)
                    for k in range(8):
                        P.op("pe", lambda: nc.tensor.matmul(psb[j % 2][0:NSEQ, :], lhsT=cact[:, k, :], rhs=w[:, k, :], start=(k == 0), stop=(k == 7)),
                             reads=["cact", wk], writes=[pk])
                    P.op("dve", lambda: nc.vector.tensor_tensor(out=modsb[:, j * 512:(j + 1) * 512], in0=psb[j % 2][0:NSEQ, :],
                                                                in1=adab[:, j * 512:(j + 1) * 512], op=ALU.add),
                         reads=[pk, "adab"], writes=["modsb"])
                P.op("sp", lambda: nc.sync.dma_start(out=mod_d[:, :], in_=modsb[:]), reads=["modsb"], writes=["mod_d"], dma="modsb")
                if "mod" in dbg:
                    t_ = tap("mod", [NSEQ, 6 * D])
                    P.op("sp", lambda: nc.sync.dma_start(out=t_[:, :], in_=modsb[:]), reads=["modsb"], writes=["dbg_mod"], dma="modsb")
                tab = sb("tab", [33, H])
                oh = sb("oh", [33, 2304])
                ef = sb("ef", [H, 2304], BF16)
                P.op("sp", lambda: nc.sync.dma_start(out=tab[:], in_=rel_tab[:, :]), writes=["tab"], dma="tab")
                P.op("sp", lambda: nc.sync.dma_start(out=oh[:], in_=c_bucket[:, :]), writes=["oh"], dma="oh")
                for j in range(5):
                    n = 512 if j < 4 else 256
                    pk = "ps%d" % (2 + j % 2)
                    P.op("pe", lambda: nc.tensor.matmul(psb[2 + j % 2][0:H, 0:n], lhsT=tab[:], rhs=oh[:, j * 512:j * 512 + n], start=True, stop=True),
                         reads=["tab", "oh"], writes=[pk])
                    P.op("act", lambda: nc.scalar.activation(out=ef[:, j * 512:j * 512 + n], in_=psb[2 + j % 2][0:H, 0:n], func=AF.Exp),
                         reads=[pk], writes=["ef"])
                P.op("sp", lambda: nc.sync.dma_start(out=expf_d[:, :], in_=ef[:]), reads=["ef"], writes=["expf_d"], dma="ef")
                wrv = [sb("wrv%d" % i, [128, SEQ], BF16) for i in range(2)]
                wnt = [sb("wnt%d" % i, [128, SEQ], BF16) for i in range(2)]
                for h in range(H):
                    wr_, wrk = wrv[h % 2], "wrv%d" % (h % 2)
                    wn_, wnk = wnt[h % 2], "wnt%d" % (h % 2)
                    P.op("sp", lambda: nc.sync.dma_start(out=wr_[:], in_=bass.AP(expf_d.tensor, h * 2304, [[1, 128], [1, SEQ]])), reads=["expf_d"], writes=[wrk], dma=wrk)
                    for c in range(4):
                        pk = "ps%d" % (4 + c)
                        P.op("pe", lambda: nc.tensor.matmul(psb[4 + c][:, :], lhsT=rev_b[:], rhs=wr_[:, c * 512:(c + 1) * 512], start=True, stop=True), reads=["rev_b", wrk], writes=[pk])
                        if c % 2 == 0:
                            P.op("act", lambda: nc.scalar.copy(out=wn_[:, c * 512:(c + 1) * 512], in_=psb[4 + c][:, :]), reads=[pk], writes=[wnk])
                        else:
                            P.op("dve", lambda: nc.vector.tensor_copy(out=wn_[:, c * 512:(c + 1) * 512], in_=psb[4 + c][:, :]), reads=[pk], writes=[wnk])
                    P.op("sp", lambda: nc.sync.dma_start(out=wnat_d[h, :, :], in_=wn_[:]), reads=[wnk], writes=["wnat_d"], dma=wnk)
                P.barrier()

        GT = 1024
        NG = T // GT
        if "s1" in stages:
            with contextlib.ExitStack() as st:
                sb = lambda n, s, d=F32: st.enter_context(nc.sbuf_tensor(n, list(s), d))
                g1bc = sb("g1bc", [128, D])
                A1 = sb("A1", [128, D])
                sh1 = sb("sh1", [128, D])
                xt = [sb("xt%d" % i, [128, D]) for i in range(2)]
                ut = [sb("ut%d" % i, [128, D]) for i in range(2)]
                sq = sb("sq", [128, D])
                ss = sb("ss", [128, 2])
                uT = sb("uT", [128, 8, GT], BF16)
                wbuf = [sb("wbuf%d" % i, [128, 8, 512], BF16) for i in range(2)]
                stg = [sb("stg%d" % i, [128, 512]) for i in range(2)]
                raw = [sb("raw%d" % i, [128, GT + 3]) for i in range(2)]
                cacc = [sb("cacc%d" % i, [128, GT]) for i in range(2)]
                carry = sb("carry", [128, 24, 3])
                cw = sb("cw", [128, 24, 4])
                cb = sb("cb", [128, 24])
                P.op("sp", lambda: nc.sync.dma_start(out=g1bc[:], in_=bcast_rows(norm1_g, D)), writes=["g1bc"], dma="g1bc")
                P.op("sp", lambda: nc.sync.dma_start(out=cw[:], in_=conv_wT.rearrange("(m p) k -> p m k", p=128)), writes=["cw"], dma="cw")
                P.op("sp", lambda: nc.sync.dma_start(out=cb[:], in_=conv_b[:, :]), writes=["cb"], dma="cb")
                chunks = []
                for j in range(6):
                    chunks.append((OFF_Q + 512 * j, 512, "tm", qkv_d, 512 * j))
                for j in range(4):
                    chunks.append((OFF_Z + 512 * j, 512, "tm", z_d, 512 * j))
                for j in range(6):
                    chunks.append((OFF_XBC + 512 * j, 512, "fm", None, j))
                chunks.append((OFF_DT, 32, "tm", dt_d, 0))
                for j in range(4):
                    chunks.append((OFF_GATE + 512 * j, 512, "tm", gl_d, 512 * j))
                wcount = 0
                scount = 0
                rcount = 0
                for g in range(NG):
                    s = (g * GT) // SEQ
                    if (g * GT) % SEQ == 0:
                        P.op("sp", lambda: nc.sync.dma_start(out=A1[:], in_=bcast_rows(mod_d[s, D:2 * D], D)), reads=["mod_d"], writes=["A1"], dma="A1")
                        P.op("sp", lambda: nc.sync.dma_start(out=sh1[:], in_=bcast_rows(mod_d[s, 0:D], D)), reads=["mod_d"], writes=["sh1"], dma="sh1")
                        P.op("dve", lambda: nc.vector.scalar_tensor_tensor(out=A1[:], in0=A1[:], scalar=1.0, in1=g1bc[:], op0=ALU.add, op1=ALU.mult),
                             reads=["A1", "g1bc"], writes=["A1"])
                        P.op("dve", lambda: nc.vector.memset(carry[:], 0.0), writes=["carry"])
                    for tt in range(GT // 128):
                        t = g * (GT // 128) + tt
                        xb_, xk = xt[t % 2], "xt%d" % (t % 2)
                        ub_, uk = ut[t % 2], "ut%d" % (t % 2)
                        P.op("sp", lambda: nc.sync.dma_start(out=xb_[:], in_=x_d[t * 128:(t + 1) * 128, :]), writes=[xk], dma=xk)
                        P.op("act", lambda: nc.scalar.activation(out=sq[:], in_=xb_[:], func=AF.Square, scale=1.0 / 32.0, accum_out=ss[:, 0:1]),
                             reads=[xk], writes=["sq", "ss"])
                        P.op("dve", lambda: nc.vector.tensor_scalar_add(out=ss[:, 1:2], in0=ss[:, 0:1], scalar1=EPS), reads=["ss"], writes=["ss"])
                        P.op("act", lambda: nc.scalar.sqrt(out=ss[:, 1:2], in_=ss[:, 1:2]), reads=["ss"], writes=["ss"])
                        P.op("dve", lambda: nc.vector.reciprocal(out=ss[:, 1:2], in_=ss[:, 1:2]), reads=["ss"], writes=["ss"])
                        P.op("dve", lambda: nc.vector.scalar_tensor_tensor(out=ub_[:], in0=xb_[:], scalar=ss[:, 1:2], in1=A1[:], op0=ALU.mult, op1=ALU.mult),
                             reads=[xk, "ss", "A1"], writes=[uk])
                        P.op("dve", lambda: nc.vector.tensor_tensor(out=ub_[:], in0=ub_[:], in1=sh1[:], op=ALU.add), reads=[uk, "sh1"], writes=[uk])
                        if "u" in dbg and t < 2:
                            if "u" not in dbg_outs:
                                tap("u", [256, D])
                            P.op("sp", lambda: nc.sync.dma_start(out=dbg_outs["u"][t * 128:(t + 1) * 128, :], in_=ub_[:]), reads=[uk], writes=["dbg_u"], dma=uk)
                        for half in range(2):
                            pb, pk = psb[half], "ps%d" % half
                            for kk in range(4):
                                k = half * 4 + kk
                                P.op("pe", lambda: nc.tensor.matmul(pb[:, kk * 128:(kk + 1) * 128], lhsT=ub_[:, k * 128:(k + 1) * 128], rhs=ident_f[:],
                                                                    start=True, stop=True), reads=[uk, "ident_f"], writes=[pk])
                            P.op("act", lambda: nc.scalar.copy(out=uT[:, half * 4:half * 4 + 4, tt * 128:(tt + 1) * 128],
                                                               in_=pb[:].rearrange("p (k t) -> p k t", k=4)), reads=[pk], writes=["uT"])
                    for (c0, ncol, kind, dest, dcol) in chunks:
                        wb_, wk = wbuf[wcount % 2], "wbuf%d" % (wcount % 2)
                        wcount += 1
                        P.op("pool", lambda: nc.gpsimd.dma_start(out=wb_[:, :, 0:ncol], in_=w_in[:, c0:c0 + ncol].rearrange("(k p) n -> p k n", p=128)),
                             writes=[wk], dma=wk)
                        if kind == "tm":
                            for tt in range(GT // 128):
                                t = g * (GT // 128) + tt
                                pi = 2 + (scount % 4)
                                pb, pk = psb[pi], "ps%d" % pi
                                sg, sk = stg[scount % 2], "stg%d" % (scount % 2)
                                scount += 1
                                for k in range(8):
                                    P.op("pe", lambda: nc.tensor.matmul(pb[:, 0:ncol], lhsT=uT[:, k, tt * 128:(tt + 1) * 128], rhs=wb_[:, k, 0:ncol],
                                                                        start=(k == 0), stop=(k == 7)), reads=["uT", wk], writes=[pk])
                                ev = "act" if scount % 2 else "dve"
                                if ev == "act":
                                    P.op("act", lambda: nc.scalar.copy(out=sg[:, 0:ncol], in_=pb[:, 0:ncol]), reads=[pk], writes=[sk])
                                else:
                                    P.op("dve", lambda: nc.vector.tensor_copy(out=sg[:, 0:ncol], in_=pb[:, 0:ncol]), reads=[pk], writes=[sk])
                                P.op("sp", lambda: nc.sync.dma_start(out=dest[t * 128:(t + 1) * 128, dcol:dcol + ncol], in_=sg[:, 0:ncol]),
                                     reads=[sk], writes=[dest.tensor.name], dma=sk)
                        else:
                            for mm in range(4):
                                m = dcol * 4 + mm
                                rw, rk = raw[rcount % 2], "raw%d" % (rcount % 2)
                                ca, ck = cacc[rcount % 2], "cacc%d" % (rcount % 2)
                                rcount += 1
                                P.op("dve", lambda: nc.vector.tensor_copy(out=rw[:, 0:3], in_=carry[:, m, :]), reads=["carry"], writes=[rk])
                                for hf in range(GT // 512):
                                    pi = 6 + (hf % 2)
                                    pb, pk = psb[pi], "ps%d" % pi
                                    for k in range(8):
                                        P.op("pe", lambda: nc.tensor.matmul(pb[:, :], lhsT=wb_[:, k, mm * 128:(mm + 1) * 128], rhs=uT[:, k, hf * 512:(hf + 1) * 512],
                                                                            start=(k == 0), stop=(k == 7)), reads=["uT", wk], writes=[pk])
                                    P.op("act", lambda: nc.scalar.copy(out=rw[:, 3 + hf * 512:3 + (hf + 1) * 512], in_=pb[:, :]), reads=[pk], writes=[rk])
                                P.op("dve", lambda: nc.vector.tensor_copy(out=carry[:, m, :], in_=rw[:, GT:GT + 3]), reads=[rk], writes=["carry"])
                                P.op("dve", lambda: nc.vector.tensor_scalar(out=ca[:], in0=rw[:, 3:GT + 3], scalar1=cw[:, m, 3:4], scalar2=cb[:, m:m + 1],
                                                                            op0=ALU.mult, op1=ALU.add), reads=[rk, "cw", "cb"], writes=[ck])
                                for tap_ in range(3):
                                    P.op("dve", lambda: nc.vector.scalar_tensor_tensor(out=ca[:], in0=rw[:, tap_:GT + tap_], scalar=cw[:, m, tap_:tap_ + 1], in1=ca[:],
                                                                                       op0=ALU.mult, op1=ALU.add), reads=[rk, ck, "cw"], writes=[ck])
                                P.op("act", lambda: nc.scalar.activation(out=ca[:], in_=ca[:], func=AF.Silu), reads=[ck], writes=[ck])
                                P.op("sp", lambda: nc.sync.dma_start(out=xbcT_d[m * 128:(m + 1) * 128, g * GT:(g + 1) * GT], in_=ca[:]),
                                     reads=[ck], writes=["xbcT_d"], dma=ck)
                P.barrier()

        if "s3" in stages:
            with contextlib.ExitStack() as st:
                sb = lambda n, s, d=F32: st.enter_context(nc.sbuf_tensor(n, list(s), d))
                gq = sb("gq", [128, HD]); gk = sb("gk", [128, HD])
                selm = sb("selm", [128, 3, 16, 8])
                qf = [sb("qf%d" % i, [128, 16, HD]) for i in range(2)]
                kf = [sb("kf%d" % i, [128, 16, HD]) for i in range(2)]
                vf = [sb("vf%d" % i, [128, 16, HD]) for i in range(2)]
                sqt = sb("sqt", [128, 16, HD])
                nrm = sb("nrm", [128, 2, 16])
                qTg = sb("qTg", [64, SEQ], BF16)
                kmT = sb("kmT", [64, 8]); kmb = sb("kmb", [64, 8], BF16)
                gate = sb("gate", [128, 16, 8]); cmpb = sb("cmpb", [128, 16, 8, 8]); rank = sb("rank", [128, 16, 8])
                qaug = sb("qaug", [128, 16, 72], BF16); kb16 = sb("kb16", [128, 16, HD], BF16)
                qTa = [sb("qTa%d" % i, [72, SEQ], BF16) for i in range(2)]
                kTa = [sb("kTa%d" % i, [72, SEQ], BF16) for i in range(2)]
                vaug = [sb("vaug%d" % i, [128, 16, 128], BF16) for i in range(2)]
                Wt = [sb("Wt%d" % i, [128, SEQ], BF16) for i in range(2)]
                pS = [sb("pS%d" % i, [128, 512], BF16) for i in range(3)]
                pW = [sb("pW%d" % i, [128, 512], BF16) for i in range(3)]
                rec = sb("rec", [128, SEQ])
                aT = [sb("aT%d" % i, [64, SEQ], BF16) for i in range(2)]
                P.op("sp", lambda: nc.sync.dma_start(out=gq[:], in_=bcast_rows(q_norm_g, HD)), writes=["gq"], dma="gq")
                P.op("sp", lambda: nc.sync.dma_start(out=gk[:], in_=bcast_rows(k_norm_g, HD)), writes=["gk"], dma="gk")
                P.op("sp", lambda: nc.sync.dma_start(out=selm[:], in_=c_selmask[:, :, :, :]), writes=["selm"], dma="selm")
                for i in range(2):
                    P.op("dve", lambda: nc.vector.memset(vaug[i][:], 1.0), writes=["vaug%d" % i])
                    P.op("pool", lambda: nc.gpsimd.dma_start(out=kTa[i][64:72, :], in_=c_kind[:, :]), writes=["kTa%d" % i], dma="kTa%d" % i)

                def prologue(idx):
                    s_, h = divmod(idx, H)
                    b2_ = idx % 2
                    r0 = s_ * SEQ
                    q_, k_, v_ = qf[b2_], kf[b2_], vf[b2_]
                    qk_, kk_, vk_ = "qf%d" % b2_, "kf%d" % b2_, "vf%d" % b2_
                    qTak, kTak, vak = "qTa%d" % b2_, "kTa%d" % b2_, "vaug%d" % b2_
                    for (buf, nm, off) in [(qf, "qf", OFF_Q), (kf, "kf", OFF_K), (vf, "vf", OFF_V)]:
                        P.op("sp", lambda: nc.sync.dma_start(out=buf[b2_][:], in_=qkv_d[r0:r0 + SEQ, off + h * HD:off + (h + 1) * HD].rearrange("(t p) d -> p t d", p=128)),
                             reads=["qkv_d"], writes=["%s%d" % (nm, b2_)], dma="%s%d" % (nm, b2_))
                    P.op("sp", lambda: nc.sync.dma_start(out=Wt[b2_][:], in_=wnat_d[h, :, :]), reads=["wnat_d"], writes=["Wt%d" % b2_], dma="Wt%d" % b2_)
                    P.op("act", lambda: nc.scalar.copy(out=vaug[b2_][:, :, 0:64], in_=v_[:]), reads=[vk_], writes=[vak])
                    for j, (t_, tk_, g_, gk_) in enumerate([(q_, qk_, gq, "gq"), (k_, kk_, gk, "gk")]):
                        P.op("act", lambda: nc.scalar.activation(out=sqt[:], in_=t_[:], func=AF.Square, scale=0.125), reads=[tk_], writes=["sqt"])
                        P.op("dve", lambda: nc.vector.reduce_sum(out=nrm[:, j, :], in_=sqt[:], axis=AX.X), reads=["sqt"], writes=["nrm"])
                        P.op("dve", lambda: nc.vector.tensor_scalar_add(out=nrm[:, j, :], in0=nrm[:, j, :], scalar1=EPS), reads=["nrm"], writes=["nrm"])
                        P.op("act", lambda: nc.scalar.sqrt(out=nrm[:, j, :], in_=nrm[:, j, :]), reads=["nrm"], writes=["nrm"])
                        P.op("dve", lambda: nc.vector.reciprocal(out=nrm[:, j, :], in_=nrm[:, j, :]), reads=["nrm"], writes=["nrm"])
                        P.op("dve", lambda: nc.vector.tensor_tensor(out=t_[:], in0=t_[:], in1=nrm[:, j, :].unsqueeze(2).to_broadcast([128, 16, HD]), op=ALU.mult),
                             reads=[tk_, "nrm"], writes=[tk_])
                        dst_, dk_ = (qaug[:, :, 0:64], "qaug") if j == 0 else (kb16[:], "kb16")
                        P.op("dve", lambda: nc.vector.tensor_tensor(out=dst_, in0=t_[:], in1=g_[:].unsqueeze(1).to_broadcast([128, 16, HD]), op=ALU.mult),
                             reads=[tk_, gk_], writes=[dk_])
                    for i in range(4):
                        for tt in range(4):
                            P.op("pe", lambda: nc.tensor.matmul(psb[7][0:64, tt * 128:(tt + 1) * 128], lhsT=kb16[:, 4 * i + tt, :], rhs=ident_b[:], start=True, stop=True),
                                 reads=["kb16", "ident_b"], writes=["ps7"])
                        P.op("dve", lambda: nc.vector.reduce_sum(out=kmT[:, 2 * i:2 * i + 2], in_=psb[7][0:64, :].rearrange("p (b k) -> p b k", b=2), axis=AX.X),
                             reads=["ps7"], writes=["kmT"])
                        P.op("dve", lambda: nc.vector.tensor_copy(out=kTa[b2_][0:64, i * 512:(i + 1) * 512], in_=psb[7][0:64, :]), reads=["ps7"], writes=[kTak])
                    P.op("dve", lambda: nc.vector.tensor_scalar_mul(out=kmb[:], in0=kmT[:], scalar1=1.0 / 256.0), reads=["kmT"], writes=["kmb"])
                    for i in range(4):
                        for tt in range(4):
                            P.op("pe", lambda: nc.tensor.matmul(psb[7][0:64, tt * 128:(tt + 1) * 128], lhsT=qaug[:, 4 * i + tt, 0:64], rhs=ident_b[:], start=True, stop=True),
                                 reads=["qaug", "ident_b"], writes=["ps7"])
                        P.op("act", lambda: nc.scalar.copy(out=qTg[:, i * 512:(i + 1) * 512], in_=psb[7][0:64, :]), reads=["ps7"], writes=["qTg"])
                    for t in range(16):
                        P.op("pe", lambda: nc.tensor.matmul(psb[7][:, t * 8:(t + 1) * 8], lhsT=qTg[:, t * 128:(t + 1) * 128], rhs=kmb[:], start=True, stop=True),
                             reads=["qTg", "kmb"], writes=["ps7"])
                    P.op("dve", lambda: nc.vector.tensor_tensor(out=gate[:], in0=psb[7][:, 0:128].rearrange("p (t n) -> p t n", t=16), in1=selm[:, 0, :, :], op=ALU.add),
                         reads=["ps7", "selm"], writes=["gate"])
                    P.op("dve", lambda: nc.vector.tensor_tensor(out=cmpb[:], in0=gate[:].unsqueeze(2).to_broadcast([128, 16, 8, 8]),
                                                                in1=gate[:].unsqueeze(3).to_broadcast([128, 16, 8, 8]), op=ALU.is_gt), reads=["gate"], writes=["cmpb"])
                    P.op("dve", lambda: nc.vector.reduce_sum(out=rank[:], in_=cmpb[:], axis=AX.X), reads=["cmpb"], writes=["rank"])
                    P.op("dve", lambda: nc.vector.tensor_single_scalar(out=rank[:], in_=rank[:], scalar=3.0, op=ALU.is_lt), reads=["rank"], writes=["rank"])
                    P.op("dve", lambda: nc.vector.tensor_tensor(out=rank[:], in0=rank[:], in1=selm[:, 1, :, :], op=ALU.mult), reads=["rank", "selm"], writes=["rank"])
                    P.op("dve", lambda: nc.vector.tensor_tensor(out=rank[:], in0=rank[:], in1=selm[:, 2, :, :], op=ALU.add), reads=["rank", "selm"], writes=["rank"])
                    P.op("dve", lambda: nc.vector.tensor_scalar(out=qaug[:, :, 64:72], in0=rank[:], scalar1=-NEG, scalar2=NEG, op0=ALU.mult, op1=ALU.add),
                         reads=["rank"], writes=["qaug"])
                    for i in range(4):
                        for tt in range(4):
                            P.op("pe", lambda: nc.tensor.matmul(psb[7][0:72, tt * 128:(tt + 1) * 128], lhsT=qaug[:, 4 * i + tt, :], rhs=ident_b[:], start=True, stop=True),
                                 reads=["qaug", "ident_b"], writes=["ps7"])
                        P.op("act", lambda: nc.scalar.copy(out=qTa[b2_][:, i * 512:(i + 1) * 512], in_=psb[7][0:72, :]), reads=["ps7"], writes=[qTak])

                cnt3 = [0]

                def main(idx, inject):
                    s_, h = divmod(idx, H)
                    b2_ = idx % 2
                    r0 = s_ * SEQ
                    qTak, kTak, vak, wk_ = "qTa%d" % b2_, "kTa%d" % b2_, "vaug%d" % b2_, "Wt%d" % b2_
                    its = []
                    for kt in range(16):
                        k0 = kt * 128
                        for c in range(k0 // 512, 4):
                            its.append((kt, k0, c, max(k0, 512 * c), 512 * (c + 1)))

                    def emit_s(j):
                        kt, k0, c, q_lo, q_hi = its[j]
                        sbk = 4 + (base + j) % 3
                        P.op("pe", lambda: nc.tensor.matmul(psb[sbk][:, 0:q_hi - q_lo], lhsT=kTa[b2_][:, k0:k0 + 128], rhs=qTa[b2_][:, q_lo:q_hi], start=True, stop=True),
                             reads=[kTak, qTak], writes=["ps%d" % sbk])

                    base = cnt3[0]
                    cnt3[0] += len(its)
                    LOOK = 2
                    for j in range(min(LOOK, len(its))):
                        emit_s(j)
                    for j in range(len(its)):
                        kt, k0, c, q_lo, q_hi = its[j]
                        n = q_hi - q_lo
                        cn = base + j
                        sbk = 4 + cn % 3
                        ps_, pw_ = pS[cn % 3], pW[cn % 3]
                        psk, pwk = "pS%d" % (cn % 3), "pW%d" % (cn % 3)
                        if j + LOOK < len(its):
                            emit_s(j + LOOK)
                        P.op("act", lambda: nc.scalar.activation(out=ps_[:, 0:n], in_=psb[sbk][:, 0:n], func=AF.Exp, scale=0.125), reads=["ps%d" % sbk], writes=[psk])
                        P.op("dve", lambda: nc.vector.tensor_tensor(out=pw_[:, 0:n], in0=ps_[:, 0:n], in1=Wt[b2_][:, q_lo - k0:q_hi - k0], op=ALU.mult),
                             reads=[psk, wk_], writes=[pwk])
                        P.op("pe", lambda: nc.tensor.matmul(psb[c][:, q_lo - 512 * c:q_hi - 512 * c], lhsT=vaug[b2_][:, kt, :], rhs=pw_[:, 0:n],
                                                            start=(kt == 0), stop=(kt == 4 * c + 3), skip_group_check=True), reads=[vak, pwk], writes=["ps%d" % c])
                        if j == 12:
                            inject()
                    for c in range(4):
                        P.op("act", lambda: nc.scalar.activation(out=rec[64:128, c * 512:(c + 1) * 512], in_=psb[c][64:128, :], func=AF.Ln), reads=["ps%d" % c], writes=["rec"])
                        P.op("act", lambda: nc.scalar.activation(out=rec[64:128, c * 512:(c + 1) * 512], in_=rec[64:128, c * 512:(c + 1) * 512], func=AF.Exp, scale=-1.0), reads=["rec"], writes=["rec"])
                        P.op("dve", lambda: nc.vector.tensor_tensor(out=aT[b2_][:, c * 512:(c + 1) * 512], in0=psb[c][0:64, :], in1=rec[64:128, c * 512:(c + 1) * 512], op=ALU.mult),
                             reads=["ps%d" % c, "rec"], writes=["aT%d" % b2_])
                    P.op("sp", lambda: nc.sync.dma_start(out=attnT_d[h * HD:(h + 1) * HD, r0:r0 + SEQ], in_=aT[b2_][:]), reads=["aT%d" % b2_], writes=["attnT_d"], dma="aT%d" % b2_)

                NHD = NSEQ * H
                prologue(0)
                for idx in range(NHD):
                    main(idx, (lambda i=idx: prologue(i + 1)) if idx + 1 < NHD else (lambda: None))
                P.barrier()

        if "s4" in stages:
            with contextlib.ExitStack() as st:
                sb = lambda n, s, d=F32: st.enter_context(nc.sbuf_tensor(n, list(s), d))
                tri = sb("tri", [128, 128]); negtri = sb("negtri", [128, 128])
                dtb = sb("dtb", [128, SSM_H]); abc = sb("abc", [128, SSM_H]); dsk = sb("dsk", [128, SSM_H])
                sng = sb("sng", [128, SSM_INNER])
                xsT = [sb("xsT%d" % i, [128, 16, 128]) for i in range(2)]
                BTb = [sb("BTb%d" % i, [128, 4, 128], BF16) for i in range(2)]
                CTb = [sb("CTb%d" % i, [128, 4, 128], BF16) for i in range(2)]
                zt = [sb("zt%d" % i, [128, SSM_INNER]) for i in range(2)]
                dtr = [sb("dtr%d" % i, [128, SSM_H]) for i in range(2)]
                sm = sb("sm", [128, 13, SSM_H])
                Rb = sb("Rb", [128, SSM_H, 128])
                xs = sb("xs", [128, SSM_INNER])
                xdt = sb("xdt", [128, SSM_INNER], BF16); xdte = sb("xdte", [128, SSM_INNER], BF16)
                Btm = sb("Btm", [128, 4, 128], BF16)
                Dm = [sb("Dm%d" % i, [128, 4, 128]) for i in range(2)]
                MT = [sb("MT%d" % i, [128, 4, 128], BF16) for i in range(2)]
                Hf = sb("Hf", [128, SSM_INNER]); Hb = sb("Hb", [128, SSM_INNER], BF16)
                ysb = sb("ysb", [128, SSM_INNER]); tmp = sb("tmp4", [128, SSM_INNER])
                nr4 = sb("nr4", [128, 8])
                ssb = sb("ssb", [128, SSM_INNER], BF16)
                sT = [sb("sT%d" % i, [128, 16, 512], BF16) for i in range(2)]
                P.op("sp", lambda: nc.sync.dma_start(out=tri[:], in_=c_tri[:, :]), writes=["tri"], dma="tri")
                P.op("sp", lambda: nc.sync.dma_start(out=negtri[:], in_=c_negtri[:, :]), writes=["negtri"], dma="negtri")
                P.op("sp", lambda: nc.sync.dma_start(out=dtb[:], in_=bcast_rows(dt_bias, SSM_H)), writes=["dtb"], dma="dtb")
                P.op("sp", lambda: nc.sync.dma_start(out=abc[:], in_=bcast_rows(a_log, SSM_H)), writes=["abc"], dma="abc")
                P.op("sp", lambda: nc.sync.dma_start(out=dsk[:], in_=bcast_rows(d_skip, SSM_H)), writes=["dsk"], dma="dsk")
                P.op("sp", lambda: nc.sync.dma_start(out=sng[:], in_=bcast_rows(ssm_norm_g, SSM_INNER)), writes=["sng"], dma="sng")
                P.op("act", lambda: nc.scalar.activation(out=abc[:], in_=abc[:], func=AF.Exp), reads=["abc"], writes=["abc"])
                P.op("dve", lambda: nc.vector.tensor_scalar_mul(out=abc[:], in0=abc[:], scalar1=-1.0), reads=["abc"], writes=["abc"])
                V_ = lambda i: sm[:, i, :]
                bc3 = lambda ap, n, w: ap.unsqueeze(2).to_broadcast([128, n, w])
                for s_ in range(NSEQ):
                    P.op("dve", lambda: nc.vector.memset(Hf[:], 0.0), writes=["Hf"])
                    P.op("dve", lambda: nc.vector.memset(Hb[:], 0.0), writes=["Hb"])
                    for c in range(16):
                        cc = s_ * 16 + c
                        b2_ = cc % 2
                        t0 = cc * 128
                        P.op("sp", lambda: nc.sync.dma_start(out=xsT[b2_][:], in_=xbcT_d[0:2048, t0:t0 + 128].rearrange("(m p) t -> p m t", p=128)),
                             reads=["xbcT_d"], writes=["xsT%d" % b2_], dma="xsT%d" % b2_)
                        P.op("pool", lambda: nc.gpsimd.dma_start(out=BTb[b2_][:], in_=xbcT_d[2048:2560, t0:t0 + 128].rearrange("(m p) t -> p m t", p=128)),
                             reads=["xbcT_d"], writes=["BTb%d" % b2_], dma="BTb%d" % b2_)
                        P.op("pool", lambda: nc.gpsimd.dma_start(out=CTb[b2_][:], in_=xbcT_d[2560:3072, t0:t0 + 128].rearrange("(m p) t -> p m t", p=128)),
                             reads=["xbcT_d"], writes=["CTb%d" % b2_], dma="CTb%d" % b2_)
                        P.op("sp", lambda: nc.sync.dma_start(out=zt[b2_][:], in_=z_d[t0:t0 + 128, :]), reads=["z_d"], writes=["zt%d" % b2_], dma="zt%d" % b2_)
                        P.op("sp", lambda: nc.sync.dma_start(out=dtr[b2_][:], in_=dt_d[t0:t0 + 128, :]), reads=["dt_d"], writes=["dtr%d" % b2_], dma="dtr%d" % b2_)
                        xk, bk, ck, zk, dk = "xsT%d" % b2_, "BTb%d" % b2_, "CTb%d" % b2_, "zt%d" % b2_, "dtr%d" % b2_
                        P.op("dve", lambda: nc.vector.tensor_tensor(out=V_(0), in0=dtr[b2_][:], in1=dtb[:], op=ALU.add), reads=[dk, "dtb"], writes=["sm0"])
                        P.op("act", lambda: nc.scalar.activation(out=V_(1), in_=V_(0), func=AF.Abs), reads=["sm0"], writes=["sm1"])
                        P.op("act", lambda: nc.scalar.activation(out=V_(2), in_=V_(1), func=AF.Exp, scale=-1.0), reads=["sm1"], writes=["sm2"])
                        P.op("act", lambda: nc.scalar.activation(out=V_(2), in_=V_(2), func=AF.Ln, bias=1.0), reads=["sm2"], writes=["sm2"])
                        P.op("dve", lambda: nc.vector.scalar_tensor_tensor(out=V_(3), in0=V_(0), scalar=0.0, in1=V_(2), op0=ALU.max, op1=ALU.add), reads=["sm0", "sm2"], writes=["sm3"])
                        P.op("dve", lambda: nc.vector.tensor_tensor(out=V_(4), in0=V_(3), in1=abc[:], op=ALU.mult), reads=["sm3", "abc"], writes=["sm4"])
                        P.op("pe", lambda: nc.tensor.matmul(psb[0][:, 0:32], lhsT=tri[:], rhs=V_(4), start=True, stop=True), reads=["tri", "sm4"], writes=["ps0"])
                        P.op("pe", lambda: nc.tensor.matmul(psb[0][:, 32:64], lhsT=ones_f[:], rhs=V_(4), start=True, stop=True), reads=["ones_f", "sm4"], writes=["ps0"])
                        P.op("act", lambda: nc.scalar.copy(out=sm[:, 5:7, :], in_=psb[0][:, 0:64].rearrange("p (a h) -> p a h", a=2)), reads=["ps0"], writes=["sm5", "sm6"])
                        P.op("dve", lambda: nc.vector.tensor_tensor(out=V_(7), in0=V_(6), in1=V_(5), op=ALU.subtract), reads=["sm5", "sm6"], writes=["sm7"])
                        P.op("act", lambda: nc.scalar.activation(out=sm[:, 10:13, :], in_=sm[:, 5:8, :], func=AF.Exp), reads=["sm5", "sm6", "sm7"], writes=["sm10", "sm11", "sm12"])
                        EACS, CD, DTE = 10, 11, 12
                        P.op("dve", lambda: nc.vector.tensor_tensor(out=Rb[:], in0=tri[:].unsqueeze(1).to_broadcast([128, SSM_H, 128]), in1=bc3(V_(4), SSM_H, 128), op=ALU.mult),
                             reads=["tri", "sm4"], writes=["Rb"])
                        for m in range(16):
                            P.op("pe", lambda: nc.tensor.matmul(psb[3 + m // 4][:, (m % 4) * 128:(m % 4 + 1) * 128], lhsT=xsT[b2_][:, m, :], rhs=ident_f[:], start=True, stop=True),
                                 reads=[xk, "ident_f"], writes=["ps%d" % (3 + m // 4)])
                        for i in range(4):
                            P.op("act", lambda: nc.scalar.copy(out=xs[:, i * 512:(i + 1) * 512], in_=psb[3 + i][:, :]), reads=["ps%d" % (3 + i)], writes=["xs"])
                        xs3 = xs[:].rearrange("p (h d) -> p h d", h=SSM_H)
                        P.op("dve", lambda: nc.vector.tensor_tensor(out=xdt[:].rearrange("p (h d) -> p h d", h=SSM_H), in0=xs3, in1=bc3(V_(3), SSM_H, 64), op=ALU.mult),
                             reads=["xs", "sm3"], writes=["xdt"])
                        P.op("dve", lambda: nc.vector.tensor_tensor(out=xdte[:].rearrange("p (h d) -> p h d", h=SSM_H), in0=xdt[:].rearrange("p (h d) -> p h d", h=SSM_H),
                                                                    in1=bc3(V_(DTE), SSM_H, 64), op=ALU.mult), reads=["xdt", "sm12"], writes=["xdte"])
                        for g in range(4):
                            P.op("pe", lambda: nc.tensor.matmul(psb[7][:, g * 128:(g + 1) * 128], lhsT=BTb[b2_][:, g, :], rhs=ident_b[:], start=True, stop=True),
                                 reads=[bk, "ident_b"], writes=["ps7"])
                        P.op("act", lambda: nc.scalar.copy(out=Btm[:], in_=psb[7][:, :].rearrange("p (g n) -> p g n", g=4)), reads=["ps7"], writes=["Btm"])
                        for g in range(4):
                            P.op("pe", lambda: nc.tensor.matmul(psb[2][:, g * 128:(g + 1) * 128], lhsT=BTb[b2_][:, g, :], rhs=CTb[b2_][:, g, :], start=True, stop=True),
                                 reads=[bk, ck], writes=["ps2"])
                        for g in range(4):
                            for j2 in range(2):
                                j = g * 2 + j2
                                pb = j % 2
                                P.op("pe", lambda: nc.tensor.matmul(psb[pb][:, :], lhsT=ones_f[:], rhs=Rb[:, 4 * j:4 * j + 4, :], start=True, stop=True),
                                     reads=["ones_f", "Rb"], writes=["ps%d" % pb])
                                dm, dmk = Dm[j % 2], "Dm%d" % (j % 2)
                                mt, mtk = MT[j % 2], "MT%d" % (j % 2)
                                P.op("dve", lambda: nc.vector.tensor_tensor(out=dm[:], in0=psb[pb][:, :].rearrange("p (h l) -> p h l", h=4), in1=bc3(sm[:, 5, 4 * j:4 * j + 4], 4, 128), op=ALU.subtract),
                                     reads=["ps%d" % pb, "sm5"], writes=[dmk])
                                P.op("dve", lambda: nc.vector.tensor_tensor(out=dm[:], in0=dm[:], in1=negtri[:].unsqueeze(1).to_broadcast([128, 4, 128]), op=ALU.add),
                                     reads=[dmk, "negtri"], writes=[dmk])
                                P.op("act", lambda: nc.scalar.activation(out=dm[:], in_=dm[:], func=AF.Exp), reads=[dmk], writes=[dmk])
                                P.op("dve", lambda: nc.vector.tensor_tensor(out=mt[:], in0=dm[:], in1=psb[2][:, g * 128:(g + 1) * 128].unsqueeze(1).to_broadcast([128, 4, 128]), op=ALU.mult),
                                     reads=[dmk, "ps2"], writes=[mtk])
                                for hh in range(4):
                                    hd_ = 4 * j + hh
                                    P.op("pe", lambda: nc.tensor.matmul(psb[3][:, (hd_ % 8) * 64:(hd_ % 8 + 1) * 64], lhsT=mt[:, hh, :], rhs=xdt[:, hd_ * 64:(hd_ + 1) * 64], start=True, stop=True),
                                         reads=[mtk, "xdt"], writes=["ps3"])
                            P.op("pe", lambda: nc.tensor.matmul(psb[4][:, :], lhsT=CTb[b2_][:, g, :], rhs=Hb[:, g * 512:(g + 1) * 512], start=True, stop=True),
                                 reads=[ck, "Hb"], writes=["ps4"])
                            P.op("pe", lambda: nc.tensor.matmul(psb[5][:, :], lhsT=Btm[:, g, :], rhs=xdte[:, g * 512:(g + 1) * 512], start=True, stop=True),
                                 reads=["Btm", "xdte"], writes=["ps5"])
                            ysl = ysb[:, g * 512:(g + 1) * 512].rearrange("p (h d) -> p h d", h=8)
                            P.op("dve", lambda: nc.vector.tensor_tensor(out=ysl, in0=psb[4][:, :].rearrange("p (h d) -> p h d", h=8), in1=bc3(sm[:, EACS, 8 * g:8 * g + 8], 8, 64), op=ALU.mult),
                                 reads=["ps4", "sm10"], writes=["ysb"])
                            P.op("dve", lambda: nc.vector.tensor_tensor(out=ysb[:, g * 512:(g + 1) * 512], in0=ysb[:, g * 512:(g + 1) * 512], in1=psb[3][:, :], op=ALU.add),
                                 reads=["ysb", "ps3"], writes=["ysb"])
                            hsl = Hf[:, g * 512:(g + 1) * 512]
                            P.op("dve", lambda: nc.vector.tensor_tensor(out=hsl.rearrange("p (h d) -> p h d", h=8), in0=hsl.rearrange("p (h d) -> p h d", h=8),
                                                                        in1=bc3(sm[:, CD, 8 * g:8 * g + 8], 8, 64), op=ALU.mult), reads=["Hf", "sm11"], writes=["Hf"])
                            P.op("dve", lambda: nc.vector.tensor_tensor(out=hsl, in0=hsl, in1=psb[5][:, :], op=ALU.add), reads=["Hf", "ps5"], writes=["Hf"])
                        P.op("act", lambda: nc.scalar.copy(out=Hb[:], in_=Hf[:]), reads=["Hf"], writes=["Hb"])
                        P.op("dve", lambda: nc.vector.tensor_tensor(out=tmp[:].rearrange("p (h d) -> p h d", h=SSM_H), in0=xs3, in1=bc3(dsk[:], SSM_H, 64), op=ALU.mult),
                             reads=["xs", "dsk"], writes=["tmp4"])
                        P.op("dve", lambda: nc.vector.tensor_tensor(out=ysb[:], in0=ysb[:], in1=tmp[:], op=ALU.add), reads=["ysb", "tmp4"], writes=["ysb"])
                        P.op("act", lambda: nc.scalar.activation(out=tmp[:], in_=zt[b2_][:], func=AF.Silu), reads=[zk], writes=["tmp4"])
                        P.op("dve", lambda: nc.vector.tensor_tensor(out=ysb[:], in0=ysb[:], in1=tmp[:], op=ALU.mult), reads=["ysb", "tmp4"], writes=["ysb"])
                        for g in range(4):
                            P.op("act", lambda: nc.scalar.activation(out=tmp[:, g * 512:(g + 1) * 512], in_=ysb[:, g * 512:(g + 1) * 512], func=AF.Square,
                                                                     scale=float(512 ** -0.5), accum_out=nr4[:, g:g + 1]), reads=["ysb"], writes=["tmp4", "nr4"])
                        P.op("dve", lambda: nc.vector.tensor_scalar_add(out=nr4[:, 4:8], in0=nr4[:, 0:4], scalar1=EPS), reads=["nr4"], writes=["nr4"])
                        P.op("act", lambda: nc.scalar.sqrt(out=nr4[:, 4:8], in_=nr4[:, 4:8]), reads=["nr4"], writes=["nr4"])
                        P.op("dve", lambda: nc.vector.reciprocal(out=nr4[:, 4:8], in_=nr4[:, 4:8]), reads=["nr4"], writes=["nr4"])
                        P.op("dve", lambda: nc.vector.tensor_tensor(out=ysb[:].rearrange("p (g d) -> p g d", g=4), in0=ysb[:].rearrange("p (g d) -> p g d", g=4),
                                                                    in1=bc3(nr4[:, 4:8], 4, 512), op=ALU.mult), reads=["ysb", "nr4"], writes=["ysb"])
                        P.op("dve", lambda: nc.vector.tensor_tensor(out=ssb[:], in0=ysb[:], in1=sng[:], op=ALU.mult), reads=["ysb", "sng"], writes=["ssb"])
                        so, sok = sT[(cc // 4) % 2], "sT%d" % ((cc // 4) % 2)
                        for m in range(16):
                            P.op("pe", lambda: nc.tensor.matmul(psb[3 + m // 4][:, (m % 4) * 128:(m % 4 + 1) * 128], lhsT=ssb[:, m * 128:(m + 1) * 128], rhs=ident_b[:], start=True, stop=True),
                                 reads=["ssb", "ident_b"], writes=["ps%d" % (3 + m // 4)])
                        for i in range(4):
                            P.op("act", lambda: nc.scalar.copy(out=so[:, 4 * i:4 * i + 4, (cc % 4) * 128:(cc % 4 + 1) * 128], in_=psb[3 + i][:, :].rearrange("p (m t) -> p m t", m=4)),
                                 reads=["ps%d" % (3 + i)], writes=[sok])
                        if cc % 4 == 3:
                            tb = (cc // 4) * 512
                            P.op("sp", lambda: nc.sync.dma_start(out=ssmT_d[:, tb:tb + 512].rearrange("(m p) t -> p m t", p=128), in_=so[:]), reads=[sok], writes=["ssmT_d"], dma=sok)
                P.barrier()

        if "s5" in stages:
            with contextlib.ExitStack() as st:
                sb = lambda n, s, d=F32: st.enter_context(nc.sbuf_tensor(n, list(s), d))
                Wa = sb("Wa", [128, 8, D], BF16); Ws = sb("Ws", [128, 16, D], BF16); Wo = sb("Wo", [128, 8, D], BF16)
                rw = sb("rw", [128, 8, NE]); rbb = sb("rbb", [128, NE])
                gbb = sb("gbb", [128, 2 * D]); g2bc = sb("g2bc", [128, D])
                g1b = [sb("g1b%d" % i, [128, D]) for i in range(2)]; A2 = [sb("A2%d" % i, [128, D]) for i in range(2)]; sh2 = [sb("sh2%d" % i, [128, D]) for i in range(2)]
                aTt = [sb("aTt%d" % i, [128, 8, 128], BF16) for i in range(2)]
                sTt = [sb("sTt%d" % i, [128, 16, 128], BF16) for i in range(2)]
                glt = [sb("glt%d" % i, [128, 2 * D]) for i in range(2)]
                xt5 = [sb("xt5%d" % i, [128, D]) for i in range(2)]
                mrg = sb("mrg", [128, D]); tm5 = sb("tm5", [128, D]); mrb = [sb("mrb%d" % i, [128, D], BF16) for i in range(2)]
                mT = sb("mT", [128, 8, 128], BF16)
                x1t = [sb("x1t%d" % i, [128, D]) for i in range(2)]
                u2 = sb("u2", [128, D]); u2Tf = sb("u2Tf", [128, 8, 128])
                u2Tb = [sb("u2Tb%d" % i, [128, 8, 512], BF16) for i in range(2)]
                ss5 = sb("ss5", [128, 2]); sq5 = sb("sq5", [128, D])
                lg = sb("lg", [128, NE]); m8 = sb("m8", [128, 8]); ex = sb("ex", [128, NE]); msk = sb("msk", [128, NE])
                sm5 = sb("sm5_", [128, 4]); gwt = [sb("gwt%d" % i, [128, NE]) for i in range(2)]
                gwT = [sb("gwT%d" % i, [NE, 128]) for i in range(2)]
                for (wsb, wnm, wdr, nk) in [(Wa, "Wa", w_attn, 8), (Ws, "Ws", w_ssm, 16), (Wo, "Wo", w_out, 8)]:
                    for k in range(0, nk, 8):
                        for n in range(2):
                            P.op("pool", lambda: nc.gpsimd.dma_start(out=wsb[:, k:k + 8, n * 512:(n + 1) * 512],
                                                                     in_=wdr[k * 128:(k + 8) * 128, n * 512:(n + 1) * 512].rearrange("(k p) n -> p k n", p=128)),
                                 reads=["wchain"], writes=[wnm, "wchain"], dma="%s_%d_%d" % (wnm, k, n))
                P.op("sp", lambda: nc.sync.dma_start(out=rw[:], in_=router_w.rearrange("(k p) n -> p k n", p=128)), writes=["rw"], dma="rw")
                P.op("sp", lambda: nc.sync.dma_start(out=rbb[:], in_=bcast_rows(router_b, NE)), writes=["rbb"], dma="rbb")
                P.op("sp", lambda: nc.sync.dma_start(out=gbb[:], in_=bcast_rows(gate_bias, 2 * D)), writes=["gbb"], dma="gbb")
                P.op("sp", lambda: nc.sync.dma_start(out=g2bc[:], in_=bcast_rows(norm2_g, D)), writes=["g2bc"], dma="g2bc")
                def st_a1(t):
                        s_ = t // 16
                        b2_ = t % 2
                        t0 = t * 128
                        ak, sk, gk_, xk = "aTt%d" % b2_, "sTt%d" % b2_, "glt%d" % b2_, "xt5%d" % b2_
                        x1_, x1k = x1t[b2_], "x1t%d" % b2_
                        gw_, gwk = gwt[b2_], "gwt%d" % b2_
                        ub_, ubk = u2Tb[(t // 4) % 2], "u2Tb%d" % ((t // 4) % 2)
                        if t % 16 == 0:
                            P.op("sp", lambda: nc.sync.dma_start(out=g1b[s_][:], in_=bcast_rows(mod_d[s_, 2 * D:3 * D], D)), reads=["mod_d"], writes=["g1b%d" % s_], dma="g1b%d" % s_)
                            P.op("sp", lambda: nc.sync.dma_start(out=sh2[s_][:], in_=bcast_rows(mod_d[s_, 3 * D:4 * D], D)), reads=["mod_d"], writes=["sh2%d" % s_], dma="sh2%d" % s_)
                            P.op("sp", lambda: nc.sync.dma_start(out=A2[s_][:], in_=bcast_rows(mod_d[s_, 4 * D:5 * D], D)), reads=["mod_d"], writes=["A2%d" % s_], dma="A2%d" % s_)
                            P.op("dve", lambda: nc.vector.scalar_tensor_tensor(out=A2[s_][:], in0=A2[s_][:], scalar=1.0, in1=g2bc[:], op0=ALU.add, op1=ALU.mult),
                                 reads=["A2%d" % s_, "g2bc"], writes=["A2%d" % s_])
                        ak, sk, gk_, xk = "aTt%d" % b2_, "sTt%d" % b2_, "glt%d" % b2_, "xt5%d" % b2_
                        P.op("sp", lambda: nc.sync.dma_start(out=aTt[b2_][:], in_=attnT_d[:, t0:t0 + 128].rearrange("(k p) t -> p k t", p=128)), reads=["attnT_d"], writes=[ak], dma=ak)
                        P.op("sp", lambda: nc.sync.dma_start(out=sTt[b2_][:], in_=ssmT_d[:, t0:t0 + 128].rearrange("(k p) t -> p k t", p=128)), reads=["ssmT_d"], writes=[sk], dma=sk)
                        P.op("sp", lambda: nc.sync.dma_start(out=glt[b2_][:], in_=gl_d[t0:t0 + 128, :]), reads=["gl_d"], writes=[gk_], dma=gk_)
                        P.op("sp", lambda: nc.sync.dma_start(out=xt5[b2_][:], in_=x_d[t0:t0 + 128, :]), writes=[xk], dma=xk)
                        P.op("dve", lambda: nc.vector.tensor_tensor(out=glt[b2_][:], in0=glt[b2_][:], in1=gbb[:], op=ALU.add), reads=[gk_, "gbb"], writes=[gk_])
                        P.op("act", lambda: nc.scalar.activation(out=glt[b2_][:], in_=glt[b2_][:], func=AF.Sigmoid), reads=[gk_], writes=[gk_])
                        for n in range(2):
                            for k in range(8):
                                P.op("pe", lambda: nc.tensor.matmul(psb[n][:, :], lhsT=aTt[b2_][:, k, :], rhs=Wa[:, k, n * 512:(n + 1) * 512], start=(k == 0), stop=(k == 7)),
                                     reads=[ak, "Wa"], writes=["ps%d" % n])
                            P.op("dve", lambda: nc.vector.tensor_tensor(out=mrg[:, n * 512:(n + 1) * 512], in0=psb[n][:, :], in1=glt[b2_][:, n * 512:(n + 1) * 512], op=ALU.mult),
                                 reads=["ps%d" % n, gk_], writes=["mrg"])
                            for k in range(16):
                                P.op("pe", lambda: nc.tensor.matmul(psb[2 + n][:, :], lhsT=sTt[b2_][:, k, :], rhs=Ws[:, k, n * 512:(n + 1) * 512], start=(k == 0), stop=(k == 15)),
                                     reads=[sk, "Ws"], writes=["ps%d" % (2 + n)])
                            P.op("dve", lambda: nc.vector.tensor_tensor(out=tm5[:, n * 512:(n + 1) * 512], in0=psb[2 + n][:, :], in1=glt[b2_][:, D + n * 512:D + (n + 1) * 512], op=ALU.mult),
                                 reads=["ps%d" % (2 + n), gk_], writes=["tm5"])
                        P.op("dve", lambda: nc.vector.tensor_tensor(out=mrb[b2_][:], in0=mrg[:], in1=tm5[:], op=ALU.add), reads=["mrg", "tm5"], writes=["mrb%d" % b2_])

                def st_a2(t):
                        s_ = t // 16
                        b2_ = t % 2
                        t0 = t * 128
                        ak, sk, gk_, xk = "aTt%d" % b2_, "sTt%d" % b2_, "glt%d" % b2_, "xt5%d" % b2_
                        x1_, x1k = x1t[b2_], "x1t%d" % b2_
                        gw_, gwk = gwt[b2_], "gwt%d" % b2_
                        ub_, ubk = u2Tb[(t // 4) % 2], "u2Tb%d" % ((t // 4) % 2)
                        for half in range(2):
                            for kk in range(4):
                                k = half * 4 + kk
                                P.op("pe", lambda: nc.tensor.matmul(psb[4][:, kk * 128:(kk + 1) * 128], lhsT=mrb[b2_][:, k * 128:(k + 1) * 128], rhs=ident_b[:], start=True, stop=True),
                                     reads=["mrb%d" % b2_, "ident_b"], writes=["ps4"])
                            P.op("act", lambda: nc.scalar.copy(out=mT[:, half * 4:half * 4 + 4, :], in_=psb[4][:, :].rearrange("p (k t) -> p k t", k=4)),
                                 reads=["ps4"], writes=["mT"])
                        x1_, x1k = x1t[b2_], "x1t%d" % b2_
                        for n in range(2):
                            for k in range(8):
                                P.op("pe", lambda: nc.tensor.matmul(psb[6 + n][:, :], lhsT=mT[:, k, :], rhs=Wo[:, k, n * 512:(n + 1) * 512], start=(k == 0), stop=(k == 7)),
                                     reads=["mT", "Wo"], writes=["ps%d" % (6 + n)])
                            P.op("dve", lambda: nc.vector.tensor_tensor(out=x1_[:, n * 512:(n + 1) * 512], in0=psb[6 + n][:, :], in1=g1b[s_][:, n * 512:(n + 1) * 512], op=ALU.mult),
                                 reads=["ps%d" % (6 + n), "g1b%d" % s_], writes=[x1k])
                        P.op("dve", lambda: nc.vector.tensor_tensor(out=x1_[:], in0=x1_[:], in1=xt5[b2_][:], op=ALU.add), reads=[x1k, xk], writes=[x1k])
                        P.op("sp", lambda: nc.sync.dma_start(out=x1_d[t0:t0 + 128, :], in_=x1_[:]), reads=[x1k], writes=["x1_d"], dma=x1k)

                def st_b(t):
                        s_ = t // 16
                        b2_ = t % 2
                        t0 = t * 128
                        ak, sk, gk_, xk = "aTt%d" % b2_, "sTt%d" % b2_, "glt%d" % b2_, "xt5%d" % b2_
                        x1_, x1k = x1t[b2_], "x1t%d" % b2_
                        gw_, gwk = gwt[b2_], "gwt%d" % b2_
                        ub_, ubk = u2Tb[(t // 4) % 2], "u2Tb%d" % ((t // 4) % 2)
                        P.op("act", lambda: nc.scalar.activation(out=sq5[:], in_=x1_[:], func=AF.Square, scale=1.0 / 32.0, accum_out=ss5[:, 0:1]), reads=[x1k], writes=["sq5", "ss5"])
                        P.op("dve", lambda: nc.vector.tensor_scalar_add(out=ss5[:, 1:2], in0=ss5[:, 0:1], scalar1=EPS), reads=["ss5"], writes=["ss5"])
                        P.op("act", lambda: nc.scalar.sqrt(out=ss5[:, 1:2], in_=ss5[:, 1:2]), reads=["ss5"], writes=["ss5"])
                        P.op("dve", lambda: nc.vector.reciprocal(out=ss5[:, 1:2], in_=ss5[:, 1:2]), reads=["ss5"], writes=["ss5"])
                        P.op("dve", lambda: nc.vector.scalar_tensor_tensor(out=u2[:], in0=x1_[:], scalar=ss5[:, 1:2], in1=A2[s_][:], op0=ALU.mult, op1=ALU.mult),
                             reads=[x1k, "ss5", "A2%d" % s_], writes=["u2"])
                        P.op("dve", lambda: nc.vector.tensor_tensor(out=u2[:], in0=u2[:], in1=sh2[s_][:], op=ALU.add), reads=["u2", "sh2%d" % s_], writes=["u2"])
                        ub_, ubk = u2Tb[(t // 4) % 2], "u2Tb%d" % ((t // 4) % 2)
                        for half in range(2):
                            for kk in range(4):
                                k = half * 4 + kk
                                P.op("pe", lambda: nc.tensor.matmul(psb[5][:, kk * 128:(kk + 1) * 128], lhsT=u2[:, k * 128:(k + 1) * 128], rhs=ident_f[:], start=True, stop=True),
                                     reads=["u2", "ident_f"], writes=["ps5"])
                            P.op("act", lambda: nc.scalar.copy(out=u2Tf[:, half * 4:half * 4 + 4, :], in_=psb[5][:, :].rearrange("p (k t) -> p k t", k=4)),
                                 reads=["ps5"], writes=["u2Tf"])
                            P.op("dve", lambda: nc.vector.tensor_copy(out=ub_[:, half * 4:half * 4 + 4, (t % 4) * 128:(t % 4 + 1) * 128], in_=psb[5][:, :].rearrange("p (k t) -> p k t", k=4)),
                                 reads=["ps5"], writes=[ubk])
                        if t % 4 == 3:
                            tb = (t // 4) * 512
                            P.op("sp", lambda: nc.sync.dma_start(out=u2T_d[:, tb:tb + 512].rearrange("(k p) t -> p k t", p=128), in_=ub_[:]), reads=[ubk], writes=["u2T_d"], dma=ubk)
                        for k in range(8):
                            P.op("pe", lambda: nc.tensor.matmul(psb[5][:, 0:NE], lhsT=u2Tf[:, k, :], rhs=rw[:, k, :], start=(k == 0), stop=(k == 7)), reads=["u2Tf", "rw"], writes=["ps5"])
                        P.op("dve", lambda: nc.vector.tensor_tensor(out=lg[:], in0=psb[5][:, 0:NE], in1=rbb[:], op=ALU.add), reads=["ps5", "rbb"], writes=["lg"])
                        P.op("dve", lambda: nc.vector.max(out=m8[:], in_=lg[:]), reads=["lg"], writes=["m8"])
                        P.op("dve", lambda: nc.vector.tensor_scalar(out=msk[:], in0=lg[:], scalar1=m8[:, 3:4], scalar2=None, op0=ALU.is_ge), reads=["lg", "m8"], writes=["msk"])
                        P.op("dve", lambda: nc.vector.tensor_scalar_mul(out=sm5[:, 0:1], in0=m8[:, 0:1], scalar1=-1.0), reads=["m8"], writes=["sm5_"])
                        P.op("act", lambda: nc.scalar.activation(out=ex[:], in_=lg[:], func=AF.Exp, bias=sm5[:, 0:1], scale=1.0), reads=["lg", "sm5_"], writes=["ex"])
                        P.op("dve", lambda: nc.vector.tensor_tensor(out=ex[:], in0=ex[:], in1=msk[:], op=ALU.mult), reads=["ex", "msk"], writes=["ex"])
                        P.op("dve", lambda: nc.vector.reduce_sum(out=sm5[:, 1:2], in_=ex[:], axis=AX.X), reads=["ex"], writes=["sm5_"])
                        P.op("dve", lambda: nc.vector.reciprocal(out=sm5[:, 2:3], in_=sm5[:, 1:2]), reads=["sm5_"], writes=["sm5_"])
                        gw_, gwk = gwt[b2_], "gwt%d" % b2_
                        P.op("dve", lambda: nc.vector.tensor_scalar(out=gw_[:], in0=ex[:], scalar1=sm5[:, 2:3], scalar2=None, op0=ALU.mult), reads=["ex", "sm5_"], writes=[gwk])
                        P.op("sp", lambda: nc.sync.dma_start(out=gwtm_d[t0:t0 + 128, :], in_=gw_[:]), reads=[gwk], writes=["gwtm_d"], dma=gwk)
                        P.op("pe", lambda: nc.tensor.matmul(psb[5][0:NE, 128:256], lhsT=gw_[:], rhs=ident_f[:], start=True, stop=True), reads=[gwk, "ident_f"], writes=["ps5"])
                        P.op("act", lambda: nc.scalar.copy(out=gwT[b2_][:], in_=psb[5][0:NE, 128:256]), reads=["ps5"], writes=["gwT%d" % b2_])
                        P.op("sp", lambda: nc.sync.dma_start(out=gw_d[:, t0:t0 + 128], in_=gwT[b2_][:]), reads=["gwT%d" % b2_], writes=["gw_d"], dma="gwT%d" % b2_)

                for i in range(NT + 2):
                    if i < NT:
                        st_a1(i)
                    if 0 <= i - 1 < NT:
                        st_a2(i - 1)
                    if 0 <= i - 2 < NT:
                        st_b(i - 2)
                P.barrier()

        if "s6" in stages:
            PT = 1024
            with contextlib.ExitStack() as st:
                sb = lambda n, s, d=F32: st.enter_context(nc.sbuf_tensor(n, list(s), d))
                u2T = sb("u2T6", [128, 8, PT], BF16)
                gwT6 = sb("gwT6", [NE, PT])
                b1s = sb("b1s", [128, NE, 16]); b2s = sb("b2s", [NE, D]); g2b = sb("g2b", [128, D])
                yacc = sb("yacc", [128, PT // 128, D])
                w1a = [sb("w1a%d" % i, [128, 8, 512], BF16) for i in range(3)]
                w1l = [sb("w1l%d" % i, [128, 8, 512], BF16) for i in range(3)]
                w2b = [sb("w2b%d" % i, [128, 8, 512], BF16) for i in range(4)]
                gwtm = sb("gwtm", [128, PT // 128, NE])
                ebuf = {nm: [sb("%s%d" % (nm, i), [128, 512]) for i in range(2)] for nm in ("gg", "ll", "t1", "t2")}
                actT = [[sb("actT%d%d" % (a, b), [128, 8, 512], BF16) for b in range(2)] for a in range(2)]
                x16 = [sb("x16%d" % i, [128, D]) for i in range(2)]
                P.op("sp", lambda: nc.sync.dma_start(out=b1s[:], in_=b1T[:, :, :]), writes=["b1s"], dma="b1s")
                P.op("sp", lambda: nc.sync.dma_start(out=b2s[:], in_=b2[:, :]), writes=["b2s"], dma="b2s")
                b17 = sb("b17", [128, NE, 8])
                P.op("dve", lambda: nc.vector.tensor_scalar_add(out=b17[:], in0=b1s[:, :, 8:16], scalar1=7.0), reads=["b1s"], writes=["b17"])
                NPASS = T // PT
                chunks = [(ps_, e, hh) for ps_ in range(NPASS) for e in range(NE) for hh in range(2)]
                state = {"w1next": 0, "ecnt": 0}

                def emit_w1(upto):
                    while state["w1next"] <= min(upto, len(chunks) - 1):
                        j = state["w1next"]
                        _, e, hh = chunks[j]
                        for (wl_, nm, c0) in [(w1a, "w1a", hh * 512), (w1l, "w1l", FF + hh * 512)]:
                            wk = "%s%d" % (nm, j % 3)
                            P.op("pool", lambda: nc.gpsimd.dma_start(out=wl_[j % 3][:], in_=w1[e, :, c0:c0 + 512].rearrange("(k p) n -> p k n", p=128)),
                                 writes=[wk], dma=wk)
                        state["w1next"] += 1

                def h_phase(ps_, e, jbase):
                    par = e % 2
                    for hh in range(2):
                        j = jbase + hh
                        emit_w1(j + 2)
                        wa_, wak = w1a[j % 3], "w1a%d" % (j % 3)
                        wl2_, wlk = w1l[j % 3], "w1l%d" % (j % 3)
                        for ii in range(4):
                            i = hh * 4 + ii
                            for grp in range(2):
                                n_ = state["ecnt"]; state["ecnt"] += 1
                                pa, pb = (n_ % 2) * 2, (n_ % 2) * 2 + 1
                                for k in range(8):
                                    P.op("pe", lambda: nc.tensor.matmul(psb[pa][:, :], lhsT=wa_[:, k, ii * 128:(ii + 1) * 128], rhs=u2T[:, k, grp * 512:(grp + 1) * 512], start=(k == 0), stop=(k == 7)),
                                         reads=[wak, "u2T6"], writes=["ps%d" % pa])
                                for k in range(8):
                                    P.op("pe", lambda: nc.tensor.matmul(psb[pb][:, :], lhsT=wl2_[:, k, ii * 128:(ii + 1) * 128], rhs=u2T[:, k, grp * 512:(grp + 1) * 512], start=(k == 0), stop=(k == 7)),
                                         reads=[wlk, "u2T6"], writes=["ps%d" % pb])
                                B = {nm: (ebuf[nm][n_ % 2], "%s%d" % (nm, n_ % 2)) for nm in ebuf}
                                P.op("dve", lambda: nc.vector.tensor_scalar(out=B["gg"][0][:], in0=psb[pa][:, :], scalar1=b1s[:, e, i:i + 1], scalar2=7.0, op0=ALU.add, op1=ALU.min),
                                     reads=["ps%d" % pa, "b1s"], writes=[B["gg"][1]])
                                P.op("act", lambda: nc.scalar.activation(out=B["t1"][0][:], in_=B["gg"][0][:], func=AF.Gelu_apprx_sigmoid), reads=[B["gg"][1]], writes=[B["t1"][1]])
                                P.op("act", lambda: nc.scalar.activation(out=B["ll"][0][:], in_=psb[pb][:, :], func=AF.Relu, bias=b17[:, e, i:i + 1], scale=1.0),
                                     reads=["ps%d" % pb, "b17"], writes=[B["ll"][1]])
                                P.op("dve", lambda: nc.vector.tensor_scalar(out=B["t2"][0][:], in0=B["ll"][0][:], scalar1=14.0, scalar2=-6.0, op0=ALU.min, op1=ALU.add),
                                     reads=[B["ll"][1]], writes=[B["t2"][1]])
                                P.op("dve", lambda: nc.vector.tensor_tensor(out=actT[par][grp][:, i, :], in0=B["t1"][0][:], in1=B["t2"][0][:], op=ALU.mult),
                                     reads=[B["t1"][1], B["t2"][1]], writes=["actT%d%d" % (par, grp)])

                def w_loads(e):
                    for n in range(2):
                        wi = (e % 2) * 2 + n
                        P.op("pool", lambda: nc.gpsimd.dma_start(out=w2b[wi][:], in_=w2[e, :, n * 512:(n + 1) * 512].rearrange("(k p) n -> p k n", p=128)),
                             writes=["w2b%d" % wi], dma="w2b%d" % wi)

                def w_phase(e):
                    par = e % 2
                    for n in range(2):
                        for tl in range(PT // 128):
                            grp, off = tl // 4, (tl % 4) * 128
                            pk = 5 + (tl % 2)
                            wi = (e % 2) * 2 + n
                            for k in range(8):
                                P.op("pe", lambda: nc.tensor.matmul(psb[pk][:, :], lhsT=actT[par][grp][:, k, off:off + 128], rhs=w2b[wi][:, k, :], start=(k == 0), stop=(k == 7)),
                                     reads=["actT%d%d" % (par, grp), "w2b%d" % wi], writes=["ps%d" % pk])
                            P.op("dve", lambda: nc.vector.scalar_tensor_tensor(out=yacc[:, tl, n * 512:(n + 1) * 512], in0=psb[pk][:, :], scalar=gwtm[:, tl, e:e + 1],
                                                                               in1=yacc[:, tl, n * 512:(n + 1) * 512], op0=ALU.mult, op1=ALU.add),
                                 reads=["yacc", "ps%d" % pk, "gwtm"], writes=["yacc"])

                for ps_ in range(NPASS):
                    tb = ps_ * PT
                    s_ = tb // SEQ
                    P.op("sp", lambda: nc.sync.dma_start(out=u2T[:], in_=u2T_d[:, tb:tb + PT].rearrange("(k p) t -> p k t", p=128)), reads=["u2T_d"], writes=["u2T6"], dma="u2T6")
                    P.op("sp", lambda: nc.sync.dma_start(out=gwT6[:], in_=gw_d[:, tb:tb + PT]), reads=["gw_d"], writes=["gwT6"], dma="gwT6")
                    P.op("sp", lambda: nc.sync.dma_start(out=gwtm[:], in_=gwtm_d[tb:tb + PT, :].rearrange("(t p) e -> p t e", p=128)), reads=["gwtm_d"], writes=["gwtm"], dma="gwtm")
                    if tb % SEQ == 0:
                        P.op("sp", lambda: nc.sync.dma_start(out=g2b[:], in_=bcast_rows(mod_d[s_, 5 * D:6 * D], D)), reads=["mod_d"], writes=["g2b"], dma="g2b")
                    for tl in range(PT // 128):
                        for n in range(2):
                            pk = 5 + (n % 2)
                            P.op("pe", lambda: nc.tensor.matmul(psb[pk][:, :], lhsT=gwT6[:, tl * 128:(tl + 1) * 128], rhs=b2s[:, n * 512:(n + 1) * 512], start=True, stop=True),
                                 reads=["gwT6", "b2s"], writes=["ps%d" % pk])
                            P.op("act", lambda: nc.scalar.copy(out=yacc[:, tl, n * 512:(n + 1) * 512], in_=psb[pk][:, :]), reads=["ps%d" % pk], writes=["yacc"])
                    jb = ps_ * NE * 2
                    h_phase(ps_, 0, jb)
                    w_loads(0)
                    for e in range(NE):
                        if e + 1 < NE:
                            w_loads(e + 1)
                        if e + 1 < NE:
                            h_phase(ps_, e + 1, jb + (e + 1) * 2)
                        w_phase(e)
                    for tl in range(PT // 128):
                        r0 = tb + tl * 128
                        xb_, xk = x16[tl % 2], "x16%d" % (tl % 2)
                        P.op("sp", lambda: nc.sync.dma_start(out=xb_[:], in_=x1_d[r0:r0 + 128, :]), reads=["x1_d"], writes=[xk], dma=xk)
                        P.op("dve", lambda: nc.vector.tensor_tensor(out=yacc[:, tl, :], in0=yacc[:, tl, :], in1=g2b[:], op=ALU.mult), reads=["yacc", "g2b"], writes=["yacc"])
                        P.op("dve", lambda: nc.vector.tensor_tensor(out=xb_[:], in0=yacc[:, tl, :], in1=xb_[:], op=ALU.add), reads=["yacc", xk], writes=[xk])
                        P.op("sp", lambda: nc.sync.dma_start(out=out_d[r0:r0 + 128, :], in_=xb_[:]), reads=[xk], writes=["out"], dma=xk)
                P.barrier()

        if "copyout" in stages:
            with nc.sbuf_tensor("cpy", [128, D], F32) as cpy:
                P.op("sp", lambda: nc.sync.dma_start(out=cpy[:], in_=x_d[0:128, :]), writes=["cpy"], dma="cpy")
                P.op("sp", lambda: nc.sync.dma_start(out=out_d[0:128, :], in_=cpy[:]), reads=["cpy"], writes=["out"], dma="cpy")
                P.drain("sp", outs_to_drain)
        for name in dbg:
            if name in ("qkv", "z", "dt", "gl", "xbcT"):
                pass
        P.drain("sp", outs_to_drain)
    return nc, P, dbg_outs


def make_consts():
    ident = np.eye(128, dtype=np.float32)
    rev = ident[::-1].copy()
    tri = np.triu(np.ones((128, 128), np.float32))
    negtri = np.where(np.arange(128)[None, :] >= np.arange(128)[:, None], 0.0, NEG).astype(np.float32)
    i = np.arange(2304)
    d = i - 127
    dd = np.maximum(d, 1).astype(np.float32)
    large = 16 + (np.log(dd / 16.0) / math.log(1024 / 16.0) * 16).astype(np.int32)
    large = np.minimum(large, 31)
    bucket = np.where(d < 16, np.maximum(d, 0), large)
    oh = np.zeros((33, 2304), np.float32)
    oh[bucket, i] = 1.0
    oh[:, d < 0] = 0.0
    oh[32, d < 0] = 1.0
    sel = np.zeros((128, 3, 16, 8), np.float32)
    for t in range(16):
        qb = t // 2
        for n in range(8):
            if n >= qb:
                sel[:, 0, t, n] = -1e30
            else:
                sel[:, 1, t, n] = 1.0
            if n == qb:
                sel[:, 2, t, n] = 1.0
    kind = (np.arange(SEQ)[None, :] // 256 == np.arange(8)[:, None]).astype(np.float32)
    return dict(c_ident=ident, c_rev=rev, c_tri=tri, c_negtri=negtri, c_bucket=oh, c_selmask=sel, c_kind=kind)


def make_in_maps(inputs):
    f = lambda a: np.ascontiguousarray(np.asarray(a, dtype=np.float32))
    consts = make_consts()
    x = f(inputs["x"]).reshape(NCORES, T, D)
    c = f(inputs["c"]).reshape(NCORES, NSEQ, D)
    rel = np.concatenate([f(inputs["rel_bias_table"]), np.full((1, H), NEG, np.float32)], 0)
    b1 = f(inputs["expert_b1"])[0]
    shared = dict(
        ada_w=f(inputs["ada_w"])[0], ada_b=f(inputs["ada_b"])[0], norm1_g=f(inputs["norm1_g"])[0], w_in=f(inputs["w_in"])[0],
        q_norm_g=f(inputs["q_norm_g"])[0], k_norm_g=f(inputs["k_norm_g"])[0], rel_tab=rel,
        conv_wT=f(f(inputs["conv_w"])[0].T), conv_b=f(f(inputs["conv_b"])[0].reshape(24, 128).T), dt_bias=f(inputs["dt_bias"])[0],
        a_log=f(inputs["a_log"])[0], d_skip=f(inputs["d_skip"])[0], ssm_norm_g=f(inputs["ssm_norm_g"])[0],
        w_attn=f(inputs["w_attn_branch"])[0], w_ssm=f(inputs["w_ssm_branch"])[0], gate_bias=f(inputs["gate_bias"])[0],
        w_out=f(inputs["w_out"])[0], norm2_g=f(inputs["norm2_g"])[0], router_w=f(inputs["router_w"])[0],
        router_b=f(inputs["router_b"])[0], w1=f(inputs["expert_w1"])[0],
        b1T=f(b1.reshape(NE, 16, 128).transpose(2, 0, 1)), w2=f(inputs["expert_w2"])[0], b2=f(inputs["expert_b2"])[0],
    )
    shared.update(consts)
    maps = []
    for i in range(NCORES):
        m = dict(shared)
        m["x"] = x[i]
        m["cT"] = f(c[i].T)
        maps.append(m)
    return maps


def kernel(**inputs):
    nc, P, _ = build_program()
    in_maps = make_in_maps(inputs)
    res = run_bass_kernel_spmd(nc, in_maps, core_ids=list(range(NCORES)))
    out = np.stack([np.asarray(r["out"], dtype=np.float32) for r in res.results], 0)
    return out.reshape(16, SEQ, D)
```

```python
import contextlib
import os
import math
import numpy as np
import concourse.bass as bass
import concourse.mybir as mybir
from concourse.bass_utils import run_bass_kernel_spmd

F32 = mybir.dt.float32
BF16 = mybir.dt.bfloat16
AF = mybir.ActivationFunctionType
ALU = mybir.AluOpType
AX = mybir.AxisListType

NCORES = 8
D = 1024
SEQ = 2048
NSEQ = 2
T = NSEQ * SEQ
NT = T // 128
H = 16
HD = 64
SSM_INNER = 2048
SSM_H = 32
SSM_G = 4
SSM_N = 128
CONV_DIM = 3072
NE = 32
FF = 1024
EPS = 1e-6
IN_PROJ = 10272
OFF_Q, OFF_K, OFF_V, OFF_Z, OFF_XBC, OFF_DT, OFF_GATE = 0, 1024, 2048, 3072, 5120, 8192, 8224
NEG = -30000.0


class Prog:
    def __init__(self, nc, es):
        self.nc, self.es = nc, es
        self.eng = {"pe": nc.tensor, "act": nc.scalar, "dve": nc.vector, "pool": nc.gpsimd, "sp": nc.sync}
        self.sems, self.val = {}, {}
        self.seen = {e: {} for e in self.eng}
        self.lastw, self.readers = {}, {}
        self.nins = 0
        self.free = []
        self.free_sw = []
        self.swkeys = set()
        self.nsem = 0

    def sem(self, key, sw=False):
        if key not in self.sems:
            fl = self.free_sw if sw else self.free
            if sw:
                self.swkeys.add(key)
            if fl:
                h, v = fl.pop()
                self.sems[key] = h
                self.val[key] = v
                for e in self.seen:
                    self.seen[e][key] = v
            else:
                self.nsem += 1
                self.sems[key] = self.es.enter_context(self.nc.semaphore("s%d" % self.nsem))
                self.val[key] = 0
        return self.sems[key]

    def sb(self, name, shape, dt=F32):
        return self.es.enter_context(self.nc.sbuf_tensor(name, list(shape), dt))

    def ps(self, name, shape, dt=F32):
        return self.es.enter_context(self.nc.psum_tensor(name, list(shape), dt))

    def op(self, eng, fn, reads=(), writes=(), dma=None):
        deps = {}
        for b in reads:
            for k, v in self.lastw.get(b, {}).items():
                deps[k] = max(deps.get(k, 0), v)
            if b.startswith("ps"):
                for k, v in self.readers.get(b, {}).items():
                    if k != eng:
                        deps[k] = max(deps.get(k, 0), v)
        for b in writes:
            for k, v in self.lastw.get(b, {}).items():
                deps[k] = max(deps.get(k, 0), v)
            for k, v in self.readers.get(b, {}).items():
                deps[k] = max(deps.get(k, 0), v)
        e = self.eng[eng]
        for k, v in deps.items():
            if dma is None and k == eng and eng == "pe":
                continue
            if self.seen[eng].get(k, 0) >= v:
                continue
            e.wait_ge(self.sems[k], v)
            self.seen[eng][k] = v
        ins = fn()
        if dma is None:
            key, inc = eng, 1
        else:
            key, inc = "dma:" + dma, 16
        s = self.sem(key, sw=(dma is not None and eng == "pool"))
        self.val[key] += inc
        ins.then_inc(s, inc)
        v = self.val[key]
        for b in writes:
            if dma is None:
                self.lastw[b] = {key: v}
            else:
                d_ = {k_: v_ for k_, v_ in self.lastw.get(b, {}).items() if k_.startswith("dma:")}
                d_[key] = v
                self.lastw[b] = d_
            self.readers[b] = {}
        for b in reads:
            self.readers.setdefault(b, {})[key] = v
        self.nins += 1
        return ins

    def barrier(self):
        for eng, e in self.eng.items():
            for k, v in self.val.items():
                if v > 0 and self.seen[eng].get(k, 0) < v:
                    e.wait_ge(self.sems[k], v)
                    self.seen[eng][k] = v
        for k in [k for k in self.sems if k.startswith("dma:")]:
            (self.free_sw if k in self.swkeys else self.free).append((self.sems.pop(k), self.val.pop(k)))
            self.swkeys.discard(k)
            for eng in self.seen:
                self.seen[eng].pop(k, None)
        self.lastw, self.readers = {}, {}

    def drain(self, eng, bufs):
        e = self.eng[eng]
        for b in bufs:
            for k, v in self.lastw.get(b, {}).items():
                if self.seen[eng].get(k, 0) < v:
                    e.wait_ge(self.sems[k], v)
                    self.seen[eng][k] = v


def bcast_rows(ap1d, n):
    return bass.AP(ap1d.tensor, ap1d.offset, [[0, 128], [1, n]])


def build_program(stages=("s0", "s1", "s3", "s4", "s5", "s6"), dbg=()):
    nc = bass.Bass("TRN2", target_bir_lowering=False)
    es = contextlib.ExitStack()

    def din(name, shape, dt=F32):
        return nc.dram_tensor(name, list(shape), dt, kind="ExternalInput").ap()

    def dscr(name, shape, dt=F32):
        if name in dbg:
            outs_to_drain.append(name)
            return nc.dram_tensor(name, list(shape), dt, kind="ExternalOutput").ap()
        return nc.dram_tensor(name, list(shape), dt, kind="Internal").ap()

    def dout(name, shape, dt=F32):
        return nc.dram_tensor(name, list(shape), dt, kind="ExternalOutput").ap()

    outs_to_drain = ["out"]
    x_d = din("x", [T, D])
    cT_d = din("cT", [D, NSEQ])
    ada_w = din("ada_w", [D, 6 * D])
    ada_b = din("ada_b", [6 * D])
    norm1_g = din("norm1_g", [D])
    w_in = din("w_in", [D, IN_PROJ])
    q_norm_g = din("q_norm_g", [HD])
    k_norm_g = din("k_norm_g", [HD])
    rel_tab = din("rel_tab", [33, H])
    conv_wT = din("conv_wT", [CONV_DIM, 4])
    conv_b = din("conv_b", [128, 24])
    dt_bias = din("dt_bias", [SSM_H])
    a_log = din("a_log", [SSM_H])
    d_skip = din("d_skip", [SSM_H])
    ssm_norm_g = din("ssm_norm_g", [SSM_INNER])
    w_attn = din("w_attn", [D, D])
    w_ssm = din("w_ssm", [SSM_INNER, D])
    gate_bias = din("gate_bias", [2 * D])
    w_out = din("w_out", [D, D])
    norm2_g = din("norm2_g", [D])
    router_w = din("router_w", [D, NE])
    router_b = din("router_b", [NE])
    w1 = din("w1", [NE, D, 2 * FF])
    b1T = din("b1T", [128, NE, 16])
    w2 = din("w2", [NE, FF, D])
    b2 = din("b2", [NE, D])
    c_ident = din("c_ident", [128, 128])
    c_rev = din("c_rev", [128, 128])
    c_tri = din("c_tri", [128, 128])
    c_negtri = din("c_negtri", [128, 128])
    c_bucket = din("c_bucket", [33, 2304])
    c_kind = din("c_kind", [8, SEQ])
    c_selmask = din("c_selmask", [128, 3, 16, 8])
    out_d = dout("out", [T, D])

    P = Prog(nc, es)
    with es:
        mod_d = dscr("mod_d", [NSEQ, 6 * D])
        qkv_d = dscr("qkv_d", [T, 3 * D])
        z_d = dscr("z_d", [T, SSM_INNER])
        dt_d = dscr("dt_d", [T, SSM_H])
        gl_d = dscr("gl_d", [T, 2 * D])
        xbcT_d = dscr("xbcT_d", [CONV_DIM, T])
        expf_d = dscr("expf_d", [H, 2304], BF16)
        wnat_d = dscr("wnat_d", [H, 128, SEQ], BF16)
        attnT_d = dscr("attnT_d", [D, T], BF16)
        ssmT_d = dscr("ssmT_d", [SSM_INNER, T], BF16)
        x1_d = dscr("x1_d", [T, D])
        u2T_d = dscr("u2T_d", [D, T], BF16)
        gw_d = dscr("gw_d", [NE, T])
        gwtm_d = dscr("gwtm_d", [T, NE])

        ident_f = P.sb("ident_f", [128, 128])
        rev_f = P.sb("rev_f", [128, 128])
        ident_b = P.sb("ident_b", [128, 128], BF16)
        rev_b = P.sb("rev_b", [128, 128], BF16)
        ones_b = P.sb("ones_b", [128, 128], BF16)
        ones_f = P.sb("ones_f", [128, 128])
        P.op("sp", lambda: nc.sync.dma_start(out=ident_f[:], in_=c_ident[:, :]), writes=["ident_f"], dma="ident_f")
        P.op("sp", lambda: nc.sync.dma_start(out=rev_f[:], in_=c_rev[:, :]), writes=["rev_f"], dma="rev_f")
        P.op("pool", lambda: nc.gpsimd.dma_start(out=ident_b[:], in_=c_ident[:, :]), writes=["ident_b"], dma="ident_b")
        P.op("pool", lambda: nc.gpsimd.dma_start(out=rev_b[:], in_=c_rev[:, :]), writes=["rev_b"], dma="rev_b")
        P.op("dve", lambda: nc.vector.memset(ones_b[:], 1.0), writes=["ones_b"])
        P.op("dve", lambda: nc.vector.memset(ones_f[:], 1.0), writes=["ones_f"])

        psb = [P.ps("psb%d" % i, [128, 512]) for i in range(8)]

        dbg_outs = {}

        def tap(name, shape, dt=F32):
            dbg_outs[name] = dout("dbg_" + name, shape, dt)
            outs_to_drain.append("dbg_" + name)
            return dbg_outs[name]

        if "s0" in stages:
            with contextlib.ExitStack() as st:
                sb = lambda n, s, d=F32: st.enter_context(nc.sbuf_tensor(n, list(s), d))
                cact = sb("cact", [128, 8, NSEQ])
                adab = sb("adab", [NSEQ, 6 * D])
                modsb = sb("modsb", [NSEQ, 6 * D])
                wb = [sb("s0w%d" % i, [128, 8, 512]) for i in range(2)]
                P.op("sp", lambda: nc.sync.dma_start(out=cact[:], in_=cT_d.rearrange("(k p) b -> p k b", p=128)),
                     writes=["cact"], dma="cact")
                P.op("sp", lambda: nc.sync.dma_start(out=adab[:], in_=bass.AP(ada_b.tensor, 0, [[0, NSEQ], [1, 6 * D]])),
                     writes=["adab"], dma="adab")
                P.op("act", lambda: nc.scalar.activation(out=cact[:], in_=cact[:], func=AF.Silu), reads=["cact"], writes=["cact"])
                for j in range(12):
                    w = wb[j % 2]
                    wk = "s0w%d" % (j % 2)
                    P.op("sp", lambda: nc.sync.dma_start(out=w[:], in_=ada_w[:, j * 512:(j + 1) * 512].rearrange("(k p) n -> p k n", p=128)),
                         writes=[wk], dma=wk)
                    pk = "ps%d" % (j % 2)
                    for k in range(8):
                        P.op("pe", lambda: nc.tensor.matmul(psb[j % 2][0:NSEQ, :], lhsT=cact[:, k, :], rhs=w[:, k, :], start=(k == 0), stop=(k == 7)),
                             reads=["cact", wk], writes=[pk])
                    P.op("dve", lambda: nc.vector.tensor_tensor(out=modsb[:, j * 512:(j + 1) * 512], in0=psb[j % 2][0:NSEQ, :],
                                                                in1=adab[:, j * 512:(j + 1) * 512], op=ALU.add),
                         reads=[pk, "adab"], writes=["modsb"])
                P.op("sp", lambda: nc.sync.dma_start(out=mod_d[:, :], in_=modsb[:]), reads=["modsb"], writes=["mod_d"], dma="modsb")
                if "mod" in dbg:
                    t_ = tap("mod", [NSEQ, 6 * D])
                    P.op("sp", lambda: nc.sync.dma_start(out=t_[:, :], in_=modsb[:]), reads=["modsb"], writes=["dbg_mod"], dma="modsb")
                tab = sb("tab", [33, H])
                oh = sb("oh", [33, 2304])
                ef = sb("ef", [H, 2304], BF16)
                P.op("sp", lambda: nc.sync.dma_start(out=tab[:], in_=rel_tab[:, :]), writes=["tab"], dma="tab")
                P.op("sp", lambda: nc.sync.dma_start(out=oh[:], in_=c_bucket[:, :]), writes=["oh"], dma="oh")
                for j in range(5):
                    n = 512 if j < 4 else 256
                    pk = "ps%d" % (2 + j % 2)
                    P.op("pe", lambda: nc.tensor.matmul(psb[2 + j % 2][0:H, 0:n], lhsT=tab[:], rhs=oh[:, j * 512:j * 512 + n], start=True, stop=True),
                         reads=["tab", "oh"], writes=[pk])
                    P.op("act", lambda: nc.scalar.activation(out=ef[:, j * 512:j * 512 + n], in_=psb[2 + j % 2][0:H, 0:n], func=AF.Exp),
                         reads=[pk], writes=["ef"])
                P.op("sp", lambda: nc.sync.dma_start(out=expf_d[:, :], in_=ef[:]), reads=["ef"], writes=["expf_d"], dma="ef")
                wrv = [sb("wrv%d" % i, [128, SEQ], BF16) for i in range(2)]
                wnt = [sb("wnt%d" % i, [128, SEQ], BF16) for i in range(2)]
                for h in range(H):
                    wr_, wrk = wrv[h % 2], "wrv%d" % (h % 2)
                    wn_, wnk = wnt[h % 2], "wnt%d" % (h % 2)
                    P.op("sp", lambda: nc.sync.dma_start(out=wr_[:], in_=bass.AP(expf_d.tensor, h * 2304, [[1, 128], [1, SEQ]])), reads=["expf_d"], writes=[wrk], dma=wrk)
                    for c in range(4):
                        pk = "ps%d" % (4 + c)
                        P.op("pe", lambda: nc.tensor.matmul(psb[4 + c][:, :], lhsT=rev_b[:], rhs=wr_[:, c * 512:(c + 1) * 512], start=True, stop=True), reads=["rev_b", wrk], writes=[pk])
                        if c % 2 == 0:
                            P.op("act", lambda: nc.scalar.copy(out=wn_[:, c * 512:(c + 1) * 512], in_=psb[4 + c][:, :]), reads=[pk], writes=[wnk])
                        else:
                            P.op("dve", lambda: nc.vector.tensor_copy(out=wn_[:, c * 512:(c + 1) * 512], in_=psb[4 + c][:, :]), reads=[pk], writes=[wnk])
                    P.op("sp", lambda: nc.sync.dma_start(out=wnat_d[h, :, :], in_=wn_[:]), reads=[wnk], writes=["wnat_d"], dma=wnk)
                P.barrier()

        GT = 1024
        NG = T // GT
        if "s1" in stages:
            with contextlib.ExitStack() as st:
                sb = lambda n, s, d=F32: st.enter_context(nc.sbuf_tensor(n, list(s), d))
                g1bc = sb("g1bc", [128, D])
                A1 = sb("A1", [128, D])
                sh1 = sb("sh1", [128, D])
                xt = [sb("xt%d" % i, [128, D]) for i in range(2)]
                ut = [sb("ut%d" % i, [128, D]) for i in range(2)]
                sq = sb("sq", [128, D])
                ss = sb("ss", [128, 2])
                uT = sb("uT", [128, 8, GT], BF16)
                wbuf = [sb("wbuf%d" % i, [128, 8, 512], BF16) for i in range(2)]
                stg = [sb("stg%d" % i, [128, 512]) for i in range(2)]
                raw = [sb("raw%d" % i, [128, GT + 3]) for i in range(2)]
                cacc = [sb("cacc%d" % i, [128, GT]) for i in range(2)]
                carry = sb("carry", [128, 24, 3])
                cw = sb("cw", [128, 24, 4])
                cb = sb("cb", [128, 24])
                P.op("sp", lambda: nc.sync.dma_start(out=g1bc[:], in_=bcast_rows(norm1_g, D)), writes=["g1bc"], dma="g1bc")
                P.op("sp", lambda: nc.sync.dma_start(out=cw[:], in_=conv_wT.rearrange("(m p) k -> p m k", p=128)), writes=["cw"], dma="cw")
                P.op("sp", lambda: nc.sync.dma_start(out=cb[:], in_=conv_b[:, :]), writes=["cb"], dma="cb")
                chunks = []
                for j in range(6):
                    chunks.append((OFF_Q + 512 * j, 512, "tm", qkv_d, 512 * j))
                for j in range(4):
                    chunks.append((OFF_Z + 512 * j, 512, "tm", z_d, 512 * j))
                for j in range(6):
                    chunks.append((OFF_XBC + 512 * j, 512, "fm", None, j))
                chunks.append((OFF_DT, 32, "tm", dt_d, 0))
                for j in range(4):
                    chunks.append((OFF_GATE + 512 * j, 512, "tm", gl_d, 512 * j))
                wcount = 0
                scount = 0
                rcount = 0
                for g in range(NG):
                    s = (g * GT) // SEQ
                    if (g * GT) % SEQ == 0:
                        P.op("sp", lambda: nc.sync.dma_start(out=A1[:], in_=bcast_rows(mod_d[s, D:2 * D], D)), reads=["mod_d"], writes=["A1"], dma="A1")
                        P.op("sp", lambda: nc.sync.dma_start(out=sh1[:], in_=bcast_rows(mod_d[s, 0:D], D)), reads=["mod_d"], writes=["sh1"], dma="sh1")
                        P.op("dve", lambda: nc.vector.scalar_tensor_tensor(out=A1[:], in0=A1[:], scalar=1.0, in1=g1bc[:], op0=ALU.add, op1=ALU.mult),
                             reads=["A1", "g1bc"], writes=["A1"])
                        P.op("dve", lambda: nc.vector.memset(carry[:], 0.0), writes=["carry"])
                    for tt in range(GT // 128):
                        t = g * (GT // 128) + tt
                        xb_, xk = xt[t % 2], "xt%d" % (t % 2)
                        ub_, uk = ut[t % 2], "ut%d" % (t % 2)
                        P.op("sp", lambda: nc.sync.dma_start(out=xb_[:], in_=x_d[t * 128:(t + 1) * 128, :]), writes=[xk], dma=xk)
                        P.op("act", lambda: nc.scalar.activation(out=sq[:], in_=xb_[:], func=AF.Square, scale=1.0 / 32.0, accum_out=ss[:, 0:1]),
                             reads=[xk], writes=["sq", "ss"])
                        P.op("dve", lambda: nc.vector.tensor_scalar_add(out=ss[:, 1:2], in0=ss[:, 0:1], scalar1=EPS), reads=["ss"], writes=["ss"])
                        P.op("act", lambda: nc.scalar.sqrt(out=ss[:, 1:2], in_=ss[:, 1:2]), reads=["ss"], writes=["ss"])
                        P.op("dve", lambda: nc.vector.reciprocal(out=ss[:, 1:2], in_=ss[:, 1:2]), reads=["ss"], writes=["ss"])
                        P.op("dve", lambda: nc.vector.scalar_tensor_tensor(out=ub_[:], in0=xb_[:], scalar=ss[:, 1:2], in1=A1[:], op0=ALU.mult, op1=ALU.mult),
                             reads=[xk, "ss", "A1"], writes=[uk])
                        P.op("dve", lambda: nc.vector.tensor_tensor(out=ub_[:], in0=ub_[:], in1=sh1[:], op=ALU.add), reads=[uk, "sh1"], writes=[uk])
                        if "u" in dbg and t < 2:
                            if "u" not in dbg_outs:
                                tap("u", [256, D])
                            P.op("sp", lambda: nc.sync.dma_start(out=dbg_outs["u"][t * 128:(t + 1) * 128, :], in_=ub_[:]), reads=[uk], writes=["dbg_u"], dma=uk)
                        for half in range(2):
                            pb, pk = psb[half], "ps%d" % half
                            for kk in range(4):
                                k = half * 4 + kk
                                P.op("pe", lambda: nc.tensor.matmul(pb[:, kk * 128:(kk + 1) * 128], lhsT=ub_[:, k * 128:(k + 1) * 128], rhs=ident_f[:],
                                                                    start=True, stop=True), reads=[uk, "ident_f"], writes=[pk])
                            P.op("act", lambda: nc.scalar.copy(out=uT[:, half * 4:half * 4 + 4, tt * 128:(tt + 1) * 128],
                                                               in_=pb[:].rearrange("p (k t) -> p k t", k=4)), reads=[pk], writes=["uT"])
                    for (c0, ncol, kind, dest, dcol) in chunks:
                        wb_, wk = wbuf[wcount % 2], "wbuf%d" % (wcount % 2)
                        wcount += 1
                        P.op("pool", lambda: nc.gpsimd.dma_start(out=wb_[:, :, 0:ncol], in_=w_in[:, c0:c0 + ncol].rearrange("(k p) n -> p k n", p=128)),
                             writes=[wk], dma=wk)
                        if kind == "tm":
                            for tt in range(GT // 128):
                                t = g * (GT // 128) + tt
                                pi = 2 + (scount % 4)
                                pb, pk = psb[pi], "ps%d" % pi
                                sg, sk = stg[scount % 2], "stg%d" % (scount % 2)
                                scount += 1
                                for k in range(8):
                                    P.op("pe", lambda: nc.tensor.matmul(pb[:, 0:ncol], lhsT=uT[:, k, tt * 128:(tt + 1) * 128], rhs=wb_[:, k, 0:ncol],
                                                                        start=(k == 0), stop=(k == 7)), reads=["uT", wk], writes=[pk])
                                ev = "act" if scount % 2 else "dve"
                                if ev == "act":
                                    P.op("act", lambda: nc.scalar.copy(out=sg[:, 0:ncol], in_=pb[:, 0:ncol]), reads=[pk], writes=[sk])
                                else:
                                    P.op("dve", lambda: nc.vector.tensor_copy(out=sg[:, 0:ncol], in_=pb[:, 0:ncol]), reads=[pk], writes=[sk])
                                P.op("sp", lambda: nc.sync.dma_start(out=dest[t * 128:(t + 1) * 128, dcol:dcol + ncol], in_=sg[:, 0:ncol]),
                                     reads=[sk], writes=[dest.tensor.name], dma=sk)
                        else:
                            for mm in range(4):
                                m = dcol * 4 + mm
                                rw, rk = raw[rcount % 2], "raw%d" % (rcount % 2)
                                ca, ck = cacc[rcount % 2], "cacc%d" % (rcount % 2)
                                rcount += 1
                                P.op("dve", lambda: nc.vector.tensor_copy(out=rw[:, 0:3], in_=carry[:, m, :]), reads=["carry"], writes=[rk])
                                for hf in range(GT // 512):
                                    pi = 6 + (hf % 2)
                                    pb, pk = psb[pi], "ps%d" % pi
                                    for k in range(8):
                                        P.op("pe", lambda: nc.tensor.matmul(pb[:, :], lhsT=wb_[:, k, mm * 128:(mm + 1) * 128], rhs=uT[:, k, hf * 512:(hf + 1) * 512],
                                                                            start=(k == 0), stop=(k == 7)), reads=["uT", wk], writes=[pk])
                                    P.op("act", lambda: nc.scalar.copy(out=rw[:, 3 + hf * 512:3 + (hf + 1) * 512], in_=pb[:, :]), reads=[pk], writes=[rk])
                                P.op("dve", lambda: nc.vector.tensor_copy(out=carry[:, m, :], in_=rw[:, GT:GT + 3]), reads=[rk], writes=["carry"])
                                P.op("dve", lambda: nc.vector.tensor_scalar(out=ca[:], in0=rw[:, 3:GT + 3], scalar1=cw[:, m, 3:4], scalar2=cb[:, m:m + 1],
                                                                            op0=ALU.mult, op1=ALU.add), reads=[rk, "cw", "cb"], writes=[ck])
                                for tap_ in range(3):
                                    P.op("dve", lambda: nc.vector.scalar_tensor_tensor(out=ca[:], in0=rw[:, tap_:GT + tap_], scalar=cw[:, m, tap_:tap_ + 1], in1=ca[:],
                                                                                       op0=ALU.mult, op1=ALU.add), reads=[rk, ck, "cw"], writes=[ck])
                                P.op("act", lambda: nc.scalar.activation(out=ca[:], in_=ca[:], func=AF.Silu), reads=[ck], writes=[ck])
                                P.op("sp", lambda: nc.sync.dma_start(out=xbcT_d[m * 128:(m + 1) * 128, g * GT:(g + 1) * GT], in_=ca[:]),
                                     reads=[ck], writes=["xbcT_d"], dma=ck)
                P.barrier()

        if "s3" in stages:
            with contextlib.ExitStack() as st:
                sb = lambda n, s, d=F32: st.enter_context(nc.sbuf_tensor(n, list(s), d))
                gq = sb("gq", [128, HD]); gk = sb("gk", [128, HD])
                selm = sb("selm", [128, 3, 16, 8])
                qf = [sb("qf%d" % i, [128, 16, HD]) for i in range(2)]
                kf = [sb("kf%d" % i, [128, 16, HD]) for i in range(2)]
                vf = [sb("vf%d" % i, [128, 16, HD]) for i in range(2)]
                sqt = sb("sqt", [128, 16, HD])
                nrm = sb("nrm", [128, 2, 16])
                qTg = sb("qTg", [64, SEQ], BF16)
                kmT = sb("kmT", [64, 8]); kmb = sb("kmb", [64, 8], BF16)
                gate = sb("gate", [128, 16, 8]); cmpb = sb("cmpb", [128, 16, 8, 8]); rank = sb("rank", [128, 16, 8])
                qaug = sb("qaug", [128, 16, 72], BF16); kb16 = sb("kb16", [128, 16, HD], BF16)
                qTa = [sb("qTa%d" % i, [72, SEQ], BF16) for i in range(2)]
                kTa = [sb("kTa%d" % i, [72, SEQ], BF16) for i in range(2)]
                vaug = [sb("vaug%d" % i, [128, 16, 128], BF16) for i in range(2)]
                Wt = [sb("Wt%d" % i, [128, SEQ], BF16) for i in range(2)]
                pS = [sb("pS%d" % i, [128, 512], BF16) for i in range(3)]
                pW = [sb("pW%d" % i, [128, 512], BF16) for i in range(3)]
                rec = sb("rec", [128, SEQ])
                aT = [sb("aT%d" % i, [64, SEQ], BF16) for i in range(2)]
                P.op("sp", lambda: nc.sync.dma_start(out=gq[:], in_=bcast_rows(q_norm_g, HD)), writes=["gq"], dma="gq")
                P.op("sp", lambda: nc.sync.dma_start(out=gk[:], in_=bcast_rows(k_norm_g, HD)), writes=["gk"], dma="gk")
                P.op("sp", lambda: nc.sync.dma_start(out=selm[:], in_=c_selmask[:, :, :, :]), writes=["selm"], dma="selm")
                for i in range(2):
                    P.op("dve", lambda: nc.vector.memset(vaug[i][:], 1.0), writes=["vaug%d" % i])
                    P.op("pool", lambda: nc.gpsimd.dma_start(out=kTa[i][64:72, :], in_=c_kind[:, :]), writes=["kTa%d" % i], dma="kTa%d" % i)

                def prologue(idx):
                    s_, h = divmod(idx, H)
                    b2_ = idx % 2
                    r0 = s_ * SEQ
                    q_, k_, v_ = qf[b2_], kf[b2_], vf[b2_]
                    qk_, kk_, vk_ = "qf%d" % b2_, "kf%d" % b2_, "vf%d" % b2_
                    qTak, kTak, vak = "qTa%d" % b2_, "kTa%d" % b2_, "vaug%d" % b2_
                    for (buf, nm, off) in [(qf, "qf", OFF_Q), (kf, "kf", OFF_K), (vf, "vf", OFF_V)]:
                        P.op("sp", lambda: nc.sync.dma_start(out=buf[b2_][:], in_=qkv_d[r0:r0 + SEQ, off + h * HD:off + (h + 1) * HD].rearrange("(t p) d -> p t d", p=128)),
                             reads=["qkv_d"], writes=["%s%d" % (nm, b2_)], dma="%s%d" % (nm, b2_))
                    P.op("sp", lambda: nc.sync.dma_start(out=Wt[b2_][:], in_=wnat_d[h, :, :]), reads=["wnat_d"], writes=["Wt%d" % b2_], dma="Wt%d" % b2_)
                    P.op("act", lambda: nc.scalar.copy(out=vaug[b2_][:, :, 0:64], in_=v_[:]), reads=[vk_], writes=[vak])
                    for j, (t_, tk_, g_, gk_) in enumerate([(q_, qk_, gq, "gq"), (k_, kk_, gk, "gk")]):
                        P.op("act", lambda: nc.scalar.activation(out=sqt[:], in_=t_[:], func=AF.Square, scale=0.125), reads=[tk_], writes=["sqt"])
                        P.op("dve", lambda: nc.vector.reduce_sum(out=nrm[:, j, :], in_=sqt[:], axis=AX.X), reads=["sqt"], writes=["nrm"])
                        P.op("dve", lambda: nc.vector.tensor_scalar_add(out=nrm[:, j, :], in0=nrm[:, j, :], scalar1=EPS), reads=["nrm"], writes=["nrm"])
                        P.op("act", lambda: nc.scalar.sqrt(out=nrm[:, j, :], in_=nrm[:, j, :]), reads=["nrm"], writes=["nrm"])
                        P.op("dve", lambda: nc.vector.reciprocal(out=nrm[:, j, :], in_=nrm[:, j, :]), reads=["nrm"], writes=["nrm"])
                        P.op("dve", lambda: nc.vector.tensor_tensor(out=t_[:], in0=t_[:], in1=nrm[:, j, :].unsqueeze(2).to_broadcast([128, 16, HD]), op=ALU.mult),
                             reads=[tk_, "nrm"], writes=[tk_])
                        dst_, dk_ = (qaug[:, :, 0:64], "qaug") if j == 0 else (kb16[:], "kb16")
                        P.op("dve", lambda: nc.vector.tensor_tensor(out=dst_, in0=t_[:], in1=g_[:].unsqueeze(1).to_broadcast([128, 16, HD]), op=ALU.mult),
                             reads=[tk_, gk_], writes=[dk_])
                    for i in range(4):
                        for tt in range(4):
                            P.op("pe", lambda: nc.tensor.matmul(psb[7][0:64, tt * 128:(tt + 1) * 128], lhsT=kb16[:, 4 * i + tt, :], rhs=ident_b[:], start=True, stop=True),
                                 reads=["kb16", "ident_b"], writes=["ps7"])
                        P.op("dve", lambda: nc.vector.reduce_sum(out=kmT[:, 2 * i:2 * i + 2], in_=psb[7][0:64, :].rearrange("p (b k) -> p b k", b=2), axis=AX.X),
                             reads=["ps7"], writes=["kmT"])
                        P.op("dve", lambda: nc.vector.tensor_copy(out=kTa[b2_][0:64, i * 512:(i + 1) * 512], in_=psb[7][0:64, :]), reads=["ps7"], writes=[kTak])
                    P.op("dve", lambda: nc.vector.tensor_scalar_mul(out=kmb[:], in0=kmT[:], scalar1=1.0 / 256.0), reads=["kmT"], writes=["kmb"])
                    for i in range(4):
                        for tt in range(4):
                            P.op("pe", lambda: nc.tensor.matmul(psb[7][0:64, tt * 128:(tt + 1) * 128], lhsT=qaug[:, 4 * i + tt, 0:64], rhs=ident_b[:], start=True, stop=True),
                                 reads=["qaug", "ident_b"], writes=["ps7"])
                        P.op("act", lambda: nc.scalar.copy(out=qTg[:, i * 512:(i + 1) * 512], in_=psb[7][0:64, :]), reads=["ps7"], writes=["qTg"])
                    for t in range(16):
                        P.op("pe", lambda: nc.tensor.matmul(psb[7][:, t * 8:(t + 1) * 8], lhsT=qTg[:, t * 128:(t + 1) * 128], rhs=kmb[:], start=True, stop=True),
                             reads=["qTg", "kmb"], writes=["ps7"])
                    P.op("dve", lambda: nc.vector.tensor_tensor(out=gate[:], in0=psb[7][:, 0:128].rearrange("p (t n) -> p t n", t=16), in1=selm[:, 0, :, :], op=ALU.add),
                         reads=["ps7", "selm"], writes=["gate"])
                    P.op("dve", lambda: nc.vector.tensor_tensor(out=cmpb[:], in0=gate[:].unsqueeze(2).to_broadcast([128, 16, 8, 8]),
                                                                in1=gate[:].unsqueeze(3).to_broadcast([128, 16, 8, 8]), op=ALU.is_gt), reads=["gate"], writes=["cmpb"])
                    P.op("dve", lambda: nc.vector.reduce_sum(out=rank[:], in_=cmpb[:], axis=AX.X), reads=["cmpb"], writes=["rank"])
                    P.op("dve", lambda: nc.vector.tensor_single_scalar(out=rank[:], in_=rank[:], scalar=3.0, op=ALU.is_lt), reads=["rank"], writes=["rank"])
                    P.op("dve", lambda: nc.vector.tensor_tensor(out=rank[:], in0=rank[:], in1=selm[:, 1, :, :], op=ALU.mult), reads=["rank", "selm"], writes=["rank"])
                    P.op("dve", lambda: nc.vector.tensor_tensor(out=rank[:], in0=rank[:], in1=selm[:, 2, :, :], op=ALU.add), reads=["rank", "selm"], writes=["rank"])
                    P.op("dve", lambda: nc.vector.tensor_scalar(out=qaug[:, :, 64:72], in0=rank[:], scalar1=-NEG, scalar2=NEG, op0=ALU.mult, op1=ALU.add),
                         reads=["rank"], writes=["qaug"])
                    for i in range(4):
                        for tt in range(4):
                            P.op("pe", lambda: nc.tensor.matmul(psb[7][0:72, tt * 128:(tt + 1) * 128], lhsT=qaug[:, 4 * i + tt, :], rhs=ident_b[:], start=True, stop=True),
                                 reads=["qaug", "ident_b"], writes=["ps7"])
                        P.op("act", lambda: nc.scalar.copy(out=qTa[b2_][:, i * 512:(i + 1) * 512], in_=psb[7][0:72, :]), reads=["ps7"], writes=[qTak])

                cnt3 = [0]

                def main(idx, inject):
                    s_, h = divmod(idx, H)
                    b2_ = idx % 2
                    r0 = s_ * SEQ
                    qTak, kTak, vak, wk_ = "qTa%d" % b2_, "kTa%d" % b2_, "vaug%d" % b2_, "Wt%d" % b2_
                    its = []
                    for kt in range(16):
                        k0 = kt * 128
                        for c in range(k0 // 512, 4):
                            its.append((kt, k0, c, max(k0, 512 * c), 512 * (c + 1)))

                    def emit_s(j):
                        kt, k0, c, q_lo, q_hi = its[j]
                        sbk = 4 + (base + j) % 3
                        P.op("pe", lambda: nc.tensor.matmul(psb[sbk][:, 0:q_hi - q_lo], lhsT=kTa[b2_][:, k0:k0 + 128], rhs=qTa[b2_][:, q_lo:q_hi], start=True, stop=True),
                             reads=[kTak, qTak], writes=["ps%d" % sbk])

                    base = cnt3[0]
                    cnt3[0] += len(its)
                    LOOK = 2
                    for j in range(min(LOOK, len(its))):
                        emit_s(j)
                    for j in range(len(its)):
                        kt, k0, c, q_lo, q_hi = its[j]
                        n = q_hi - q_lo
                        cn = base + j
                        sbk = 4 + cn % 3
                        ps_, pw_ = pS[cn % 3], pW[cn % 3]
                        psk, pwk = "pS%d" % (cn % 3), "pW%d" % (cn % 3)
                        if j + LOOK < len(its):
                            emit_s(j + LOOK)
                        P.op("act", lambda: nc.scalar.activation(out=ps_[:, 0:n], in_=psb[sbk][:, 0:n], func=AF.Exp, scale=0.125), reads=["ps%d" % sbk], writes=[psk])
                        P.op("dve", lambda: nc.vector.tensor_tensor(out=pw_[:, 0:n], in0=ps_[:, 0:n], in1=Wt[b2_][:, q_lo - k0:q_hi - k0], op=ALU.mult),
                             reads=[psk, wk_], writes=[pwk])
                        P.op("pe", lambda: nc.tensor.matmul(psb[c][:, q_lo - 512 * c:q_hi - 512 * c], lhsT=vaug[b2_][:, kt, :], rhs=pw_[:, 0:n],
                                                            start=(kt == 0), stop=(kt == 4 * c + 3), skip_group_check=True), reads=[vak, pwk], writes=["ps%d" % c])
                        if j == 12:
                            inject()
                    for c in range(4):
                        P.op("act", lambda: nc.scalar.activation(out=rec[64:128, c * 512:(c + 1) * 512], in_=psb[c][64:128, :], func=AF.Ln), reads=["ps%d" % c], writes=["rec"])
                        P.op("act", lambda: nc.scalar.activation(out=rec[64:128, c * 512:(c + 1) * 512], in_=rec[64:128, c * 512:(c + 1) * 512], func=AF.Exp, scale=-1.0), reads=["rec"], writes=["rec"])
                        P.op("dve", lambda: nc.vector.tensor_tensor(out=aT[b2_][:, c * 512:(c + 1) * 512], in0=psb[c][0:64, :], in1=rec[64:128, c * 512:(c + 1) * 512], op=ALU.mult),
                             reads=["ps%d" % c, "rec"], writes=["aT%d" % b2_])
                    P.op("sp", lambda: nc.sync.dma_start(out=attnT_d[h * HD:(h + 1) * HD, r0:r0 + SEQ], in_=aT[b2_][:]), reads=["aT%d" % b2_], writes=["attnT_d"], dma="aT%d" % b2_)

                NHD = NSEQ * H
                prologue(0)
                for idx in range(NHD):
                    main(idx, (lambda i=idx: prologue(i + 1)) if idx + 1 < NHD else (lambda: None))
                P.barrier()

        if "s4" in stages:
            with contextlib.ExitStack() as st:
                sb = lambda n, s, d=F32: st.enter_context(nc.sbuf_tensor(n, list(s), d))
                tri = sb("tri", [128, 128]); negtri = sb("negtri", [128, 128])
                dtb = sb("dtb", [128, SSM_H]); abc = sb("abc", [128, SSM_H]); dsk = sb("dsk", [128, SSM_H])
                sng = sb("sng", [128, SSM_INNER])
                xsT = [sb("xsT%d" % i, [128, 16, 128]) for i in range(2)]
                BTb = [sb("BTb%d" % i, [128, 4, 128], BF16) for i in range(2)]
                CTb = [sb("CTb%d" % i, [128, 4, 128], BF16) for i in range(2)]
                zt = [sb("zt%d" % i, [128, SSM_INNER]) for i in range(2)]
                dtr = [sb("dtr%d" % i, [128, SSM_H]) for i in range(2)]
                sm = sb("sm", [128, 13, SSM_H])
                Rb = sb("Rb", [128, SSM_H, 128])
                xs = sb("xs", [128, SSM_INNER])
                xdt = sb("xdt", [128, SSM_INNER], BF16); xdte = sb("xdte", [128, SSM_INNER], BF16)
                Btm = sb("Btm", [128, 4, 128], BF16)
                Dm = [sb("Dm%d" % i, [128, 4, 128]) for i in range(2)]
                MT = [sb("MT%d" % i, [128, 4, 128], BF16) for i in range(2)]
                Hf = sb("Hf", [128, SSM_INNER]); Hb = sb("Hb", [128, SSM_INNER], BF16)
                ysb = sb("ysb", [128, SSM_INNER]); tmp = sb("tmp4", [128, SSM_INNER])
                nr4 = sb("nr4", [128, 8])
                ssb = sb("ssb", [128, SSM_INNER], BF16)
                sT = [sb("sT%d" % i, [128, 16, 512], BF16) for i in range(2)]
                P.op("sp", lambda: nc.sync.dma_start(out=tri[:], in_=c_tri[:, :]), writes=["tri"], dma="tri")
                P.op("sp", lambda: nc.sync.dma_start(out=negtri[:], in_=c_negtri[:, :]), writes=["negtri"], dma="negtri")
                P.op("sp", lambda: nc.sync.dma_start(out=dtb[:], in_=bcast_rows(dt_bias, SSM_H)), writes=["dtb"], dma="dtb")
                P.op("sp", lambda: nc.sync.dma_start(out=abc[:], in_=bcast_rows(a_log, SSM_H)), writes=["abc"], dma="abc")
                P.op("sp", lambda: nc.sync.dma_start(out=dsk[:], in_=bcast_rows(d_skip, SSM_H)), writes=["dsk"], dma="dsk")
                P.op("sp", lambda: nc.sync.dma_start(out=sng[:], in_=bcast_rows(ssm_norm_g, SSM_INNER)), writes=["sng"], dma="sng")
                P.op("act", lambda: nc.scalar.activation(out=abc[:], in_=abc[:], func=AF.Exp), reads=["abc"], writes=["abc"])
                P.op("dve", lambda: nc.vector.tensor_scalar_mul(out=abc[:], in0=abc[:], scalar1=-1.0), reads=["abc"], writes=["abc"])
                V_ = lambda i: sm[:, i, :]
                bc3 = lambda ap, n, w: ap.unsqueeze(2).to_broadcast([128, n, w])
                for s_ in range(NSEQ):
                    P.op("dve", lambda: nc.vector.memset(Hf[:], 0.0), writes=["Hf"])
                    P.op("dve", lambda: nc.vector.memset(Hb[:], 0.0), writes=["Hb"])
                    for c in range(16):
                        cc = s_ * 16 + c
                        b2_ = cc % 2
                        t0 = cc * 128
                        P.op("sp", lambda: nc.sync.dma_start(out=xsT[b2_][:], in_=xbcT_d[0:2048, t0:t0 + 128].rearrange("(m p) t -> p m t", p=128)),
                             reads=["xbcT_d"], writes=["xsT%d" % b2_], dma="xsT%d" % b2_)
                        P.op("pool", lambda: nc.gpsimd.dma_start(out=BTb[b2_][:], in_=xbcT_d[2048:2560, t0:t0 + 128].rearrange("(m p) t -> p m t", p=128)),
                             reads=["xbcT_d"], writes=["BTb%d" % b2_], dma="BTb%d" % b2_)
                        P.op("pool", lambda: nc.gpsimd.dma_start(out=CTb[b2_][:], in_=xbcT_d[2560:3072, t0:t0 + 128].rearrange("(m p) t -> p m t", p=128)),
                             reads=["xbcT_d"], writes=["CTb%d" % b2_], dma="CTb%d" % b2_)
                        P.op("sp", lambda: nc.sync.dma_start(out=zt[b2_][:], in_=z_d[t0:t0 + 128, :]), reads=["z_d"], writes=["zt%d" % b2_], dma="zt%d" % b2_)
                        P.op("sp", lambda: nc.sync.dma_start(out=dtr[b2_][:], in_=dt_d[t0:t0 + 128, :]), reads=["dt_d"], writes=["dtr%d" % b2_], dma="dtr%d" % b2_)
                        xk, bk, ck, zk, dk = "xsT%d" % b2_, "BTb%d" % b2_, "CTb%d" % b2_, "zt%d" % b2_, "dtr%d" % b2_
                        P.op("dve", lambda: nc.vector.tensor_tensor(out=V_(0), in0=dtr[b2_][:], in1=dtb[:], op=ALU.add), reads=[dk, "dtb"], writes=["sm0"])
                        P.op("act", lambda: nc.scalar.activation(out=V_(1), in_=V_(0), func=AF.Abs), reads=["sm0"], writes=["sm1"])
                        P.op("act", lambda: nc.scalar.activation(out=V_(2), in_=V_(1), func=AF.Exp, scale=-1.0), reads=["sm1"], writes=["sm2"])
                        P.op("act", lambda: nc.scalar.activation(out=V_(2), in_=V_(2), func=AF.Ln, bias=1.0), reads=["sm2"], writes=["sm2"])
                        P.op("dve", lambda: nc.vector.scalar_tensor_tensor(out=V_(3), in0=V_(0), scalar=0.0, in1=V_(2), op0=ALU.max, op1=ALU.add), reads=["sm0", "sm2"], writes=["sm3"])
                        P.op("dve", lambda: nc.vector.tensor_tensor(out=V_(4), in0=V_(3), in1=abc[:], op=ALU.mult), reads=["sm3", "abc"], writes=["sm4"])
                        P.op("pe", lambda: nc.tensor.matmul(psb[0][:, 0:32], lhsT=tri[:], rhs=V_(4), start=True, stop=True), reads=["tri", "sm4"], writes=["ps0"])
                        P.op("pe", lambda: nc.tensor.matmul(psb[0][:, 32:64], lhsT=ones_f[:], rhs=V_(4), start=True, stop=True), reads=["ones_f", "sm4"], writes=["ps0"])
                        P.op("act", lambda: nc.scalar.copy(out=sm[:, 5:7, :], in_=psb[0][:, 0:64].rearrange("p (a h) -> p a h", a=2)), reads=["ps0"], writes=["sm5", "sm6"])
                        P.op("dve", lambda: nc.vector.tensor_tensor(out=V_(7), in0=V_(6), in1=V_(5), op=ALU.subtract), reads=["sm5", "sm6"], writes=["sm7"])
                        P.op("act", lambda: nc.scalar.activation(out=sm[:, 10:13, :], in_=sm[:, 5:8, :], func=AF.Exp), reads=["sm5", "sm6", "sm7"], writes=["sm10", "sm11", "sm12"])
                        EACS, CD, DTE = 10, 11, 12
                        P.op("dve", lambda: nc.vector.tensor_tensor(out=Rb[:], in0=tri[:].unsqueeze(1).to_broadcast([128, SSM_H, 128]), in1=bc3(V_(4), SSM_H, 128), op=ALU.mult),
                             reads=["tri", "sm4"], writes=["Rb"])
                        for m in range(16):
                            P.op("pe", lambda: nc.tensor.matmul(psb[3 + m // 4][:, (m % 4) * 128:(m % 4 + 1) * 128], lhsT=xsT[b2_][:, m, :], rhs=ident_f[:], start=True, stop=True),
                                 reads=[xk, "ident_f"], writes=["ps%d" % (3 + m // 4)])
                        for i in range(4):
                            P.op("act", lambda: nc.scalar.copy(out=xs[:, i * 512:(i + 1) * 512], in_=psb[3 + i][:, :]), reads=["ps%d" % (3 + i)], writes=["xs"])
                        xs3 = xs[:].rearrange("p (h d) -> p h d", h=SSM_H)
                        P.op("dve", lambda: nc.vector.tensor_tensor(out=xdt[:].rearrange("p (h d) -> p h d", h=SSM_H), in0=xs3, in1=bc3(V_(3), SSM_H, 64), op=ALU.mult),
                             reads=["xs", "sm3"], writes=["xdt"])
                        P.op("dve", lambda: nc.vector.tensor_tensor(out=xdte[:].rearrange("p (h d) -> p h d", h=SSM_H), in0=xdt[:].rearrange("p (h d) -> p h d", h=SSM_H),
                                                                    in1=bc3(V_(DTE), SSM_H, 64), op=ALU.mult), reads=["xdt", "sm12"], writes=["xdte"])
                        for g in range(4):
                            P.op("pe", lambda: nc.tensor.matmul(psb[7][:, g * 128:(g + 1) * 128], lhsT=BTb[b2_][:, g, :], rhs=ident_b[:], start=True, stop=True),
                                 reads=[bk, "ident_b"], writes=["ps7"])
                        P.op("act", lambda: nc.scalar.copy(out=Btm[:], in_=psb[7][:, :].rearrange("p (g n) -> p g n", g=4)), reads=["ps7"], writes=["Btm"])
                        for g in range(4):
                            P.op("pe", lambda: nc.tensor.matmul(psb[2][:, g * 128:(g + 1) * 128], lhsT=BTb[b2_][:, g, :], rhs=CTb[b2_][:, g, :], start=True, stop=True),
                                 reads=[bk, ck], writes=["ps2"])
                        def emit_ones(j):
                            P.op("pe", lambda: nc.tensor.matmul(psb[j % 2][:, :], lhsT=ones_f[:], rhs=Rb[:, 4 * j:4 * j + 4, :], start=True, stop=True),
                                 reads=["ones_f", "Rb"], writes=["ps%d" % (j % 2)])
                        emit_ones(0)
                        for g in range(4):
                            P.op("pe", lambda: nc.tensor.matmul(psb[4][:, :], lhsT=CTb[b2_][:, g, :], rhs=Hb[:, g * 512:(g + 1) * 512], start=True, stop=True),
                                 reads=[ck, "Hb"], writes=["ps4"])
                            P.op("pe", lambda: nc.tensor.matmul(psb[5][:, :], lhsT=Btm[:, g, :], rhs=xdte[:, g * 512:(g + 1) * 512], start=True, stop=True),
                                 reads=["Btm", "xdte"], writes=["ps5"])
                            for j2 in range(2):
                                j = g * 2 + j2
                                pb = j % 2
                                dm, dmk = Dm[j % 2], "Dm%d" % (j % 2)
                                mt, mtk = MT[j % 2], "MT%d" % (j % 2)
                                P.op("dve", lambda: nc.vector.tensor_tensor(out=dm[:], in0=psb[pb][:, :].rearrange("p (h l) -> p h l", h=4), in1=bc3(sm[:, 5, 4 * j:4 * j + 4], 4, 128), op=ALU.subtract),
                                     reads=["ps%d" % pb, "sm5"], writes=[dmk])
                                if j + 1 < 8:
                                    emit_ones(j + 1)
                                P.op("dve", lambda: nc.vector.tensor_tensor(out=dm[:], in0=dm[:], in1=negtri[:].unsqueeze(1).to_broadcast([128, 4, 128]), op=ALU.add),
                                     reads=[dmk, "negtri"], writes=[dmk])
                                P.op("act", lambda: nc.scalar.activation(out=dm[:], in_=dm[:], func=AF.Exp), reads=[dmk], writes=[dmk])
                                P.op("dve", lambda: nc.vector.tensor_tensor(out=mt[:], in0=dm[:], in1=psb[2][:, g * 128:(g + 1) * 128].unsqueeze(1).to_broadcast([128, 4, 128]), op=ALU.mult),
                                     reads=[dmk, "ps2"], writes=[mtk])
                                for hh in range(4):
                                    hd_ = 4 * j + hh
                                    P.op("pe", lambda: nc.tensor.matmul(psb[3][:, (hd_ % 8) * 64:(hd_ % 8 + 1) * 64], lhsT=mt[:, hh, :], rhs=xdt[:, hd_ * 64:(hd_ + 1) * 64], start=True, stop=True),
                                         reads=[mtk, "xdt"], writes=["ps3"])
                            ysl = ysb[:, g * 512:(g + 1) * 512].rearrange("p (h d) -> p h d", h=8)
                            P.op("dve", lambda: nc.vector.tensor_tensor(out=ysl, in0=psb[4][:, :].rearrange("p (h d) -> p h d", h=8), in1=bc3(sm[:, EACS, 8 * g:8 * g + 8], 8, 64), op=ALU.mult),
                                 reads=["ps4", "sm10"], writes=["ysb"])
                            P.op("dve", lambda: nc.vector.tensor_tensor(out=ysb[:, g * 512:(g + 1) * 512], in0=ysb[:, g * 512:(g + 1) * 512], in1=psb[3][:, :], op=ALU.add),
                                 reads=["ysb", "ps3"], writes=["ysb"])
                            hsl = Hf[:, g * 512:(g + 1) * 512]
                            P.op("dve", lambda: nc.vector.tensor_tensor(out=hsl.rearrange("p (h d) -> p h d", h=8), in0=hsl.rearrange("p (h d) -> p h d", h=8),
                                                                        in1=bc3(sm[:, CD, 8 * g:8 * g + 8], 8, 64), op=ALU.mult), reads=["Hf", "sm11"], writes=["Hf"])
                            P.op("dve", lambda: nc.vector.tensor_tensor(out=hsl, in0=hsl, in1=psb[5][:, :], op=ALU.add), reads=["Hf", "ps5"], writes=["Hf"])
                        P.op("act", lambda: nc.scalar.copy(out=Hb[:], in_=Hf[:]), reads=["Hf"], writes=["Hb"])
                        P.op("dve", lambda: nc.vector.tensor_tensor(out=tmp[:].rearrange("p (h d) -> p h d", h=SSM_H), in0=xs3, in1=bc3(dsk[:], SSM_H, 64), op=ALU.mult),
                             reads=["xs", "dsk"], writes=["tmp4"])
                        P.op("dve", lambda: nc.vector.tensor_tensor(out=ysb[:], in0=ysb[:], in1=tmp[:], op=ALU.add), reads=["ysb", "tmp4"], writes=["ysb"])
                        P.op("act", lambda: nc.scalar.activation(out=tmp[:], in_=zt[b2_][:], func=AF.Silu), reads=[zk], writes=["tmp4"])
                        P.op("dve", lambda: nc.vector.tensor_tensor(out=ysb[:], in0=ysb[:], in1=tmp[:], op=ALU.mult), reads=["ysb", "tmp4"], writes=["ysb"])
                        for g in range(4):
                            P.op("act", lambda: nc.scalar.activation(out=tmp[:, g * 512:(g + 1) * 512], in_=ysb[:, g * 512:(g + 1) * 512], func=AF.Square,
                                                                     scale=float(512 ** -0.5), accum_out=nr4[:, g:g + 1]), reads=["ysb"], writes=["tmp4", "nr4"])
                        P.op("dve", lambda: nc.vector.tensor_scalar_add(out=nr4[:, 4:8], in0=nr4[:, 0:4], scalar1=EPS), reads=["nr4"], writes=["nr4"])
                        P.op("act", lambda: nc.scalar.sqrt(out=nr4[:, 4:8], in_=nr4[:, 4:8]), reads=["nr4"], writes=["nr4"])
                        P.op("dve", lambda: nc.vector.reciprocal(out=nr4[:, 4:8], in_=nr4[:, 4:8]), reads=["nr4"], writes=["nr4"])
                        P.op("dve", lambda: nc.vector.tensor_tensor(out=ysb[:].rearrange("p (g d) -> p g d", g=4), in0=ysb[:].rearrange("p (g d) -> p g d", g=4),
                                                                    in1=bc3(nr4[:, 4:8], 4, 512), op=ALU.mult), reads=["ysb", "nr4"], writes=["ysb"])
                        P.op("dve", lambda: nc.vector.tensor_tensor(out=ssb[:], in0=ysb[:], in1=sng[:], op=ALU.mult), reads=["ysb", "sng"], writes=["ssb"])
                        so, sok = sT[(cc // 4) % 2], "sT%d" % ((cc // 4) % 2)
                        for m in range(16):
                            P.op("pe", lambda: nc.tensor.matmul(psb[3 + m // 4][:, (m % 4) * 128:(m % 4 + 1) * 128], lhsT=ssb[:, m * 128:(m + 1) * 128], rhs=ident_b[:], start=True, stop=True),
                                 reads=["ssb", "ident_b"], writes=["ps%d" % (3 + m // 4)])
                        for i in range(4):
                            P.op("act", lambda: nc.scalar.copy(out=so[:, 4 * i:4 * i + 4, (cc % 4) * 128:(cc % 4 + 1) * 128], in_=psb[3 + i][:, :].rearrange("p (m t) -> p m t", m=4)),
                                 reads=["ps%d" % (3 + i)], writes=[sok])
                        if cc % 4 == 3:
                            tb = (cc // 4) * 512
                            P.op("sp", lambda: nc.sync.dma_start(out=ssmT_d[:, tb:tb + 512].rearrange("(m p) t -> p m t", p=128), in_=so[:]), reads=[sok], writes=["ssmT_d"], dma=sok)
                P.barrier()

        if "s5" in stages:
            with contextlib.ExitStack() as st:
                sb = lambda n, s, d=F32: st.enter_context(nc.sbuf_tensor(n, list(s), d))
                Wa = sb("Wa", [128, 8, D], BF16); Ws = sb("Ws", [128, 16, D], BF16); Wo = sb("Wo", [128, 8, D], BF16)
                rw = sb("rw", [128, 8, NE]); rbb = sb("rbb", [128, NE])
                gbb = sb("gbb", [128, 2 * D]); g2bc = sb("g2bc", [128, D])
                g1b = [sb("g1b%d" % i, [128, D]) for i in range(2)]; A2 = [sb("A2%d" % i, [128, D]) for i in range(2)]; sh2 = [sb("sh2%d" % i, [128, D]) for i in range(2)]
                aTt = [sb("aTt%d" % i, [128, 8, 128], BF16) for i in range(2)]
                sTt = [sb("sTt%d" % i, [128, 16, 128], BF16) for i in range(2)]
                glt = [sb("glt%d" % i, [128, 2 * D]) for i in range(2)]
                xt5 = [sb("xt5%d" % i, [128, D]) for i in range(2)]
                mrg = sb("mrg", [128, D]); tm5 = sb("tm5", [128, D]); mrb = [sb("mrb%d" % i, [128, D], BF16) for i in range(2)]
                mT = sb("mT", [128, 8, 128], BF16)
                x1t = [sb("x1t%d" % i, [128, D]) for i in range(2)]
                u2 = sb("u2", [128, D]); u2Tf = sb("u2Tf", [128, 8, 128])
                u2Tb = [sb("u2Tb%d" % i, [128, 8, 512], BF16) for i in range(2)]
                ss5 = sb("ss5", [128, 2]); sq5 = sb("sq5", [128, D])
                lg = sb("lg", [128, NE]); m8 = sb("m8", [128, 8]); ex = sb("ex", [128, NE]); msk = sb("msk", [128, NE])
                sm5 = sb("sm5_", [128, 4]); gwt = [sb("gwt%d" % i, [128, NE]) for i in range(2)]
                gwT = [sb("gwT%d" % i, [NE, 128]) for i in range(2)]
                for (wsb, wnm, wdr, nk) in [(Wa, "Wa", w_attn, 8), (Ws, "Ws", w_ssm, 16), (Wo, "Wo", w_out, 8)]:
                    for k in range(0, nk, 8):
                        for n in range(2):
                            P.op("pool", lambda: nc.gpsimd.dma_start(out=wsb[:, k:k + 8, n * 512:(n + 1) * 512],
                                                                     in_=wdr[k * 128:(k + 8) * 128, n * 512:(n + 1) * 512].rearrange("(k p) n -> p k n", p=128)),
                                 reads=["wchain"], writes=[wnm, "wchain"], dma="%s_%d_%d" % (wnm, k, n))
                P.op("sp", lambda: nc.sync.dma_start(out=rw[:], in_=router_w.rearrange("(k p) n -> p k n", p=128)), writes=["rw"], dma="rw")
                P.op("sp", lambda: nc.sync.dma_start(out=rbb[:], in_=bcast_rows(router_b, NE)), writes=["rbb"], dma="rbb")
                P.op("sp", lambda: nc.sync.dma_start(out=gbb[:], in_=bcast_rows(gate_bias, 2 * D)), writes=["gbb"], dma="gbb")
                P.op("sp", lambda: nc.sync.dma_start(out=g2bc[:], in_=bcast_rows(norm2_g, D)), writes=["g2bc"], dma="g2bc")
                def st_a1(t):
                        s_ = t // 16
                        b2_ = t % 2
                        t0 = t * 128
                        ak, sk, gk_, xk = "aTt%d" % b2_, "sTt%d" % b2_, "glt%d" % b2_, "xt5%d" % b2_
                        x1_, x1k = x1t[b2_], "x1t%d" % b2_
                        gw_, gwk = gwt[b2_], "gwt%d" % b2_
                        ub_, ubk = u2Tb[(t // 4) % 2], "u2Tb%d" % ((t // 4) % 2)
                        if t % 16 == 0:
                            P.op("sp", lambda: nc.sync.dma_start(out=g1b[s_][:], in_=bcast_rows(mod_d[s_, 2 * D:3 * D], D)), reads=["mod_d"], writes=["g1b%d" % s_], dma="g1b%d" % s_)
                            P.op("sp", lambda: nc.sync.dma_start(out=sh2[s_][:], in_=bcast_rows(mod_d[s_, 3 * D:4 * D], D)), reads=["mod_d"], writes=["sh2%d" % s_], dma="sh2%d" % s_)
                            P.op("sp", lambda: nc.sync.dma_start(out=A2[s_][:], in_=bcast_rows(mod_d[s_, 4 * D:5 * D], D)), reads=["mod_d"], writes=["A2%d" % s_], dma="A2%d" % s_)
                            P.op("dve", lambda: nc.vector.scalar_tensor_tensor(out=A2[s_][:], in0=A2[s_][:], scalar=1.0, in1=g2bc[:], op0=ALU.add, op1=ALU.mult),
                                 reads=["A2%d" % s_, "g2bc"], writes=["A2%d" % s_])
                        ak, sk, gk_, xk = "aTt%d" % b2_, "sTt%d" % b2_, "glt%d" % b2_, "xt5%d" % b2_
                        P.op("sp", lambda: nc.sync.dma_start(out=aTt[b2_][:], in_=attnT_d[:, t0:t0 + 128].rearrange("(k p) t -> p k t", p=128)), reads=["attnT_d"], writes=[ak], dma=ak)
                        P.op("sp", lambda: nc.sync.dma_start(out=sTt[b2_][:], in_=ssmT_d[:, t0:t0 + 128].rearrange("(k p) t -> p k t", p=128)), reads=["ssmT_d"], writes=[sk], dma=sk)
                        P.op("sp", lambda: nc.sync.dma_start(out=glt[b2_][:], in_=gl_d[t0:t0 + 128, :]), reads=["gl_d"], writes=[gk_], dma=gk_)
                        P.op("sp", lambda: nc.sync.dma_start(out=xt5[b2_][:], in_=x_d[t0:t0 + 128, :]), writes=[xk], dma=xk)
                        P.op("dve", lambda: nc.vector.tensor_tensor(out=glt[b2_][:], in0=glt[b2_][:], in1=gbb[:], op=ALU.add), reads=[gk_, "gbb"], writes=[gk_])
                        P.op("act", lambda: nc.scalar.activation(out=glt[b2_][:], in_=glt[b2_][:], func=AF.Sigmoid), reads=[gk_], writes=[gk_])
                        for n in range(2):
                            for k in range(8):
                                P.op("pe", lambda: nc.tensor.matmul(psb[n][:, :], lhsT=aTt[b2_][:, k, :], rhs=Wa[:, k, n * 512:(n + 1) * 512], start=(k == 0), stop=(k == 7)),
                                     reads=[ak, "Wa"], writes=["ps%d" % n])
                            P.op("dve", lambda: nc.vector.tensor_tensor(out=mrg[:, n * 512:(n + 1) * 512], in0=psb[n][:, :], in1=glt[b2_][:, n * 512:(n + 1) * 512], op=ALU.mult),
                                 reads=["ps%d" % n, gk_], writes=["mrg"])
                            for k in range(16):
                                P.op("pe", lambda: nc.tensor.matmul(psb[2 + n][:, :], lhsT=sTt[b2_][:, k, :], rhs=Ws[:, k, n * 512:(n + 1) * 512], start=(k == 0), stop=(k == 15)),
                                     reads=[sk, "Ws"], writes=["ps%d" % (2 + n)])
                            P.op("dve", lambda: nc.vector.tensor_tensor(out=tm5[:, n * 512:(n + 1) * 512], in0=psb[2 + n][:, :], in1=glt[b2_][:, D + n * 512:D + (n + 1) * 512], op=ALU.mult),
                                 reads=["ps%d" % (2 + n), gk_], writes=["tm5"])
                        P.op("dve", lambda: nc.vector.tensor_tensor(out=mrb[b2_][:], in0=mrg[:], in1=tm5[:], op=ALU.add), reads=["mrg", "tm5"], writes=["mrb%d" % b2_])

                def st_a2(t):
                        s_ = t // 16
                        b2_ = t % 2
                        t0 = t * 128
                        ak, sk, gk_, xk = "aTt%d" % b2_, "sTt%d" % b2_, "glt%d" % b2_, "xt5%d" % b2_
                        x1_, x1k = x1t[b2_], "x1t%d" % b2_
                        gw_, gwk = gwt[b2_], "gwt%d" % b2_
                        ub_, ubk = u2Tb[(t // 4) % 2], "u2Tb%d" % ((t // 4) % 2)
                        for half in range(2):
                            for kk in range(4):
                                k = half * 4 + kk
                                P.op("pe", lambda: nc.tensor.matmul(psb[4][:, kk * 128:(kk + 1) * 128], lhsT=mrb[b2_][:, k * 128:(k + 1) * 128], rhs=ident_b[:], start=True, stop=True),
                                     reads=["mrb%d" % b2_, "ident_b"], writes=["ps4"])
                            P.op("act", lambda: nc.scalar.copy(out=mT[:, half * 4:half * 4 + 4, :], in_=psb[4][:, :].rearrange("p (k t) -> p k t", k=4)),
                                 reads=["ps4"], writes=["mT"])
                        x1_, x1k = x1t[b2_], "x1t%d" % b2_
                        for n in range(2):
                            for k in range(8):
                                P.op("pe", lambda: nc.tensor.matmul(psb[6 + n][:, :], lhsT=mT[:, k, :], rhs=Wo[:, k, n * 512:(n + 1) * 512], start=(k == 0), stop=(k == 7)),
                                     reads=["mT", "Wo"], writes=["ps%d" % (6 + n)])
                            P.op("dve", lambda: nc.vector.tensor_tensor(out=x1_[:, n * 512:(n + 1) * 512], in0=psb[6 + n][:, :], in1=g1b[s_][:, n * 512:(n + 1) * 512], op=ALU.mult),
                                 reads=["ps%d" % (6 + n), "g1b%d" % s_], writes=[x1k])
                        P.op("dve", lambda: nc.vector.tensor_tensor(out=x1_[:], in0=x1_[:], in1=xt5[b2_][:], op=ALU.add), reads=[x1k, xk], writes=[x1k])
                        P.op("sp", lambda: nc.sync.dma_start(out=x1_d[t0:t0 + 128, :], in_=x1_[:]), reads=[x1k], writes=["x1_d"], dma=x1k)

                def st_b(t):
                        s_ = t // 16
                        b2_ = t % 2
                        t0 = t * 128
                        ak, sk, gk_, xk = "aTt%d" % b2_, "sTt%d" % b2_, "glt%d" % b2_, "xt5%d" % b2_
                        x1_, x1k = x1t[b2_], "x1t%d" % b2_
                        gw_, gwk = gwt[b2_], "gwt%d" % b2_
                        ub_, ubk = u2Tb[(t // 4) % 2], "u2Tb%d" % ((t // 4) % 2)
                        P.op("act", lambda: nc.scalar.activation(out=sq5[:], in_=x1_[:], func=AF.Square, scale=1.0 / 32.0, accum_out=ss5[:, 0:1]), reads=[x1k], writes=["sq5", "ss5"])
                        P.op("dve", lambda: nc.vector.tensor_scalar_add(out=ss5[:, 1:2], in0=ss5[:, 0:1], scalar1=EPS), reads=["ss5"], writes=["ss5"])
                        P.op("act", lambda: nc.scalar.sqrt(out=ss5[:, 1:2], in_=ss5[:, 1:2]), reads=["ss5"], writes=["ss5"])
                        P.op("dve", lambda: nc.vector.reciprocal(out=ss5[:, 1:2], in_=ss5[:, 1:2]), reads=["ss5"], writes=["ss5"])
                        P.op("dve", lambda: nc.vector.scalar_tensor_tensor(out=u2[:], in0=x1_[:], scalar=ss5[:, 1:2], in1=A2[s_][:], op0=ALU.mult, op1=ALU.mult),
                             reads=[x1k, "ss5", "A2%d" % s_], writes=["u2"])
                        P.op("dve", lambda: nc.vector.tensor_tensor(out=u2[:], in0=u2[:], in1=sh2[s_][:], op=ALU.add), reads=["u2", "sh2%d" % s_], writes=["u2"])
                        ub_, ubk = u2Tb[(t // 4) % 2], "u2Tb%d" % ((t // 4) % 2)
                        for half in range(2):
                            for kk in range(4):
                                k = half * 4 + kk
                                P.op("pe", lambda: nc.tensor.matmul(psb[5][:, kk * 128:(kk + 1) * 128], lhsT=u2[:, k * 128:(k + 1) * 128], rhs=ident_f[:], start=True, stop=True),
                                     reads=["u2", "ident_f"], writes=["ps5"])
                            P.op("act", lambda: nc.scalar.copy(out=u2Tf[:, half * 4:half * 4 + 4, :], in_=psb[5][:, :].rearrange("p (k t) -> p k t", k=4)),
                                 reads=["ps5"], writes=["u2Tf"])
                            P.op("dve", lambda: nc.vector.tensor_copy(out=ub_[:, half * 4:half * 4 + 4, (t % 4) * 128:(t % 4 + 1) * 128], in_=psb[5][:, :].rearrange("p (k t) -> p k t", k=4)),
                                 reads=["ps5"], writes=[ubk])
                        if t % 4 == 3:
                            tb = (t // 4) * 512
                            P.op("sp", lambda: nc.sync.dma_start(out=u2T_d[:, tb:tb + 512].rearrange("(k p) t -> p k t", p=128), in_=ub_[:]), reads=[ubk], writes=["u2T_d"], dma=ubk)
                        for k in range(8):
                            P.op("pe", lambda: nc.tensor.matmul(psb[5][:, 0:NE], lhsT=u2Tf[:, k, :], rhs=rw[:, k, :], start=(k == 0), stop=(k == 7)), reads=["u2Tf", "rw"], writes=["ps5"])
                        P.op("dve", lambda: nc.vector.tensor_tensor(out=lg[:], in0=psb[5][:, 0:NE], in1=rbb[:], op=ALU.add), reads=["ps5", "rbb"], writes=["lg"])
                        P.op("dve", lambda: nc.vector.max(out=m8[:], in_=lg[:]), reads=["lg"], writes=["m8"])
                        P.op("dve", lambda: nc.vector.tensor_scalar(out=msk[:], in0=lg[:], scalar1=m8[:, 3:4], scalar2=None, op0=ALU.is_ge), reads=["lg", "m8"], writes=["msk"])
                        P.op("dve", lambda: nc.vector.tensor_scalar_mul(out=sm5[:, 0:1], in0=m8[:, 0:1], scalar1=-1.0), reads=["m8"], writes=["sm5_"])
                        P.op("act", lambda: nc.scalar.activation(out=ex[:], in_=lg[:], func=AF.Exp, bias=sm5[:, 0:1], scale=1.0), reads=["lg", "sm5_"], writes=["ex"])
                        P.op("dve", lambda: nc.vector.tensor_tensor(out=ex[:], in0=ex[:], in1=msk[:], op=ALU.mult), reads=["ex", "msk"], writes=["ex"])
                        P.op("dve", lambda: nc.vector.reduce_sum(out=sm5[:, 1:2], in_=ex[:], axis=AX.X), reads=["ex"], writes=["sm5_"])
                        P.op("dve", lambda: nc.vector.reciprocal(out=sm5[:, 2:3], in_=sm5[:, 1:2]), reads=["sm5_"], writes=["sm5_"])
                        gw_, gwk = gwt[b2_], "gwt%d" % b2_
                        P.op("dve", lambda: nc.vector.tensor_scalar(out=gw_[:], in0=ex[:], scalar1=sm5[:, 2:3], scalar2=None, op0=ALU.mult), reads=["ex", "sm5_"], writes=[gwk])
                        P.op("sp", lambda: nc.sync.dma_start(out=gwtm_d[t0:t0 + 128, :], in_=gw_[:]), reads=[gwk], writes=["gwtm_d"], dma=gwk)
                        P.op("pe", lambda: nc.tensor.matmul(psb[5][0:NE, 128:256], lhsT=gw_[:], rhs=ident_f[:], start=True, stop=True), reads=[gwk, "ident_f"], writes=["ps5"])
                        P.op("act", lambda: nc.scalar.copy(out=gwT[b2_][:], in_=psb[5][0:NE, 128:256]), reads=["ps5"], writes=["gwT%d" % b2_])
                        P.op("sp", lambda: nc.sync.dma_start(out=gw_d[:, t0:t0 + 128], in_=gwT[b2_][:]), reads=["gwT%d" % b2_], writes=["gw_d"], dma="gwT%d" % b2_)

                for i in range(NT + 2):
                    if i < NT:
                        st_a1(i)
                    if 0 <= i - 1 < NT:
                        st_a2(i - 1)
                    if 0 <= i - 2 < NT:
                        st_b(i - 2)
                P.barrier()

        if "s6" in stages:
            PT = 1024
            with contextlib.ExitStack() as st:
                sb = lambda n, s, d=F32: st.enter_context(nc.sbuf_tensor(n, list(s), d))
                u2T = sb("u2T6", [128, 8, PT], BF16)
                gwT6 = sb("gwT6", [NE, PT])
                b1s = sb("b1s", [128, NE, 16]); b2s = sb("b2s", [NE, D]); g2b = sb("g2b", [128, D])
                yacc = sb("yacc", [128, PT // 128, D])
                w1a = [sb("w1a%d" % i, [128, 8, 512], BF16) for i in range(3)]
                w1l = [sb("w1l%d" % i, [128, 8, 512], BF16) for i in range(3)]
                w2b = [sb("w2b%d" % i, [128, 8, 512], BF16) for i in range(4)]
                gwtm = sb("gwtm", [128, PT // 128, NE])
                ebuf = {nm: [sb("%s%d" % (nm, i), [128, 512]) for i in range(2)] for nm in ("gg", "ll", "t1", "t2")}
                actT = [[sb("actT%d%d" % (a, b), [128, 8, 512], BF16) for b in range(2)] for a in range(2)]
                x16 = [sb("x16%d" % i, [128, D]) for i in range(2)]
                P.op("sp", lambda: nc.sync.dma_start(out=b1s[:], in_=b1T[:, :, :]), writes=["b1s"], dma="b1s")
                P.op("sp", lambda: nc.sync.dma_start(out=b2s[:], in_=b2[:, :]), writes=["b2s"], dma="b2s")
                b17 = sb("b17", [128, NE, 8])
                P.op("dve", lambda: nc.vector.tensor_scalar_add(out=b17[:], in0=b1s[:, :, 8:16], scalar1=7.0), reads=["b1s"], writes=["b17"])
                NPASS = T // PT
                chunks = [(ps_, e, hh) for ps_ in range(NPASS) for e in range(NE) for hh in range(2)]
                state = {"w1next": 0, "ecnt": 0}

                def emit_w1(upto):
                    while state["w1next"] <= min(upto, len(chunks) - 1):
                        j = state["w1next"]
                        _, e, hh = chunks[j]
                        for (wl_, nm, c0) in [(w1a, "w1a", hh * 512), (w1l, "w1l", FF + hh * 512)]:
                            wk = "%s%d" % (nm, j % 3)
                            P.op("pool", lambda: nc.gpsimd.dma_start(out=wl_[j % 3][:], in_=w1[e, :, c0:c0 + 512].rearrange("(k p) n -> p k n", p=128)),
                                 writes=[wk], dma=wk)
                        state["w1next"] += 1

                def h_phase(ps_, e, jbase):
                    par = e % 2
                    for hh in range(2):
                        j = jbase + hh
                        emit_w1(j + 2)
                        wa_, wak = w1a[j % 3], "w1a%d" % (j % 3)
                        wl2_, wlk = w1l[j % 3], "w1l%d" % (j % 3)
                        for ii in range(4):
                            i = hh * 4 + ii
                            for grp in range(2):
                                n_ = state["ecnt"]; state["ecnt"] += 1
                                pa, pb = (n_ % 2) * 2, (n_ % 2) * 2 + 1
                                for k in range(8):
                                    P.op("pe", lambda: nc.tensor.matmul(psb[pa][:, :], lhsT=wa_[:, k, ii * 128:(ii + 1) * 128], rhs=u2T[:, k, grp * 512:(grp + 1) * 512], start=(k == 0), stop=(k == 7)),
                                         reads=[wak, "u2T6"], writes=["ps%d" % pa])
                                for k in range(8):
                                    P.op("pe", lambda: nc.tensor.matmul(psb[pb][:, :], lhsT=wl2_[:, k, ii * 128:(ii + 1) * 128], rhs=u2T[:, k, grp * 512:(grp + 1) * 512], start=(k == 0), stop=(k == 7)),
                                         reads=[wlk, "u2T6"], writes=["ps%d" % pb])
                                B = {nm: (ebuf[nm][n_ % 2], "%s%d" % (nm, n_ % 2)) for nm in ebuf}
                                P.op("dve", lambda: nc.vector.tensor_scalar(out=B["gg"][0][:], in0=psb[pa][:, :], scalar1=b1s[:, e, i:i + 1], scalar2=7.0, op0=ALU.add, op1=ALU.min),
                                     reads=["ps%d" % pa, "b1s"], writes=[B["gg"][1]])
                                P.op("act", lambda: nc.scalar.activation(out=B["t1"][0][:], in_=B["gg"][0][:], func=AF.Gelu_apprx_sigmoid), reads=[B["gg"][1]], writes=[B["t1"][1]])
                                P.op("act", lambda: nc.scalar.activation(out=B["ll"][0][:], in_=psb[pb][:, :], func=AF.Relu, bias=b17[:, e, i:i + 1], scale=1.0),
                                     reads=["ps%d" % pb, "b17"], writes=[B["ll"][1]])
                                P.op("dve", lambda: nc.vector.tensor_scalar(out=B["t2"][0][:], in0=B["ll"][0][:], scalar1=14.0, scalar2=-6.0, op0=ALU.min, op1=ALU.add),
                                     reads=[B["ll"][1]], writes=[B["t2"][1]])
                                P.op("dve", lambda: nc.vector.tensor_tensor(out=actT[par][grp][:, i, :], in0=B["t1"][0][:], in1=B["t2"][0][:], op=ALU.mult),
                                     reads=[B["t1"][1], B["t2"][1]], writes=["actT%d%d" % (par, grp)])

                def w_loads(e):
                    for n in range(2):
                        wi = (e % 2) * 2 + n
                        P.op("pool", lambda: nc.gpsimd.dma_start(out=w2b[wi][:], in_=w2[e, :, n * 512:(n + 1) * 512].rearrange("(k p) n -> p k n", p=128)),
                             writes=["w2b%d" % wi], dma="w2b%d" % wi)

                def w_phase(e):
                    par = e % 2
                    for n in range(2):
                        for tl in range(PT // 128):
                            grp, off = tl // 4, (tl % 4) * 128
                            pk = 5 + (tl % 2)
                            wi = (e % 2) * 2 + n
                            for k in range(8):
                                P.op("pe", lambda: nc.tensor.matmul(psb[pk][:, :], lhsT=actT[par][grp][:, k, off:off + 128], rhs=w2b[wi][:, k, :], start=(k == 0), stop=(k == 7)),
                                     reads=["actT%d%d" % (par, grp), "w2b%d" % wi], writes=["ps%d" % pk])
                            P.op("dve", lambda: nc.vector.scalar_tensor_tensor(out=yacc[:, tl, n * 512:(n + 1) * 512], in0=psb[pk][:, :], scalar=gwtm[:, tl, e:e + 1],
                                                                               in1=yacc[:, tl, n * 512:(n + 1) * 512], op0=ALU.mult, op1=ALU.add),
                                 reads=["yacc", "ps%d" % pk, "gwtm"], writes=["yacc"])

                for ps_ in range(NPASS):
                    tb = ps_ * PT
                    s_ = tb // SEQ
                    P.op("sp", lambda: nc.sync.dma_start(out=u2T[:], in_=u2T_d[:, tb:tb + PT].rearrange("(k p) t -> p k t", p=128)), reads=["u2T_d"], writes=["u2T6"], dma="u2T6")
                    P.op("sp", lambda: nc.sync.dma_start(out=gwT6[:], in_=gw_d[:, tb:tb + PT]), reads=["gw_d"], writes=["gwT6"], dma="gwT6")
                    P.op("sp", lambda: nc.sync.dma_start(out=gwtm[:], in_=gwtm_d[tb:tb + PT, :].rearrange("(t p) e -> p t e", p=128)), reads=["gwtm_d"], writes=["gwtm"], dma="gwtm")
                    if tb % SEQ == 0:
                        P.op("sp", lambda: nc.sync.dma_start(out=g2b[:], in_=bcast_rows(mod_d[s_, 5 * D:6 * D], D)), reads=["mod_d"], writes=["g2b"], dma="g2b")
                    for tl in range(PT // 128):
                        for n in range(2):
                            pk = 5 + (n % 2)
                            P.op("pe", lambda: nc.tensor.matmul(psb[pk][:, :], lhsT=gwT6[:, tl * 128:(tl + 1) * 128], rhs=b2s[:, n * 512:(n + 1) * 512], start=True, stop=True),
                                 reads=["gwT6", "b2s"], writes=["ps%d" % pk])
                            P.op("act", lambda: nc.scalar.copy(out=yacc[:, tl, n * 512:(n + 1) * 512], in_=psb[pk][:, :]), reads=["ps%d" % pk], writes=["yacc"])
                    jb = ps_ * NE * 2
                    h_phase(ps_, 0, jb)
                    w_loads(0)
                    for e in range(NE):
                        if e + 1 < NE:
                            w_loads(e + 1)
                        if e + 1 < NE:
                            h_phase(ps_, e + 1, jb + (e + 1) * 2)
                        w_phase(e)
                    for tl in range(PT // 128):
                        r0 = tb + tl * 128
                        xb_, xk = x16[tl % 2], "x16%d" % (tl % 2)
                        P.op("sp", lambda: nc.sync.dma_start(out=xb_[:], in_=x1_d[r0:r0 + 128, :]), reads=["x1_d"], writes=[xk], dma=xk)
                        P.op("dve", lambda: nc.vector.tensor_tensor(out=yacc[:, tl, :], in0=yacc[:, tl, :], in1=g2b[:], op=ALU.mult), reads=["yacc", "g2b"], writes=["yacc"])
                        P.op("dve", lambda: nc.vector.tensor_tensor(out=xb_[:], in0=yacc[:, tl, :], in1=xb_[:], op=ALU.add), reads=["yacc", xk], writes=[xk])
                        P.op("sp", lambda: nc.sync.dma_start(out=out_d[r0:r0 + 128, :], in_=xb_[:]), reads=[xk], writes=["out"], dma=xk)
                P.barrier()

        if "copyout" in stages:
            with nc.sbuf_tensor("cpy", [128, D], F32) as cpy:
                P.op("sp", lambda: nc.sync.dma_start(out=cpy[:], in_=x_d[0:128, :]), writes=["cpy"], dma="cpy")
                P.op("sp", lambda: nc.sync.dma_start(out=out_d[0:128, :], in_=cpy[:]), reads=["cpy"], writes=["out"], dma="cpy")
                P.drain("sp", outs_to_drain)
        for name in dbg:
            if name in ("qkv", "z", "dt", "gl", "xbcT"):
                pass
        P.drain("sp", outs_to_drain)
    return nc, P, dbg_outs


def make_consts():
    ident = np.eye(128, dtype=np.float32)
    rev = ident[::-1].copy()
    tri = np.triu(np.ones((128, 128), np.float32))
    negtri = np.where(np.arange(128)[None, :] >= np.arange(128)[:, None], 0.0, NEG).astype(np.float32)
    i = np.arange(2304)
    d = i - 127
    dd = np.maximum(d, 1).astype(np.float32)
    large = 16 + (np.log(dd / 16.0) / math.log(1024 / 16.0) * 16).astype(np.int32)
    large = np.minimum(large, 31)
    bucket = np.where(d < 16, np.maximum(d, 0), large)
    oh = np.zeros((33, 2304), np.float32)
    oh[bucket, i] = 1.0
    oh[:, d < 0] = 0.0
    oh[32, d < 0] = 1.0
    sel = np.zeros((128, 3, 16, 8), np.float32)
    for t in range(16):
        qb = t // 2
        for n in range(8):
            if n >= qb:
                sel[:, 0, t, n] = -1e30
            else:
                sel[:, 1, t, n] = 1.0
            if n == qb:
                sel[:, 2, t, n] = 1.0
    kind = (np.arange(SEQ)[None, :] // 256 == np.arange(8)[:, None]).astype(np.float32)
    return dict(c_ident=ident, c_rev=rev, c_tri=tri, c_negtri=negtri, c_bucket=oh, c_selmask=sel, c_kind=kind)


def make_in_maps(inputs):
    f = lambda a: np.ascontiguousarray(np.asarray(a, dtype=np.float32))
    consts = make_consts()
    x = f(inputs["x"]).reshape(NCORES, T, D)
    c = f(inputs["c"]).reshape(NCORES, NSEQ, D)
    rel = np.concatenate([f(inputs["rel_bias_table"]), np.full((1, H), NEG, np.float32)], 0)
    b1 = f(inputs["expert_b1"])[0]
    shared = dict(
        ada_w=f(inputs["ada_w"])[0], ada_b=f(inputs["ada_b"])[0], norm1_g=f(inputs["norm1_g"])[0], w_in=f(inputs["w_in"])[0],
        q_norm_g=f(inputs["q_norm_g"])[0], k_norm_g=f(inputs["k_norm_g"])[0], rel_tab=rel,
        conv_wT=f(f(inputs["conv_w"])[0].T), conv_b=f(f(inputs["conv_b"])[0].reshape(24, 128).T), dt_bias=f(inputs["dt_bias"])[0],
        a_log=f(inputs["a_log"])[0], d_skip=f(inputs["d_skip"])[0], ssm_norm_g=f(inputs["ssm_norm_g"])[0],
        w_attn=f(inputs["w_attn_branch"])[0], w_ssm=f(inputs["w_ssm_branch"])[0], gate_bias=f(inputs["gate_bias"])[0],
        w_out=f(inputs["w_out"])[0], norm2_g=f(inputs["norm2_g"])[0], router_w=f(inputs["router_w"])[0],
        router_b=f(inputs["router_b"])[0], w1=f(inputs["expert_w1"])[0],
        b1T=f(b1.reshape(NE, 16, 128).transpose(2, 0, 1)), w2=f(inputs["expert_w2"])[0], b2=f(inputs["expert_b2"])[0],
    )
    shared.update(consts)
    maps = []
    for i in range(NCORES):
        m = dict(shared)
        m["x"] = x[i]
        m["cT"] = f(c[i].T)
        maps.append(m)
    return maps


def kernel(**inputs):
    nc, P, _ = build_program()
    in_maps = make_in_maps(inputs)
    res = run_bass_kernel_spmd(nc, in_maps, core_ids=list(range(NCORES)))
    out = np.stack([np.asarray(r["out"], dtype=np.float32) for r in res.results], 0)
    return out.reshape(16, SEQ, D)
```

```python
import contextlib
import os
import math
import numpy as np
import concourse.bass as bass
import concourse.mybir as mybir
from concourse.bass_utils import run_bass_kernel_spmd

F32 = mybir.dt.float32
BF16 = mybir.dt.bfloat16
AF = mybir.ActivationFunctionType
ALU = mybir.AluOpType
AX = mybir.AxisListType

NCORES = 8
D = 1024
SEQ = 2048
NSEQ = 2
T = NSEQ * SEQ
NT = T // 128
H = 16
HD = 64
SSM_INNER = 2048
SSM_H = 32
SSM_G = 4
SSM_N = 128
CONV_DIM = 3072
NE = 32
FF = 1024
EPS = 1e-6
IN_PROJ = 10272
OFF_Q, OFF_K, OFF_V, OFF_Z, OFF_XBC, OFF_DT, OFF_GATE = 0, 1024, 2048, 3072, 5120, 8192, 8224
NEG = -30000.0


class Prog:
    def __init__(self, nc, es):
        self.nc, self.es = nc, es
        self.eng = {"pe": nc.tensor, "act": nc.scalar, "dve": nc.vector, "pool": nc.gpsimd, "sp": nc.sync}
        self.sems, self.val = {}, {}
        self.seen = {e: {} for e in self.eng}
        self.lastw, self.readers = {}, {}
        self.nins = 0
        self.free = []
        self.free_sw = []
        self.swkeys = set()
        self.nsem = 0

    def sem(self, key, sw=False):
        if key not in self.sems:
            fl = self.free_sw if sw else self.free
            if sw:
                self.swkeys.add(key)
            if fl:
                h, v = fl.pop()
                self.sems[key] = h
                self.val[key] = v
                for e in self.seen:
                    self.seen[e][key] = v
            else:
                self.nsem += 1
                self.sems[key] = self.es.enter_context(self.nc.semaphore("s%d" % self.nsem))
                self.val[key] = 0
        return self.sems[key]

    def sb(self, name, shape, dt=F32):
        return self.es.enter_context(self.nc.sbuf_tensor(name, list(shape), dt))

    def ps(self, name, shape, dt=F32):
        return self.es.enter_context(self.nc.psum_tensor(name, list(shape), dt))

    def op(self, eng, fn, reads=(), writes=(), dma=None):
        deps = {}
        for b in reads:
            for k, v in self.lastw.get(b, {}).items():
                deps[k] = max(deps.get(k, 0), v)
            if b.startswith("ps"):
                for k, v in self.readers.get(b, {}).items():
                    if k != eng:
                        deps[k] = max(deps.get(k, 0), v)
        for b in writes:
            for k, v in self.lastw.get(b, {}).items():
                deps[k] = max(deps.get(k, 0), v)
            for k, v in self.readers.get(b, {}).items():
                deps[k] = max(deps.get(k, 0), v)
        e = self.eng[eng]
        for k, v in deps.items():
            if dma is None and k == eng and eng == "pe":
                continue
            if self.seen[eng].get(k, 0) >= v:
                continue
            e.wait_ge(self.sems[k], v)
            self.seen[eng][k] = v
        ins = fn()
        if dma is None:
            key, inc = eng, 1
        else:
            key, inc = "dma:" + dma, 16
        s = self.sem(key, sw=(dma is not None and eng == "pool"))
        self.val[key] += inc
        ins.then_inc(s, inc)
        v = self.val[key]
        for b in writes:
            if dma is None:
                self.lastw[b] = {key: v}
            else:
                d_ = {k_: v_ for k_, v_ in self.lastw.get(b, {}).items() if k_.startswith("dma:")}
                d_[key] = v
                self.lastw[b] = d_
            self.readers[b] = {}
        for b in reads:
            self.readers.setdefault(b, {})[key] = v
        self.nins += 1
        return ins

    def barrier(self):
        for eng, e in self.eng.items():
            for k, v in self.val.items():
                if v > 0 and self.seen[eng].get(k, 0) < v:
                    e.wait_ge(self.sems[k], v)
                    self.seen[eng][k] = v
        for k in [k for k in self.sems if k.startswith("dma:")]:
            (self.free_sw if k in self.swkeys else self.free).append((self.sems.pop(k), self.val.pop(k)))
            self.swkeys.discard(k)
            for eng in self.seen:
                self.seen[eng].pop(k, None)
        self.lastw, self.readers = {}, {}

    def drain(self, eng, bufs):
        e = self.eng[eng]
        for b in bufs:
            for k, v in self.lastw.get(b, {}).items():
                if self.seen[eng].get(k, 0) < v:
                    e.wait_ge(self.sems[k], v)
                    self.seen[eng][k] = v


def bcast_rows(ap1d, n):
    return bass.AP(ap1d.tensor, ap1d.offset, [[0, 128], [1, n]])


def build_program(stages=("s0", "s1", "s3", "s4", "s5", "s6"), dbg=()):
    nc = bass.Bass("TRN2", target_bir_lowering=False)
    es = contextlib.ExitStack()

    def din(name, shape, dt=F32):
        return nc.dram_tensor(name, list(shape), dt, kind="ExternalInput").ap()

    def dscr(name, shape, dt=F32):
        if name in dbg:
            outs_to_drain.append(name)
            return nc.dram_tensor(name, list(shape), dt, kind="ExternalOutput").ap()
        return nc.dram_tensor(name, list(shape), dt, kind="Internal").ap()

    def dout(name, shape, dt=F32):
        return nc.dram_tensor(name, list(shape), dt, kind="ExternalOutput").ap()

    outs_to_drain = ["out"]
    x_d = din("x", [T, D])
    cT_d = din("cT", [D, NSEQ])
    ada_w = din("ada_w", [D, 6 * D])
    ada_b = din("ada_b", [6 * D])
    norm1_g = din("norm1_g", [D])
    w_in = din("w_in", [D, IN_PROJ])
    q_norm_g = din("q_norm_g", [HD])
    k_norm_g = din("k_norm_g", [HD])
    rel_tab = din("rel_tab", [33, H])
    conv_wT = din("conv_wT", [CONV_DIM, 4])
    conv_b = din("conv_b", [128, 24])
    dt_bias = din("dt_bias", [SSM_H])
    a_log = din("a_log", [SSM_H])
    d_skip = din("d_skip", [SSM_H])
    ssm_norm_g = din("ssm_norm_g", [SSM_INNER])
    w_attn = din("w_attn", [D, D])
    w_ssm = din("w_ssm", [SSM_INNER, D])
    gate_bias = din("gate_bias", [2 * D])
    w_out = din("w_out", [D, D])
    norm2_g = din("norm2_g", [D])
    router_w = din("router_w", [D, NE])
    router_b = din("router_b", [NE])
    w1 = din("w1", [NE, D, 2 * FF])
    b1T = din("b1T", [128, NE, 16])
    w2 = din("w2", [NE, FF, D])
    b2 = din("b2", [NE, D])
    c_ident = din("c_ident", [128, 128])
    c_rev = din("c_rev", [128, 128])
    c_tri = din("c_tri", [128, 128])
    c_negtri = din("c_negtri", [128, 128])
    c_bucket = din("c_bucket", [33, 2304])
    c_kind = din("c_kind", [8, SEQ])
    c_selmask = din("c_selmask", [128, 3, 16, 8])
    out_d = dout("out", [T, D])

    P = Prog(nc, es)
    with es:
        mod_d = dscr("mod_d", [NSEQ, 6 * D])
        qkv_d = dscr("qkv_d", [T, 3 * D])
        z_d = dscr("z_d", [T, SSM_INNER])
        dt_d = dscr("dt_d", [T, SSM_H])
        gl_d = dscr("gl_d", [T, 2 * D])
        xbcT_d = dscr("xbcT_d", [CONV_DIM, T])
        expf_d = dscr("expf_d", [H, 2304], BF16)
        wnat_d = dscr("wnat_d", [H, 128, SEQ], BF16)
        attnT_d = dscr("attnT_d", [D, T], BF16)
        ssmT_d = dscr("ssmT_d", [SSM_INNER, T], BF16)
        x1_d = dscr("x1_d", [T, D])
        u2T_d = dscr("u2T_d", [D, T], BF16)
        gw_d = dscr("gw_d", [NE, T])
        gwtm_d = dscr("gwtm_d", [T, NE])

        ident_f = P.sb("ident_f", [128, 128])
        rev_f = P.sb("rev_f", [128, 128])
        ident_b = P.sb("ident_b", [128, 128], BF16)
        rev_b = P.sb("rev_b", [128, 128], BF16)
        ones_b = P.sb("ones_b", [128, 128], BF16)
        ones_f = P.sb("ones_f", [128, 128])
        P.op("sp", lambda: nc.sync.dma_start(out=ident_f[:], in_=c_ident[:, :]), writes=["ident_f"], dma="ident_f")
        P.op("sp", lambda: nc.sync.dma_start(out=rev_f[:], in_=c_rev[:, :]), writes=["rev_f"], dma="rev_f")
        P.op("pool", lambda: nc.gpsimd.dma_start(out=ident_b[:], in_=c_ident[:, :]), writes=["ident_b"], dma="ident_b")
        P.op("pool", lambda: nc.gpsimd.dma_start(out=rev_b[:], in_=c_rev[:, :]), writes=["rev_b"], dma="rev_b")
        P.op("dve", lambda: nc.vector.memset(ones_b[:], 1.0), writes=["ones_b"])
        P.op("dve", lambda: nc.vector.memset(ones_f[:], 1.0), writes=["ones_f"])

        psb = [P.ps("psb%d" % i, [128, 512]) for i in range(8)]

        dbg_outs = {}

        def tap(name, shape, dt=F32):
            dbg_outs[name] = dout("dbg_" + name, shape, dt)
            outs_to_drain.append("dbg_" + name)
            return dbg_outs[name]

        if "s0" in stages:
            with contextlib.ExitStack() as st:
                sb = lambda n, s, d=F32: st.enter_context(nc.sbuf_tensor(n, list(s), d))
                cact = sb("cact", [128, 8, NSEQ])
                adab = sb("adab", [NSEQ, 6 * D])
                modsb = sb("modsb", [NSEQ, 6 * D])
                wb = [sb("s0w%d" % i, [128, 8, 512]) for i in range(2)]
                P.op("sp", lambda: nc.sync.dma_start(out=cact[:], in_=cT_d.rearrange("(k p) b -> p k b", p=128)),
                     writes=["cact"], dma="cact")
                P.op("sp", lambda: nc.sync.dma_start(out=adab[:], in_=bass.AP(ada_b.tensor, 0, [[0, NSEQ], [1, 6 * D]])),
                     writes=["adab"], dma="adab")
                P.op("act", lambda: nc.scalar.activation(out=cact[:], in_=cact[:], func=AF.Silu), reads=["cact"], writes=["cact"])
                for j in range(12):
                    w = wb[j % 2]
                    wk = "s0w%d" % (j % 2)
                    P.op("sp", lambda: nc.sync.dma_start(out=w[:], in_=ada_w[:, j * 512:(j + 1) * 512].rearrange("(k p) n -> p k n", p=128)),
                         writes=[wk], dma=wk)
                    pk = "ps%d" % (j % 2)
                    for k in range(8):
                        P.op("pe", lambda: nc.tensor.matmul(psb[j % 2][0:NSEQ, :], lhsT=cact[:, k, :], rhs=w[:, k, :], start=(k == 0), stop=(k == 7)),
                             reads=["cact", wk], writes=[pk])
                    P.op("dve", lambda: nc.vector.tensor_tensor(out=modsb[:, j * 512:(j + 1) * 512], in0=psb[j % 2][0:NSEQ, :],
                                                                in1=adab[:, j * 512:(j + 1) * 512], op=ALU.add),
                         reads=[pk, "adab"], writes=["modsb"])
                P.op("sp", lambda: nc.sync.dma_start(out=mod_d[:, :], in_=modsb[:]), reads=["modsb"], writes=["mod_d"], dma="modsb")
                if "mod" in dbg:
                    t_ = tap("mod", [NSEQ, 6 * D])
                    P.op("sp", lambda: nc.sync.dma_start(out=t_[:, :], in_=modsb[:]), reads=["modsb"], writes=["dbg_mod"], dma="modsb")
                tab = sb("tab", [33, H])
                oh = sb("oh", [33, 2304])
                ef = sb("ef", [H, 2304], BF16)
                P.op("sp", lambda: nc.sync.dma_start(out=tab[:], in_=rel_tab[:, :]), writes=["tab"], dma="tab")
                P.op("sp", lambda: nc.sync.dma_start(out=oh[:], in_=c_bucket[:, :]), writes=["oh"], dma="oh")
                for j in range(5):
                    n = 512 if j < 4 else 256
                    pk = "ps%d" % (2 + j % 2)
                    P.op("pe", lambda: nc.tensor.matmul(psb[2 + j % 2][0:H, 0:n], lhsT=tab[:], rhs=oh[:, j * 512:j * 512 + n], start=True, stop=True),
                         reads=["tab", "oh"], writes=[pk])
                    P.op("act", lambda: nc.scalar.activation(out=ef[:, j * 512:j * 512 + n], in_=psb[2 + j % 2][0:H, 0:n], func=AF.Exp),
                         reads=[pk], writes=["ef"])
                P.op("sp", lambda: nc.sync.dma_start(out=expf_d[:, :], in_=ef[:]), reads=["ef"], writes=["expf_d"], dma="ef")
                wrv = [sb("wrv%d" % i, [128, SEQ], BF16) for i in range(2)]
                wnt = [sb("wnt%d" % i, [128, SEQ], BF16) for i in range(2)]
                for h in range(H):
                    wr_, wrk = wrv[h % 2], "wrv%d" % (h % 2)
                    wn_, wnk = wnt[h % 2], "wnt%d" % (h % 2)
                    P.op("sp", lambda: nc.sync.dma_start(out=wr_[:], in_=bass.AP(expf_d.tensor, h * 2304, [[1, 128], [1, SEQ]])), reads=["expf_d"], writes=[wrk], dma=wrk)
                    for c in range(4):
                        pk = "ps%d" % (4 + c)
                        P.op("pe", lambda: nc.tensor.matmul(psb[4 + c][:, :], lhsT=rev_b[:], rhs=wr_[:, c * 512:(c + 1) * 512], start=True, stop=True), reads=["rev_b", wrk], writes=[pk])
                        if c % 2 == 0:
                            P.op("act", lambda: nc.scalar.copy(out=wn_[:, c * 512:(c + 1) * 512], in_=psb[4 + c][:, :]), reads=[pk], writes=[wnk])
                        else:
                            P.op("dve", lambda: nc.vector.tensor_copy(out=wn_[:, c * 512:(c + 1) * 512], in_=psb[4 + c][:, :]), reads=[pk], writes=[wnk])
                    P.op("sp", lambda: nc.sync.dma_start(out=wnat_d[h, :, :], in_=wn_[:]), reads=[wnk], writes=["wnat_d"], dma=wnk)
                P.barrier()

        GT = 1024
        NG = T // GT
        if "s1" in stages:
            with contextlib.ExitStack() as st:
                sb = lambda n, s, d=F32: st.enter_context(nc.sbuf_tensor(n, list(s), d))
                g1bc = sb("g1bc", [128, D])
                A1 = sb("A1", [128, D])
                sh1 = sb("sh1", [128, D])
                xt = [sb("xt%d" % i, [128, D]) for i in range(2)]
                ut = [sb("ut%d" % i, [128, D]) for i in range(2)]
                sq = sb("sq", [128, D])
                ss = sb("ss", [128, 2])
                uT = sb("uT", [128, 8, GT], BF16)
                wbuf = [sb("wbuf%d" % i, [128, 8, 512], BF16) for i in range(2)]
                stg = [sb("stg%d" % i, [128, 512]) for i in range(2)]
                raw = [sb("raw%d" % i, [128, GT + 3]) for i in range(2)]
                cacc = [sb("cacc%d" % i, [128, GT]) for i in range(2)]
                carry = sb("carry", [128, 24, 3])
                cw = sb("cw", [128, 24, 4])
                cb = sb("cb", [128, 24])
                P.op("sp", lambda: nc.sync.dma_start(out=g1bc[:], in_=bcast_rows(norm1_g, D)), writes=["g1bc"], dma="g1bc")
                P.op("sp", lambda: nc.sync.dma_start(out=cw[:], in_=conv_wT.rearrange("(m p) k -> p m k", p=128)), writes=["cw"], dma="cw")
                P.op("sp", lambda: nc.sync.dma_start(out=cb[:], in_=conv_b[:, :]), writes=["cb"], dma="cb")
                chunks = []
                for j in range(6):
                    chunks.append((OFF_Q + 512 * j, 512, "tm", qkv_d, 512 * j))
                for j in range(4):
                    chunks.append((OFF_Z + 512 * j, 512, "tm", z_d, 512 * j))
                for j in range(6):
                    chunks.append((OFF_XBC + 512 * j, 512, "fm", None, j))
                chunks.append((OFF_DT, 32, "tm", dt_d, 0))
                for j in range(4):
                    chunks.append((OFF_GATE + 512 * j, 512, "tm", gl_d, 512 * j))
                wcount = 0
                scount = 0
                rcount = 0
                for g in range(NG):
                    s = (g * GT) // SEQ
                    if (g * GT) % SEQ == 0:
                        P.op("sp", lambda: nc.sync.dma_start(out=A1[:], in_=bcast_rows(mod_d[s, D:2 * D], D)), reads=["mod_d"], writes=["A1"], dma="A1")
                        P.op("sp", lambda: nc.sync.dma_start(out=sh1[:], in_=bcast_rows(mod_d[s, 0:D], D)), reads=["mod_d"], writes=["sh1"], dma="sh1")
                        P.op("dve", lambda: nc.vector.scalar_tensor_tensor(out=A1[:], in0=A1[:], scalar=1.0, in1=g1bc[:], op0=ALU.add, op1=ALU.mult),
                             reads=["A1", "g1bc"], writes=["A1"])
                        P.op("dve", lambda: nc.vector.memset(carry[:], 0.0), writes=["carry"])
                    for tt in range(GT // 128):
                        t = g * (GT // 128) + tt
                        xb_, xk = xt[t % 2], "xt%d" % (t % 2)
                        ub_, uk = ut[t % 2], "ut%d" % (t % 2)
                        P.op("sp", lambda: nc.sync.dma_start(out=xb_[:], in_=x_d[t * 128:(t + 1) * 128, :]), writes=[xk], dma=xk)
                        P.op("act", lambda: nc.scalar.activation(out=sq[:], in_=xb_[:], func=AF.Square, scale=1.0 / 32.0, accum_out=ss[:, 0:1]),
                             reads=[xk], writes=["sq", "ss"])
                        P.op("dve", lambda: nc.vector.tensor_scalar_add(out=ss[:, 1:2], in0=ss[:, 0:1], scalar1=EPS), reads=["ss"], writes=["ss"])
                        P.op("act", lambda: nc.scalar.sqrt(out=ss[:, 1:2], in_=ss[:, 1:2]), reads=["ss"], writes=["ss"])
                        P.op("dve", lambda: nc.vector.reciprocal(out=ss[:, 1:2], in_=ss[:, 1:2]), reads=["ss"], writes=["ss"])
                        P.op("dve", lambda: nc.vector.scalar_tensor_tensor(out=ub_[:], in0=xb_[:], scalar=ss[:, 1:2], in1=A1[:], op0=ALU.mult, op1=ALU.mult),
                             reads=[xk, "ss", "A1"], writes=[uk])
                        P.op("dve", lambda: nc.vector.tensor_tensor(out=ub_[:], in0=ub_[:], in1=sh1[:], op=ALU.add), reads=[uk, "sh1"], writes=[uk])
                        if "u" in dbg and t < 2:
                            if "u" not in dbg_outs:
                                tap("u", [256, D])
                            P.op("sp", lambda: nc.sync.dma_start(out=dbg_outs["u"][t * 128:(t + 1) * 128, :], in_=ub_[:]), reads=[uk], writes=["dbg_u"], dma=uk)
                        for half in range(2):
                            pb, pk = psb[half], "ps%d" % half
                            for kk in range(4):
                                k = half * 4 + kk
                                P.op("pe", lambda: nc.tensor.matmul(pb[:, kk * 128:(kk + 1) * 128], lhsT=ub_[:, k * 128:(k + 1) * 128], rhs=ident_f[:],
                                                                    start=True, stop=True), reads=[uk, "ident_f"], writes=[pk])
                            P.op("act", lambda: nc.scalar.copy(out=uT[:, half * 4:half * 4 + 4, tt * 128:(tt + 1) * 128],
                                                               in_=pb[:].rearrange("p (k t) -> p k t", k=4)), reads=[pk], writes=["uT"])
                    for (c0, ncol, kind, dest, dcol) in chunks:
                        wb_, wk = wbuf[wcount % 2], "wbuf%d" % (wcount % 2)
                        wcount += 1
                        P.op("pool", lambda: nc.gpsimd.dma_start(out=wb_[:, :, 0:ncol], in_=w_in[:, c0:c0 + ncol].rearrange("(k p) n -> p k n", p=128)),
                             writes=[wk], dma=wk)
                        if kind == "tm":
                            for tt in range(GT // 128):
                                t = g * (GT // 128) + tt
                                pi = 2 + (scount % 4)
                                pb, pk = psb[pi], "ps%d" % pi
                                sg, sk = stg[scount % 2], "stg%d" % (scount % 2)
                                scount += 1
                                for k in range(8):
                                    P.op("pe", lambda: nc.tensor.matmul(pb[:, 0:ncol], lhsT=uT[:, k, tt * 128:(tt + 1) * 128], rhs=wb_[:, k, 0:ncol],
                                                                        start=(k == 0), stop=(k == 7)), reads=["uT", wk], writes=[pk])
                                ev = "act" if scount % 2 else "dve"
                                if ev == "act":
                                    P.op("act", lambda: nc.scalar.copy(out=sg[:, 0:ncol], in_=pb[:, 0:ncol]), reads=[pk], writes=[sk])
                                else:
                                    P.op("dve", lambda: nc.vector.tensor_copy(out=sg[:, 0:ncol], in_=pb[:, 0:ncol]), reads=[pk], writes=[sk])
                                P.op("sp", lambda: nc.sync.dma_start(out=dest[t * 128:(t + 1) * 128, dcol:dcol + ncol], in_=sg[:, 0:ncol]),
                                     reads=[sk], writes=[dest.tensor.name], dma=sk)
                        else:
                            for mm in range(4):
                                m = dcol * 4 + mm
                                rw, rk = raw[rcount % 2], "raw%d" % (rcount % 2)
                                ca, ck = cacc[rcount % 2], "cacc%d" % (rcount % 2)
                                rcount += 1
                                P.op("dve", lambda: nc.vector.tensor_copy(out=rw[:, 0:3], in_=carry[:, m, :]), reads=["carry"], writes=[rk])
                                for hf in range(GT // 512):
                                    pi = 6 + (hf % 2)
                                    pb, pk = psb[pi], "ps%d" % pi
                                    for k in range(8):
                                        P.op("pe", lambda: nc.tensor.matmul(pb[:, :], lhsT=wb_[:, k, mm * 128:(mm + 1) * 128], rhs=uT[:, k, hf * 512:(hf + 1) * 512],
                                                                            start=(k == 0), stop=(k == 7)), reads=["uT", wk], writes=[pk])
                                    P.op("act", lambda: nc.scalar.copy(out=rw[:, 3 + hf * 512:3 + (hf + 1) * 512], in_=pb[:, :]), reads=[pk], writes=[rk])
                                P.op("dve", lambda: nc.vector.tensor_copy(out=carry[:, m, :], in_=rw[:, GT:GT + 3]), reads=[rk], writes=["carry"])
                                P.op("dve", lambda: nc.vector.tensor_scalar(out=ca[:], in0=rw[:, 3:GT + 3], scalar1=cw[:, m, 3:4], scalar2=cb[:, m:m + 1],
                                                                            op0=ALU.mult, op1=ALU.add), reads=[rk, "cw", "cb"], writes=[ck])
                                for tap_ in range(3):
                                    P.op("dve", lambda: nc.vector.scalar_tensor_tensor(out=ca[:], in0=rw[:, tap_:GT + tap_], scalar=cw[:, m, tap_:tap_ + 1], in1=ca[:],
                                                                                       op0=ALU.mult, op1=ALU.add), reads=[rk, ck, "cw"], writes=[ck])
                                P.op("act", lambda: nc.scalar.activation(out=ca[:], in_=ca[:], func=AF.Silu), reads=[ck], writes=[ck])
                                P.op("sp", lambda: nc.sync.dma_start(out=xbcT_d[m * 128:(m + 1) * 128, g * GT:(g + 1) * GT], in_=ca[:]),
                                     reads=[ck], writes=["xbcT_d"], dma=ck)
                P.barrier()

        if "s3" in stages:
            with contextlib.ExitStack() as st:
                sb = lambda n, s, d=F32: st.enter_context(nc.sbuf_tensor(n, list(s), d))
                gq = sb("gq", [128, HD]); gk = sb("gk", [128, HD])
                selm = sb("selm", [128, 3, 16, 8])
                qf = [sb("qf%d" % i, [128, 16, HD]) for i in range(2)]
                kf = [sb("kf%d" % i, [128, 16, HD]) for i in range(2)]
                vf = [sb("vf%d" % i, [128, 16, HD]) for i in range(2)]
                sqt = sb("sqt", [128, 16, HD])
                nrm = sb("nrm", [128, 2, 16])
                qTg = sb("qTg", [64, SEQ], BF16)
                kmT = sb("kmT", [64, 8]); kmb = sb("kmb", [64, 8], BF16)
                gate = sb("gate", [128, 16, 8]); cmpb = sb("cmpb", [128, 16, 8, 8]); rank = sb("rank", [128, 16, 8])
                qaug = sb("qaug", [128, 16, 72], BF16); kb16 = sb("kb16", [128, 16, HD], BF16)
                qTa = [sb("qTa%d" % i, [72, SEQ], BF16) for i in range(2)]
                kTa = [sb("kTa%d" % i, [72, SEQ], BF16) for i in range(2)]
                vaug = [sb("vaug%d" % i, [128, 16, 128], BF16) for i in range(2)]
                Wt = [sb("Wt%d" % i, [128, SEQ], BF16) for i in range(2)]
                pS = [sb("pS%d" % i, [128, 512], BF16) for i in range(3)]
                pW = [sb("pW%d" % i, [128, 512], BF16) for i in range(3)]
                rec = sb("rec", [128, SEQ])
                aT = [sb("aT%d" % i, [64, SEQ], BF16) for i in range(2)]
                P.op("sp", lambda: nc.sync.dma_start(out=gq[:], in_=bcast_rows(q_norm_g, HD)), writes=["gq"], dma="gq")
                P.op("sp", lambda: nc.sync.dma_start(out=gk[:], in_=bcast_rows(k_norm_g, HD)), writes=["gk"], dma="gk")
                P.op("sp", lambda: nc.sync.dma_start(out=selm[:], in_=c_selmask[:, :, :, :]), writes=["selm"], dma="selm")
                for i in range(2):
                    P.op("dve", lambda: nc.vector.memset(vaug[i][:], 1.0), writes=["vaug%d" % i])
                    P.op("pool", lambda: nc.gpsimd.dma_start(out=kTa[i][64:72, :], in_=c_kind[:, :]), writes=["kTa%d" % i], dma="kTa%d" % i)

                def prologue(idx):
                    s_, h = divmod(idx, H)
                    b2_ = idx % 2
                    r0 = s_ * SEQ
                    q_, k_, v_ = qf[b2_], kf[b2_], vf[b2_]
                    qk_, kk_, vk_ = "qf%d" % b2_, "kf%d" % b2_, "vf%d" % b2_
                    qTak, kTak, vak = "qTa%d" % b2_, "kTa%d" % b2_, "vaug%d" % b2_
                    for (buf, nm, off) in [(qf, "qf", OFF_Q), (kf, "kf", OFF_K), (vf, "vf", OFF_V)]:
                        P.op("sp", lambda: nc.sync.dma_start(out=buf[b2_][:], in_=qkv_d[r0:r0 + SEQ, off + h * HD:off + (h + 1) * HD].rearrange("(t p) d -> p t d", p=128)),
                             reads=["qkv_d"], writes=["%s%d" % (nm, b2_)], dma="%s%d" % (nm, b2_))
                    P.op("sp", lambda: nc.sync.dma_start(out=Wt[b2_][:], in_=wnat_d[h, :, :]), reads=["wnat_d"], writes=["Wt%d" % b2_], dma="Wt%d" % b2_)
                    P.op("act", lambda: nc.scalar.copy(out=vaug[b2_][:, :, 0:64], in_=v_[:]), reads=[vk_], writes=[vak])
                    for j, (t_, tk_, g_, gk_) in enumerate([(q_, qk_, gq, "gq"), (k_, kk_, gk, "gk")]):
                        P.op("act", lambda: nc.scalar.activation(out=sqt[:], in_=t_[:], func=AF.Square, scale=0.125), reads=[tk_], writes=["sqt"])
                        P.op("dve", lambda: nc.vector.reduce_sum(out=nrm[:, j, :], in_=sqt[:], axis=AX.X), reads=["sqt"], writes=["nrm"])
                        P.op("dve", lambda: nc.vector.tensor_scalar_add(out=nrm[:, j, :], in0=nrm[:, j, :], scalar1=EPS), reads=["nrm"], writes=["nrm"])
                        P.op("act", lambda: nc.scalar.sqrt(out=nrm[:, j, :], in_=nrm[:, j, :]), reads=["nrm"], writes=["nrm"])
                        P.op("dve", lambda: nc.vector.reciprocal(out=nrm[:, j, :], in_=nrm[:, j, :]), reads=["nrm"], writes=["nrm"])
                        P.op("dve", lambda: nc.vector.tensor_tensor(out=t_[:], in0=t_[:], in1=nrm[:, j, :].unsqueeze(2).to_broadcast([128, 16, HD]), op=ALU.mult),
                             reads=[tk_, "nrm"], writes=[tk_])
                        dst_, dk_ = (qaug[:, :, 0:64], "qaug") if j == 0 else (kb16[:], "kb16")
                        P.op("dve", lambda: nc.vector.tensor_tensor(out=dst_, in0=t_[:], in1=g_[:].unsqueeze(1).to_broadcast([128, 16, HD]), op=ALU.mult),
                             reads=[tk_, gk_], writes=[dk_])
                    yield
                    for i in range(4):
                        for tt in range(4):
                            P.op("pe", lambda: nc.tensor.matmul(psb[7][0:64, tt * 128:(tt + 1) * 128], lhsT=kb16[:, 4 * i + tt, :], rhs=ident_b[:], start=True, stop=True),
                                 reads=["kb16", "ident_b"], writes=["ps7"])
                        P.op("dve", lambda: nc.vector.reduce_sum(out=kmT[:, 2 * i:2 * i + 2], in_=psb[7][0:64, :].rearrange("p (b k) -> p b k", b=2), axis=AX.X),
                             reads=["ps7"], writes=["kmT"])
                        P.op("dve", lambda: nc.vector.tensor_copy(out=kTa[b2_][0:64, i * 512:(i + 1) * 512], in_=psb[7][0:64, :]), reads=["ps7"], writes=[kTak])
                    P.op("dve", lambda: nc.vector.tensor_scalar_mul(out=kmb[:], in0=kmT[:], scalar1=1.0 / 256.0), reads=["kmT"], writes=["kmb"])
                    for i in range(4):
                        for tt in range(4):
                            P.op("pe", lambda: nc.tensor.matmul(psb[7][0:64, tt * 128:(tt + 1) * 128], lhsT=qaug[:, 4 * i + tt, 0:64], rhs=ident_b[:], start=True, stop=True),
                                 reads=["qaug", "ident_b"], writes=["ps7"])
                        P.op("act", lambda: nc.scalar.copy(out=qTg[:, i * 512:(i + 1) * 512], in_=psb[7][0:64, :]), reads=["ps7"], writes=["qTg"])
                    yield
                    for t in range(16):
                        P.op("pe", lambda: nc.tensor.matmul(psb[7][:, t * 8:(t + 1) * 8], lhsT=qTg[:, t * 128:(t + 1) * 128], rhs=kmb[:], start=True, stop=True),
                             reads=["qTg", "kmb"], writes=["ps7"])
                    P.op("dve", lambda: nc.vector.tensor_tensor(out=gate[:], in0=psb[7][:, 0:128].rearrange("p (t n) -> p t n", t=16), in1=selm[:, 0, :, :], op=ALU.add),
                         reads=["ps7", "selm"], writes=["gate"])
                    P.op("dve", lambda: nc.vector.tensor_tensor(out=cmpb[:], in0=gate[:].unsqueeze(2).to_broadcast([128, 16, 8, 8]),
                                                                in1=gate[:].unsqueeze(3).to_broadcast([128, 16, 8, 8]), op=ALU.is_gt), reads=["gate"], writes=["cmpb"])
                    P.op("dve", lambda: nc.vector.reduce_sum(out=rank[:], in_=cmpb[:], axis=AX.X), reads=["cmpb"], writes=["rank"])
                    P.op("dve", lambda: nc.vector.tensor_single_scalar(out=rank[:], in_=rank[:], scalar=3.0, op=ALU.is_lt), reads=["rank"], writes=["rank"])
                    P.op("dve", lambda: nc.vector.tensor_tensor(out=rank[:], in0=rank[:], in1=selm[:, 1, :, :], op=ALU.mult), reads=["rank", "selm"], writes=["rank"])
                    P.op("dve", lambda: nc.vector.tensor_tensor(out=rank[:], in0=rank[:], in1=selm[:, 2, :, :], op=ALU.add), reads=["rank", "selm"], writes=["rank"])
                    P.op("dve", lambda: nc.vector.tensor_scalar(out=qaug[:, :, 64:72], in0=rank[:], scalar1=-NEG, scalar2=NEG, op0=ALU.mult, op1=ALU.add),
                         reads=["rank"], writes=["qaug"])
                    for i in range(4):
                        for tt in range(4):
                            P.op("pe", lambda: nc.tensor.matmul(psb[7][0:72, tt * 128:(tt + 1) * 128], lhsT=qaug[:, 4 * i + tt, :], rhs=ident_b[:], start=True, stop=True),
                                 reads=["qaug", "ident_b"], writes=["ps7"])
                        P.op("act", lambda: nc.scalar.copy(out=qTa[b2_][:, i * 512:(i + 1) * 512], in_=psb[7][0:72, :]), reads=["ps7"], writes=[qTak])

                cnt3 = [0]

                def main(idx, inject):
                    s_, h = divmod(idx, H)
                    b2_ = idx % 2
                    r0 = s_ * SEQ
                    qTak, kTak, vak, wk_ = "qTa%d" % b2_, "kTa%d" % b2_, "vaug%d" % b2_, "Wt%d" % b2_
                    its = []
                    for kt in range(16):
                        k0 = kt * 128
                        for c in range(k0 // 512, 4):
                            its.append((kt, k0, c, max(k0, 512 * c), 512 * (c + 1)))

                    def emit_s(j):
                        kt, k0, c, q_lo, q_hi = its[j]
                        sbk = 4 + (base + j) % 3
                        P.op("pe", lambda: nc.tensor.matmul(psb[sbk][:, 0:q_hi - q_lo], lhsT=kTa[b2_][:, k0:k0 + 128], rhs=qTa[b2_][:, q_lo:q_hi], start=True, stop=True),
                             reads=[kTak, qTak], writes=["ps%d" % sbk])

                    base = cnt3[0]
                    cnt3[0] += len(its)
                    LOOK = 2
                    for j in range(min(LOOK, len(its))):
                        emit_s(j)
                    for j in range(len(its)):
                        kt, k0, c, q_lo, q_hi = its[j]
                        n = q_hi - q_lo
                        cn = base + j
                        sbk = 4 + cn % 3
                        ps_, pw_ = pS[cn % 3], pW[cn % 3]
                        psk, pwk = "pS%d" % (cn % 3), "pW%d" % (cn % 3)
                        if j + LOOK < len(its):
                            emit_s(j + LOOK)
                        P.op("act", lambda: nc.scalar.activation(out=ps_[:, 0:n], in_=psb[sbk][:, 0:n], func=AF.Exp, scale=0.125), reads=["ps%d" % sbk], writes=[psk])
                        P.op("dve", lambda: nc.vector.tensor_tensor(out=pw_[:, 0:n], in0=ps_[:, 0:n], in1=Wt[b2_][:, q_lo - k0:q_hi - k0], op=ALU.mult),
                             reads=[psk, wk_], writes=[pwk])
                        P.op("pe", lambda: nc.tensor.matmul(psb[c][:, q_lo - 512 * c:q_hi - 512 * c], lhsT=vaug[b2_][:, kt, :], rhs=pw_[:, 0:n],
                                                            start=(kt == 0), stop=(kt == 4 * c + 3), skip_group_check=True), reads=[vak, pwk], writes=["ps%d" % c])
                        if j in (3, 14, 25):
                            inject()
                    for c in range(4):
                        P.op("act", lambda: nc.scalar.activation(out=rec[64:128, c * 512:(c + 1) * 512], in_=psb[c][64:128, :], func=AF.Ln), reads=["ps%d" % c], writes=["rec"])
                        P.op("act", lambda: nc.scalar.activation(out=rec[64:128, c * 512:(c + 1) * 512], in_=rec[64:128, c * 512:(c + 1) * 512], func=AF.Exp, scale=-1.0), reads=["rec"], writes=["rec"])
                        P.op("dve", lambda: nc.vector.tensor_tensor(out=aT[b2_][:, c * 512:(c + 1) * 512], in0=psb[c][0:64, :], in1=rec[64:128, c * 512:(c + 1) * 512], op=ALU.mult),
                             reads=["ps%d" % c, "rec"], writes=["aT%d" % b2_])
                    P.op("sp", lambda: nc.sync.dma_start(out=attnT_d[h * HD:(h + 1) * HD, r0:r0 + SEQ], in_=aT[b2_][:]), reads=["aT%d" % b2_], writes=["attnT_d"], dma="aT%d" % b2_)

                NHD = NSEQ * H
                for _ in prologue(0):
                    pass
                for idx in range(NHD):
                    gen = prologue(idx + 1) if idx + 1 < NHD else iter(())
                    main(idx, lambda g_=gen: next(g_, None))
                    for _ in gen:
                        pass
                P.barrier()

        if "s4" in stages:
            with contextlib.ExitStack() as st:
                sb = lambda n, s, d=F32: st.enter_context(nc.sbuf_tensor(n, list(s), d))
                tri = sb("tri", [128, 128]); negtri = sb("negtri", [128, 128])
                dtb = sb("dtb", [128, SSM_H]); abc = sb("abc", [128, SSM_H]); dsk = sb("dsk", [128, SSM_H])
                sng = sb("sng", [128, SSM_INNER])
                xsT = [sb("xsT%d" % i, [128, 16, 128]) for i in range(2)]
                BTb = [sb("BTb%d" % i, [128, 4, 128], BF16) for i in range(2)]
                CTb = [sb("CTb%d" % i, [128, 4, 128], BF16) for i in range(2)]
                zt = [sb("zt%d" % i, [128, SSM_INNER]) for i in range(2)]
                dtr = [sb("dtr%d" % i, [128, SSM_H]) for i in range(2)]
                sm = sb("sm", [128, 13, SSM_H])
                Rb = sb("Rb", [128, SSM_H, 128])
                xs = sb("xs", [128, SSM_INNER])
                xdt = sb("xdt", [128, SSM_INNER], BF16); xdte = sb("xdte", [128, SSM_INNER], BF16)
                Btm = sb("Btm", [128, 4, 128], BF16)
                Dm = [sb("Dm%d" % i, [128, 4, 128]) for i in range(2)]
                MT = [sb("MT%d" % i, [128, 4, 128], BF16) for i in range(2)]
                Hf = sb("Hf", [128, SSM_INNER]); Hb = sb("Hb", [128, SSM_INNER], BF16)
                ysb = sb("ysb", [128, SSM_INNER]); tmp = sb("tmp4", [128, SSM_INNER])
                nr4 = sb("nr4", [128, 8])
                ssb = sb("ssb", [128, SSM_INNER], BF16)
                sT = [sb("sT%d" % i, [128, 16, 512], BF16) for i in range(2)]
                P.op("sp", lambda: nc.sync.dma_start(out=tri[:], in_=c_tri[:, :]), writes=["tri"], dma="tri")
                P.op("sp", lambda: nc.sync.dma_start(out=negtri[:], in_=c_negtri[:, :]), writes=["negtri"], dma="negtri")
                P.op("sp", lambda: nc.sync.dma_start(out=dtb[:], in_=bcast_rows(dt_bias, SSM_H)), writes=["dtb"], dma="dtb")
                P.op("sp", lambda: nc.sync.dma_start(out=abc[:], in_=bcast_rows(a_log, SSM_H)), writes=["abc"], dma="abc")
                P.op("sp", lambda: nc.sync.dma_start(out=dsk[:], in_=bcast_rows(d_skip, SSM_H)), writes=["dsk"], dma="dsk")
                P.op("sp", lambda: nc.sync.dma_start(out=sng[:], in_=bcast_rows(ssm_norm_g, SSM_INNER)), writes=["sng"], dma="sng")
                P.op("act", lambda: nc.scalar.activation(out=abc[:], in_=abc[:], func=AF.Exp), reads=["abc"], writes=["abc"])
                P.op("dve", lambda: nc.vector.tensor_scalar_mul(out=abc[:], in0=abc[:], scalar1=-1.0), reads=["abc"], writes=["abc"])
                V_ = lambda i: sm[:, i, :]
                bc3 = lambda ap, n, w: ap.unsqueeze(2).to_broadcast([128, n, w])
                for s_ in range(NSEQ):
                    P.op("dve", lambda: nc.vector.memset(Hf[:], 0.0), writes=["Hf"])
                    P.op("dve", lambda: nc.vector.memset(Hb[:], 0.0), writes=["Hb"])
                    for c in range(16):
                        cc = s_ * 16 + c
                        b2_ = cc % 2
                        t0 = cc * 128
                        P.op("sp", lambda: nc.sync.dma_start(out=xsT[b2_][:], in_=xbcT_d[0:2048, t0:t0 + 128].rearrange("(m p) t -> p m t", p=128)),
                             reads=["xbcT_d"], writes=["xsT%d" % b2_], dma="xsT%d" % b2_)
                        P.op("pool", lambda: nc.gpsimd.dma_start(out=BTb[b2_][:], in_=xbcT_d[2048:2560, t0:t0 + 128].rearrange("(m p) t -> p m t", p=128)),
                             reads=["xbcT_d"], writes=["BTb%d" % b2_], dma="BTb%d" % b2_)
                        P.op("pool", lambda: nc.gpsimd.dma_start(out=CTb[b2_][:], in_=xbcT_d[2560:3072, t0:t0 + 128].rearrange("(m p) t -> p m t", p=128)),
                             reads=["xbcT_d"], writes=["CTb%d" % b2_], dma="CTb%d" % b2_)
                        P.op("sp", lambda: nc.sync.dma_start(out=zt[b2_][:], in_=z_d[t0:t0 + 128, :]), reads=["z_d"], writes=["zt%d" % b2_], dma="zt%d" % b2_)
                        P.op("sp", lambda: nc.sync.dma_start(out=dtr[b2_][:], in_=dt_d[t0:t0 + 128, :]), reads=["dt_d"], writes=["dtr%d" % b2_], dma="dtr%d" % b2_)
                        xk, bk, ck, zk, dk = "xsT%d" % b2_, "BTb%d" % b2_, "CTb%d" % b2_, "zt%d" % b2_, "dtr%d" % b2_
                        P.op("dve", lambda: nc.vector.tensor_tensor(out=V_(0), in0=dtr[b2_][:], in1=dtb[:], op=ALU.add), reads=[dk, "dtb"], writes=["sm0"])
                        P.op("act", lambda: nc.scalar.activation(out=V_(1), in_=V_(0), func=AF.Abs), reads=["sm0"], writes=["sm1"])
                        P.op("act", lambda: nc.scalar.activation(out=V_(2), in_=V_(1), func=AF.Exp, scale=-1.0), reads=["sm1"], writes=["sm2"])
                        P.op("act", lambda: nc.scalar.activation(out=V_(2), in_=V_(2), func=AF.Ln, bias=1.0), reads=["sm2"], writes=["sm2"])
                        P.op("dve", lambda: nc.vector.scalar_tensor_tensor(out=V_(3), in0=V_(0), scalar=0.0, in1=V_(2), op0=ALU.max, op1=ALU.add), reads=["sm0", "sm2"], writes=["sm3"])
                        P.op("dve", lambda: nc.vector.tensor_tensor(out=V_(4), in0=V_(3), in1=abc[:], op=ALU.mult), reads=["sm3", "abc"], writes=["sm4"])
                        P.op("pe", lambda: nc.tensor.matmul(psb[0][:, 0:32], lhsT=tri[:], rhs=V_(4), start=True, stop=True), reads=["tri", "sm4"], writes=["ps0"])
                        P.op("pe", lambda: nc.tensor.matmul(psb[0][:, 32:64], lhsT=ones_f[:], rhs=V_(4), start=True, stop=True), reads=["ones_f", "sm4"], writes=["ps0"])
                        P.op("act", lambda: nc.scalar.copy(out=sm[:, 5:7, :], in_=psb[0][:, 0:64].rearrange("p (a h) -> p a h", a=2)), reads=["ps0"], writes=["sm5", "sm6"])
                        P.op("dve", lambda: nc.vector.tensor_tensor(out=V_(7), in0=V_(6), in1=V_(5), op=ALU.subtract), reads=["sm5", "sm6"], writes=["sm7"])
                        P.op("act", lambda: nc.scalar.activation(out=sm[:, 10:13, :], in_=sm[:, 5:8, :], func=AF.Exp), reads=["sm5", "sm6", "sm7"], writes=["sm10", "sm11", "sm12"])
                        EACS, CD, DTE = 10, 11, 12
                        P.op("dve", lambda: nc.vector.tensor_tensor(out=Rb[:], in0=tri[:].unsqueeze(1).to_broadcast([128, SSM_H, 128]), in1=bc3(V_(4), SSM_H, 128), op=ALU.mult),
                             reads=["tri", "sm4"], writes=["Rb"])
                        for m in range(16):
                            P.op("pe", lambda: nc.tensor.matmul(psb[3 + m // 4][:, (m % 4) * 128:(m % 4 + 1) * 128], lhsT=xsT[b2_][:, m, :], rhs=ident_f[:], start=True, stop=True),
                                 reads=[xk, "ident_f"], writes=["ps%d" % (3 + m // 4)])
                        for i in range(4):
                            P.op("act", lambda: nc.scalar.copy(out=xs[:, i * 512:(i + 1) * 512], in_=psb[3 + i][:, :]), reads=["ps%d" % (3 + i)], writes=["xs"])
                        xs3 = xs[:].rearrange("p (h d) -> p h d", h=SSM_H)
                        P.op("dve", lambda: nc.vector.tensor_tensor(out=xdt[:].rearrange("p (h d) -> p h d", h=SSM_H), in0=xs3, in1=bc3(V_(3), SSM_H, 64), op=ALU.mult),
                             reads=["xs", "sm3"], writes=["xdt"])
                        P.op("dve", lambda: nc.vector.tensor_tensor(out=xdte[:].rearrange("p (h d) -> p h d", h=SSM_H), in0=xdt[:].rearrange("p (h d) -> p h d", h=SSM_H),
                                                                    in1=bc3(V_(DTE), SSM_H, 64), op=ALU.mult), reads=["xdt", "sm12"], writes=["xdte"])
                        for g in range(4):
                            P.op("pe", lambda: nc.tensor.matmul(psb[7][:, g * 128:(g + 1) * 128], lhsT=BTb[b2_][:, g, :], rhs=ident_b[:], start=True, stop=True),
                                 reads=[bk, "ident_b"], writes=["ps7"])
                        P.op("act", lambda: nc.scalar.copy(out=Btm[:], in_=psb[7][:, :].rearrange("p (g n) -> p g n", g=4)), reads=["ps7"], writes=["Btm"])
                        for g in range(4):
                            P.op("pe", lambda: nc.tensor.matmul(psb[2][:, g * 128:(g + 1) * 128], lhsT=BTb[b2_][:, g, :], rhs=CTb[b2_][:, g, :], start=True, stop=True),
                                 reads=[bk, ck], writes=["ps2"])
                        def emit_ones(j):
                            P.op("pe", lambda: nc.tensor.matmul(psb[j % 2][:, :], lhsT=ones_f[:], rhs=Rb[:, 4 * j:4 * j + 4, :], start=True, stop=True),
                                 reads=["ones_f", "Rb"], writes=["ps%d" % (j % 2)])
                        emit_ones(0)
                        for g in range(4):
                            P.op("pe", lambda: nc.tensor.matmul(psb[4][:, :], lhsT=CTb[b2_][:, g, :], rhs=Hb[:, g * 512:(g + 1) * 512], start=True, stop=True),
                                 reads=[ck, "Hb"], writes=["ps4"])
                            P.op("pe", lambda: nc.tensor.matmul(psb[5][:, :], lhsT=Btm[:, g, :], rhs=xdte[:, g * 512:(g + 1) * 512], start=True, stop=True),
                                 reads=["Btm", "xdte"], writes=["ps5"])
                            for j2 in range(2):
                                j = g * 2 + j2
                                pb = j % 2
                                dm, dmk = Dm[j % 2], "Dm%d" % (j % 2)
                                mt, mtk = MT[j % 2], "MT%d" % (j % 2)
                                P.op("dve", lambda: nc.vector.tensor_tensor(out=dm[:], in0=psb[pb][:, :].rearrange("p (h l) -> p h l", h=4), in1=bc3(sm[:, 5, 4 * j:4 * j + 4], 4, 128), op=ALU.subtract),
                                     reads=["ps%d" % pb, "sm5"], writes=[dmk])
                                if j + 1 < 8:
                                    emit_ones(j + 1)
                                P.op("dve", lambda: nc.vector.tensor_tensor(out=dm[:], in0=dm[:], in1=negtri[:].unsqueeze(1).to_broadcast([128, 4, 128]), op=ALU.add),
                                     reads=[dmk, "negtri"], writes=[dmk])
                                P.op("act", lambda: nc.scalar.activation(out=dm[:], in_=dm[:], func=AF.Exp), reads=[dmk], writes=[dmk])
                                P.op("dve", lambda: nc.vector.tensor_tensor(out=mt[:], in0=dm[:], in1=psb[2][:, g * 128:(g + 1) * 128].unsqueeze(1).to_broadcast([128, 4, 128]), op=ALU.mult),
                                     reads=[dmk, "ps2"], writes=[mtk])
                                for hh in range(4):
                                    hd_ = 4 * j + hh
                                    P.op("pe", lambda: nc.tensor.matmul(psb[3][:, (hd_ % 8) * 64:(hd_ % 8 + 1) * 64], lhsT=mt[:, hh, :], rhs=xdt[:, hd_ * 64:(hd_ + 1) * 64], start=True, stop=True),
                                         reads=[mtk, "xdt"], writes=["ps3"])
                            ysl = ysb[:, g * 512:(g + 1) * 512].rearrange("p (h d) -> p h d", h=8)
                            P.op("dve", lambda: nc.vector.tensor_tensor(out=ysl, in0=psb[4][:, :].rearrange("p (h d) -> p h d", h=8), in1=bc3(sm[:, EACS, 8 * g:8 * g + 8], 8, 64), op=ALU.mult),
                                 reads=["ps4", "sm10"], writes=["ysb"])
                            P.op("dve", lambda: nc.vector.tensor_tensor(out=ysb[:, g * 512:(g + 1) * 512], in0=ysb[:, g * 512:(g + 1) * 512], in1=psb[3][:, :], op=ALU.add),
                                 reads=["ysb", "ps3"], writes=["ysb"])
                            hsl = Hf[:, g * 512:(g + 1) * 512]
                            P.op("dve", lambda: nc.vector.tensor_tensor(out=hsl.rearrange("p (h d) -> p h d", h=8), in0=hsl.rearrange("p (h d) -> p h d", h=8),
                                                                        in1=bc3(sm[:, CD, 8 * g:8 * g + 8], 8, 64), op=ALU.mult), reads=["Hf", "sm11"], writes=["Hf"])
                            P.op("dve", lambda: nc.vector.tensor_tensor(out=hsl, in0=hsl, in1=psb[5][:, :], op=ALU.add), reads=["Hf", "ps5"], writes=["Hf"])
                        P.op("act", lambda: nc.scalar.copy(out=Hb[:], in_=Hf[:]), reads=["Hf"], writes=["Hb"])
                        P.op("dve", lambda: nc.vector.tensor_tensor(out=tmp[:].rearrange("p (h d) -> p h d", h=SSM_H), in0=xs3, in1=bc3(dsk[:], SSM_H, 64), op=ALU.mult),
                             reads=["xs", "dsk"], writes=["tmp4"])
                        P.op("dve", lambda: nc.vector.tensor_tensor(out=ysb[:], in0=ysb[:], in1=tmp[:], op=ALU.add), reads=["ysb", "tmp4"], writes=["ysb"])
                        P.op("act", lambda: nc.scalar.activation(out=tmp[:], in_=zt[b2_][:], func=AF.Silu), reads=[zk], writes=["tmp4"])
                        P.op("dve", lambda: nc.vector.tensor_tensor(out=ysb[:], in0=ysb[:], in1=tmp[:], op=ALU.mult), reads=["ysb", "tmp4"], writes=["ysb"])
                        for g in range(4):
                            P.op("act", lambda: nc.scalar.activation(out=tmp[:, g * 512:(g + 1) * 512], in_=ysb[:, g * 512:(g + 1) * 512], func=AF.Square,
                                                                     scale=float(512 ** -0.5), accum_out=nr4[:, g:g + 1]), reads=["ysb"], writes=["tmp4", "nr4"])
                        P.op("dve", lambda: nc.vector.tensor_scalar_add(out=nr4[:, 4:8], in0=nr4[:, 0:4], scalar1=EPS), reads=["nr4"], writes=["nr4"])
                        P.op("act", lambda: nc.scalar.sqrt(out=nr4[:, 4:8], in_=nr4[:, 4:8]), reads=["nr4"], writes=["nr4"])
                        P.op("dve", lambda: nc.vector.reciprocal(out=nr4[:, 4:8], in_=nr4[:, 4:8]), reads=["nr4"], writes=["nr4"])
                        P.op("dve", lambda: nc.vector.tensor_tensor(out=ysb[:].rearrange("p (g d) -> p g d", g=4), in0=ysb[:].rearrange("p (g d) -> p g d", g=4),
                                                                    in1=bc3(nr4[:, 4:8], 4, 512), op=ALU.mult), reads=["ysb", "nr4"], writes=["ysb"])
                        P.op("dve", lambda: nc.vector.tensor_tensor(out=ssb[:], in0=ysb[:], in1=sng[:], op=ALU.mult), reads=["ysb", "sng"], writes=["ssb"])
                        so, sok = sT[(cc // 4) % 2], "sT%d" % ((cc // 4) % 2)
                        for m in range(16):
                            P.op("pe", lambda: nc.tensor.matmul(psb[3 + m // 4][:, (m % 4) * 128:(m % 4 + 1) * 128], lhsT=ssb[:, m * 128:(m + 1) * 128], rhs=ident_b[:], start=True, stop=True),
                                 reads=["ssb", "ident_b"], writes=["ps%d" % (3 + m // 4)])
                        for i in range(4):
                            P.op("act", lambda: nc.scalar.copy(out=so[:, 4 * i:4 * i + 4, (cc % 4) * 128:(cc % 4 + 1) * 128], in_=psb[3 + i][:, :].rearrange("p (m t) -> p m t", m=4)),
                                 reads=["ps%d" % (3 + i)], writes=[sok])
                        if cc % 4 == 3:
                            tb = (cc // 4) * 512
                            P.op("sp", lambda: nc.sync.dma_start(out=ssmT_d[:, tb:tb + 512].rearrange("(m p) t -> p m t", p=128), in_=so[:]), reads=[sok], writes=["ssmT_d"], dma=sok)
                P.barrier()

        if "s5" in stages:
            with contextlib.ExitStack() as st:
                sb = lambda n, s, d=F32: st.enter_context(nc.sbuf_tensor(n, list(s), d))
                Wa = sb("Wa", [128, 8, D], BF16); Ws = sb("Ws", [128, 16, D], BF16); Wo = sb("Wo", [128, 8, D], BF16)
                rw = sb("rw", [128, 8, NE]); rbb = sb("rbb", [128, NE])
                gbb = sb("gbb", [128, 2 * D]); g2bc = sb("g2bc", [128, D])
                g1b = [sb("g1b%d" % i, [128, D]) for i in range(2)]; A2 = [sb("A2%d" % i, [128, D]) for i in range(2)]; sh2 = [sb("sh2%d" % i, [128, D]) for i in range(2)]
                aTt = [sb("aTt%d" % i, [128, 8, 128], BF16) for i in range(2)]
                sTt = [sb("sTt%d" % i, [128, 16, 128], BF16) for i in range(2)]
                glt = [sb("glt%d" % i, [128, 2 * D]) for i in range(2)]
                xt5 = [sb("xt5%d" % i, [128, D]) for i in range(2)]
                mrg = sb("mrg", [128, D]); tm5 = sb("tm5", [128, D]); mrb = [sb("mrb%d" % i, [128, D], BF16) for i in range(2)]
                mT = sb("mT", [128, 8, 128], BF16)
                x1t = [sb("x1t%d" % i, [128, D]) for i in range(2)]
                u2 = sb("u2", [128, D]); u2Tf = sb("u2Tf", [128, 8, 128])
                u2Tb = [sb("u2Tb%d" % i, [128, 8, 512], BF16) for i in range(2)]
                ss5 = sb("ss5", [128, 2]); sq5 = sb("sq5", [128, D])
                lg = sb("lg", [128, NE]); m8 = sb("m8", [128, 8]); ex = sb("ex", [128, NE]); msk = sb("msk", [128, NE])
                sm5 = sb("sm5_", [128, 4]); gwt = [sb("gwt%d" % i, [128, NE]) for i in range(2)]
                gwT = [sb("gwT%d" % i, [NE, 128]) for i in range(2)]
                for (wsb, wnm, wdr, nk) in [(Wa, "Wa", w_attn, 8), (Ws, "Ws", w_ssm, 16), (Wo, "Wo", w_out, 8)]:
                    for k in range(0, nk, 8):
                        for n in range(2):
                            P.op("pool", lambda: nc.gpsimd.dma_start(out=wsb[:, k:k + 8, n * 512:(n + 1) * 512],
                                                                     in_=wdr[k * 128:(k + 8) * 128, n * 512:(n + 1) * 512].rearrange("(k p) n -> p k n", p=128)),
                                 reads=["wchain"], writes=[wnm, "wchain"], dma="%s_%d_%d" % (wnm, k, n))
                P.op("sp", lambda: nc.sync.dma_start(out=rw[:], in_=router_w.rearrange("(k p) n -> p k n", p=128)), writes=["rw"], dma="rw")
                P.op("sp", lambda: nc.sync.dma_start(out=rbb[:], in_=bcast_rows(router_b, NE)), writes=["rbb"], dma="rbb")
                P.op("sp", lambda: nc.sync.dma_start(out=gbb[:], in_=bcast_rows(gate_bias, 2 * D)), writes=["gbb"], dma="gbb")
                P.op("sp", lambda: nc.sync.dma_start(out=g2bc[:], in_=bcast_rows(norm2_g, D)), writes=["g2bc"], dma="g2bc")
                def st_a1(t):
                        s_ = t // 16
                        b2_ = t % 2
                        t0 = t * 128
                        ak, sk, gk_, xk = "aTt%d" % b2_, "sTt%d" % b2_, "glt%d" % b2_, "xt5%d" % b2_
                        x1_, x1k = x1t[b2_], "x1t%d" % b2_
                        gw_, gwk = gwt[b2_], "gwt%d" % b2_
                        ub_, ubk = u2Tb[(t // 4) % 2], "u2Tb%d" % ((t // 4) % 2)
                        if t % 16 == 0:
                            P.op("sp", lambda: nc.sync.dma_start(out=g1b[s_][:], in_=bcast_rows(mod_d[s_, 2 * D:3 * D], D)), reads=["mod_d"], writes=["g1b%d" % s_], dma="g1b%d" % s_)
                            P.op("sp", lambda: nc.sync.dma_start(out=sh2[s_][:], in_=bcast_rows(mod_d[s_, 3 * D:4 * D], D)), reads=["mod_d"], writes=["sh2%d" % s_], dma="sh2%d" % s_)
                            P.op("sp", lambda: nc.sync.dma_start(out=A2[s_][:], in_=bcast_rows(mod_d[s_, 4 * D:5 * D], D)), reads=["mod_d"], writes=["A2%d" % s_], dma="A2%d" % s_)
                            P.op("dve", lambda: nc.vector.scalar_tensor_tensor(out=A2[s_][:], in0=A2[s_][:], scalar=1.0, in1=g2bc[:], op0=ALU.add, op1=ALU.mult),
                                 reads=["A2%d" % s_, "g2bc"], writes=["A2%d" % s_])
                        ak, sk, gk_, xk = "aTt%d" % b2_, "sTt%d" % b2_, "glt%d" % b2_, "xt5%d" % b2_
                        P.op("sp", lambda: nc.sync.dma_start(out=aTt[b2_][:], in_=attnT_d[:, t0:t0 + 128].rearrange("(k p) t -> p k t", p=128)), reads=["attnT_d"], writes=[ak], dma=ak)
                        P.op("sp", lambda: nc.sync.dma_start(out=sTt[b2_][:], in_=ssmT_d[:, t0:t0 + 128].rearrange("(k p) t -> p k t", p=128)), reads=["ssmT_d"], writes=[sk], dma=sk)
                        P.op("sp", lambda: nc.sync.dma_start(out=glt[b2_][:], in_=gl_d[t0:t0 + 128, :]), reads=["gl_d"], writes=[gk_], dma=gk_)
                        P.op("sp", lambda: nc.sync.dma_start(out=xt5[b2_][:], in_=x_d[t0:t0 + 128, :]), writes=[xk], dma=xk)
                        P.op("dve", lambda: nc.vector.tensor_tensor(out=glt[b2_][:], in0=glt[b2_][:], in1=gbb[:], op=ALU.add), reads=[gk_, "gbb"], writes=[gk_])
                        P.op("act", lambda: nc.scalar.activation(out=glt[b2_][:], in_=glt[b2_][:], func=AF.Sigmoid), reads=[gk_], writes=[gk_])
                        for n in range(2):
                            for k in range(8):
                                P.op("pe", lambda: nc.tensor.matmul(psb[n][:, :], lhsT=aTt[b2_][:, k, :], rhs=Wa[:, k, n * 512:(n + 1) * 512], start=(k == 0), stop=(k == 7)),
                                     reads=[ak, "Wa"], writes=["ps%d" % n])
                            P.op("dve", lambda: nc.vector.tensor_tensor(out=mrg[:, n * 512:(n + 1) * 512], in0=psb[n][:, :], in1=glt[b2_][:, n * 512:(n + 1) * 512], op=ALU.mult),
                                 reads=["ps%d" % n, gk_], writes=["mrg"])
                            for k in range(16):
                                P.op("pe", lambda: nc.tensor.matmul(psb[2 + n][:, :], lhsT=sTt[b2_][:, k, :], rhs=Ws[:, k, n * 512:(n + 1) * 512], start=(k == 0), stop=(k == 15)),
                                     reads=[sk, "Ws"], writes=["ps%d" % (2 + n)])
                            P.op("dve", lambda: nc.vector.tensor_tensor(out=tm5[:, n * 512:(n + 1) * 512], in0=psb[2 + n][:, :], in1=glt[b2_][:, D + n * 512:D + (n + 1) * 512], op=ALU.mult),
                                 reads=["ps%d" % (2 + n), gk_], writes=["tm5"])
                        P.op("dve", lambda: nc.vector.tensor_tensor(out=mrb[b2_][:], in0=mrg[:], in1=tm5[:], op=ALU.add), reads=["mrg", "tm5"], writes=["mrb%d" % b2_])

                def st_a2(t):
                        s_ = t // 16
                        b2_ = t % 2
                        t0 = t * 128
                        ak, sk, gk_, xk = "aTt%d" % b2_, "sTt%d" % b2_, "glt%d" % b2_, "xt5%d" % b2_
                        x1_, x1k = x1t[b2_], "x1t%d" % b2_
                        gw_, gwk = gwt[b2_], "gwt%d" % b2_
                        ub_, ubk = u2Tb[(t // 4) % 2], "u2Tb%d" % ((t // 4) % 2)
                        for half in range(2):
                            for kk in range(4):
                                k = half * 4 + kk
                                P.op("pe", lambda: nc.tensor.matmul(psb[4][:, kk * 128:(kk + 1) * 128], lhsT=mrb[b2_][:, k * 128:(k + 1) * 128], rhs=ident_b[:], start=True, stop=True),
                                     reads=["mrb%d" % b2_, "ident_b"], writes=["ps4"])
                            P.op("act", lambda: nc.scalar.copy(out=mT[:, half * 4:half * 4 + 4, :], in_=psb[4][:, :].rearrange("p (k t) -> p k t", k=4)),
                                 reads=["ps4"], writes=["mT"])
                        x1_, x1k = x1t[b2_], "x1t%d" % b2_
                        for n in range(2):
                            for k in range(8):
                                P.op("pe", lambda: nc.tensor.matmul(psb[6 + n][:, :], lhsT=mT[:, k, :], rhs=Wo[:, k, n * 512:(n + 1) * 512], start=(k == 0), stop=(k == 7)),
                                     reads=["mT", "Wo"], writes=["ps%d" % (6 + n)])
                            P.op("dve", lambda: nc.vector.tensor_tensor(out=x1_[:, n * 512:(n + 1) * 512], in0=psb[6 + n][:, :], in1=g1b[s_][:, n * 512:(n + 1) * 512], op=ALU.mult),
                                 reads=["ps%d" % (6 + n), "g1b%d" % s_], writes=[x1k])
                        P.op("dve", lambda: nc.vector.tensor_tensor(out=x1_[:], in0=x1_[:], in1=xt5[b2_][:], op=ALU.add), reads=[x1k, xk], writes=[x1k])
                        P.op("sp", lambda: nc.sync.dma_start(out=x1_d[t0:t0 + 128, :], in_=x1_[:]), reads=[x1k], writes=["x1_d"], dma=x1k)

                def st_b(t):
                        s_ = t // 16
                        b2_ = t % 2
                        t0 = t * 128
                        ak, sk, gk_, xk = "aTt%d" % b2_, "sTt%d" % b2_, "glt%d" % b2_, "xt5%d" % b2_
                        x1_, x1k = x1t[b2_], "x1t%d" % b2_
                        gw_, gwk = gwt[b2_], "gwt%d" % b2_
                        ub_, ubk = u2Tb[(t // 4) % 2], "u2Tb%d" % ((t // 4) % 2)
                        P.op("act", lambda: nc.scalar.activation(out=sq5[:], in_=x1_[:], func=AF.Square, scale=1.0 / 32.0, accum_out=ss5[:, 0:1]), reads=[x1k], writes=["sq5", "ss5"])
                        P.op("dve", lambda: nc.vector.tensor_scalar_add(out=ss5[:, 1:2], in0=ss5[:, 0:1], scalar1=EPS), reads=["ss5"], writes=["ss5"])
                        P.op("act", lambda: nc.scalar.sqrt(out=ss5[:, 1:2], in_=ss5[:, 1:2]), reads=["ss5"], writes=["ss5"])
                        P.op("dve", lambda: nc.vector.reciprocal(out=ss5[:, 1:2], in_=ss5[:, 1:2]), reads=["ss5"], writes=["ss5"])
                        P.op("dve", lambda: nc.vector.scalar_tensor_tensor(out=u2[:], in0=x1_[:], scalar=ss5[:, 1:2], in1=A2[s_][:], op0=ALU.mult, op1=ALU.mult),
                             reads=[x1k, "ss5", "A2%d" % s_], writes=["u2"])
                        P.op("dve", lambda: nc.vector.tensor_tensor(out=u2[:], in0=u2[:], in1=sh2[s_][:], op=ALU.add), reads=["u2", "sh2%d" % s_], writes=["u2"])
                        ub_, ubk = u2Tb[(t // 4) % 2], "u2Tb%d" % ((t // 4) % 2)
                        for half in range(2):
                            for kk in range(4):
                                k = half * 4 + kk
                                P.op("pe", lambda: nc.tensor.matmul(psb[5][:, kk * 128:(kk + 1) * 128], lhsT=u2[:, k * 128:(k + 1) * 128], rhs=ident_f[:], start=True, stop=True),
                                     reads=["u2", "ident_f"], writes=["ps5"])
                            P.op("act", lambda: nc.scalar.copy(out=u2Tf[:, half * 4:half * 4 + 4, :], in_=psb[5][:, :].rearrange("p (k t) -> p k t", k=4)),
                                 reads=["ps5"], writes=["u2Tf"])
                            P.op("dve", lambda: nc.vector.tensor_copy(out=ub_[:, half * 4:half * 4 + 4, (t % 4) * 128:(t % 4 + 1) * 128], in_=psb[5][:, :].rearrange("p (k t) -> p k t", k=4)),
                                 reads=["ps5"], writes=[ubk])
                        if t % 4 == 3:
                            tb = (t // 4) * 512
                            P.op("sp", lambda: nc.sync.dma_start(out=u2T_d[:, tb:tb + 512].rearrange("(k p) t -> p k t", p=128), in_=ub_[:]), reads=[ubk], writes=["u2T_d"], dma=ubk)
                        for k in range(8):
                            P.op("pe", lambda: nc.tensor.matmul(psb[5][:, 0:NE], lhsT=u2Tf[:, k, :], rhs=rw[:, k, :], start=(k == 0), stop=(k == 7)), reads=["u2Tf", "rw"], writes=["ps5"])
                        P.op("dve", lambda: nc.vector.tensor_tensor(out=lg[:], in0=psb[5][:, 0:NE], in1=rbb[:], op=ALU.add), reads=["ps5", "rbb"], writes=["lg"])
                        P.op("dve", lambda: nc.vector.max(out=m8[:], in_=lg[:]), reads=["lg"], writes=["m8"])
                        P.op("dve", lambda: nc.vector.tensor_scalar(out=msk[:], in0=lg[:], scalar1=m8[:, 3:4], scalar2=None, op0=ALU.is_ge), reads=["lg", "m8"], writes=["msk"])
                        P.op("dve", lambda: nc.vector.tensor_scalar_mul(out=sm5[:, 0:1], in0=m8[:, 0:1], scalar1=-1.0), reads=["m8"], writes=["sm5_"])
                        P.op("act", lambda: nc.scalar.activation(out=ex[:], in_=lg[:], func=AF.Exp, bias=sm5[:, 0:1], scale=1.0), reads=["lg", "sm5_"], writes=["ex"])
                        P.op("dve", lambda: nc.vector.tensor_tensor(out=ex[:], in0=ex[:], in1=msk[:], op=ALU.mult), reads=["ex", "msk"], writes=["ex"])
                        P.op("dve", lambda: nc.vector.reduce_sum(out=sm5[:, 1:2], in_=ex[:], axis=AX.X), reads=["ex"], writes=["sm5_"])
                        P.op("dve", lambda: nc.vector.reciprocal(out=sm5[:, 2:3], in_=sm5[:, 1:2]), reads=["sm5_"], writes=["sm5_"])
                        gw_, gwk = gwt[b2_], "gwt%d" % b2_
                        P.op("dve", lambda: nc.vector.tensor_scalar(out=gw_[:], in0=ex[:], scalar1=sm5[:, 2:3], scalar2=None, op0=ALU.mult), reads=["ex", "sm5_"], writes=[gwk])
                        P.op("sp", lambda: nc.sync.dma_start(out=gwtm_d[t0:t0 + 128, :], in_=gw_[:]), reads=[gwk], writes=["gwtm_d"], dma=gwk)
                        P.op("pe", lambda: nc.tensor.matmul(psb[5][0:NE, 128:256], lhsT=gw_[:], rhs=ident_f[:], start=True, stop=True), reads=[gwk, "ident_f"], writes=["ps5"])
                        P.op("act", lambda: nc.scalar.copy(out=gwT[b2_][:], in_=psb[5][0:NE, 128:256]), reads=["ps5"], writes=["gwT%d" % b2_])
                        P.op("sp", lambda: nc.sync.dma_start(out=gw_d[:, t0:t0 + 128], in_=gwT[b2_][:]), reads=["gwT%d" % b2_], writes=["gw_d"], dma="gwT%d" % b2_)

                for i in range(NT + 2):
                    if i < NT:
                        st_a1(i)
                    if 0 <= i - 1 < NT:
                        st_a2(i - 1)
                    if 0 <= i - 2 < NT:
                        st_b(i - 2)
                P.barrier()

        if "s6" in stages:
            PT = 1024
            with contextlib.ExitStack() as st:
                sb = lambda n, s, d=F32: st.enter_context(nc.sbuf_tensor(n, list(s), d))
                u2T = sb("u2T6", [128, 8, PT], BF16)
                gwT6 = sb("gwT6", [NE, PT])
                b1s = sb("b1s", [128, NE, 16]); b2s = sb("b2s", [NE, D]); g2b = sb("g2b", [128, D])
                yacc = sb("yacc", [128, PT // 128, D])
                w1a = [sb("w1a%d" % i, [128, 8, 512], BF16) for i in range(3)]
                w1l = [sb("w1l%d" % i, [128, 8, 512], BF16) for i in range(3)]
                w2b = [sb("w2b%d" % i, [128, 8, 512], BF16) for i in range(4)]
                gwtm = sb("gwtm", [128, PT // 128, NE])
                ebuf = {nm: [sb("%s%d" % (nm, i), [128, 512]) for i in range(2)] for nm in ("gg", "ll", "t1", "t2")}
                actT = [[sb("actT%d%d" % (a, b), [128, 8, 512], BF16) for b in range(2)] for a in range(2)]
                x16 = [sb("x16%d" % i, [128, D]) for i in range(2)]
                P.op("sp", lambda: nc.sync.dma_start(out=b1s[:], in_=b1T[:, :, :]), writes=["b1s"], dma="b1s")
                P.op("sp", lambda: nc.sync.dma_start(out=b2s[:], in_=b2[:, :]), writes=["b2s"], dma="b2s")
                b17 = sb("b17", [128, NE, 8])
                P.op("dve", lambda: nc.vector.tensor_scalar_add(out=b17[:], in0=b1s[:, :, 8:16], scalar1=7.0), reads=["b1s"], writes=["b17"])
                NPASS = T // PT
                chunks = [(ps_, e, hh) for ps_ in range(NPASS) for e in range(NE) for hh in range(2)]
                state = {"w1next": 0, "ecnt": 0}

                def emit_w1(upto):
                    while state["w1next"] <= min(upto, len(chunks) - 1):
                        j = state["w1next"]
                        _, e, hh = chunks[j]
                        for (wl_, nm, c0) in [(w1a, "w1a", hh * 512), (w1l, "w1l", FF + hh * 512)]:
                            wk = "%s%d" % (nm, j % 3)
                            P.op("pool", lambda: nc.gpsimd.dma_start(out=wl_[j % 3][:], in_=w1[e, :, c0:c0 + 512].rearrange("(k p) n -> p k n", p=128)),
                                 writes=[wk], dma=wk)
                        state["w1next"] += 1

                def h_phase(ps_, e, jbase):
                    par = e % 2
                    for hh in range(2):
                        j = jbase + hh
                        emit_w1(j + 2)
                        wa_, wak = w1a[j % 3], "w1a%d" % (j % 3)
                        wl2_, wlk = w1l[j % 3], "w1l%d" % (j % 3)
                        for ii in range(4):
                            i = hh * 4 + ii
                            for grp in range(2):
                                n_ = state["ecnt"]; state["ecnt"] += 1
                                pa, pb = (n_ % 2) * 2, (n_ % 2) * 2 + 1
                                for k in range(8):
                                    P.op("pe", lambda: nc.tensor.matmul(psb[pa][:, :], lhsT=wa_[:, k, ii * 128:(ii + 1) * 128], rhs=u2T[:, k, grp * 512:(grp + 1) * 512], start=(k == 0), stop=(k == 7)),
                                         reads=[wak, "u2T6"], writes=["ps%d" % pa])
                                for k in range(8):
                                    P.op("pe", lambda: nc.tensor.matmul(psb[pb][:, :], lhsT=wl2_[:, k, ii * 128:(ii + 1) * 128], rhs=u2T[:, k, grp * 512:(grp + 1) * 512], start=(k == 0), stop=(k == 7)),
                                         reads=[wlk, "u2T6"], writes=["ps%d" % pb])
                                B = {nm: (ebuf[nm][n_ % 2], "%s%d" % (nm, n_ % 2)) for nm in ebuf}
                                P.op("dve", lambda: nc.vector.tensor_scalar(out=B["gg"][0][:], in0=psb[pa][:, :], scalar1=b1s[:, e, i:i + 1], scalar2=7.0, op0=ALU.add, op1=ALU.min),
                                     reads=["ps%d" % pa, "b1s"], writes=[B["gg"][1]])
                                P.op("act", lambda: nc.scalar.activation(out=B["t1"][0][:], in_=B["gg"][0][:], func=AF.Gelu_apprx_sigmoid), reads=[B["gg"][1]], writes=[B["t1"][1]])
                                P.op("act", lambda: nc.scalar.activation(out=B["ll"][0][:], in_=psb[pb][:, :], func=AF.Relu, bias=b17[:, e, i:i + 1], scale=1.0),
                                     reads=["ps%d" % pb, "b17"], writes=[B["ll"][1]])
                                P.op("dve", lambda: nc.vector.tensor_scalar(out=B["t2"][0][:], in0=B["ll"][0][:], scalar1=14.0, scalar2=-6.0, op0=ALU.min, op1=ALU.add),
                                     reads=[B["ll"][1]], writes=[B["t2"][1]])
                                P.op("dve", lambda: nc.vector.tensor_tensor(out=actT[par][grp][:, i, :], in0=B["t1"][0][:], in1=B["t2"][0][:], op=ALU.mult),
                                     reads=[B["t1"][1], B["t2"][1]], writes=["actT%d%d" % (par, grp)])

                def w_loads(e):
                    for n in range(2):
                        wi = (e % 2) * 2 + n
                        P.op("pool", lambda: nc.gpsimd.dma_start(out=w2b[wi][:], in_=w2[e, :, n * 512:(n + 1) * 512].rearrange("(k p) n -> p k n", p=128)),
                             writes=["w2b%d" % wi], dma="w2b%d" % wi)

                def w_phase(e):
                    par = e % 2
                    for n in range(2):
                        for tl in range(PT // 128):
                            grp, off = tl // 4, (tl % 4) * 128
                            pk = 5 + (tl % 2)
                            wi = (e % 2) * 2 + n
                            for k in range(8):
                                P.op("pe", lambda: nc.tensor.matmul(psb[pk][:, :], lhsT=actT[par][grp][:, k, off:off + 128], rhs=w2b[wi][:, k, :], start=(k == 0), stop=(k == 7)),
                                     reads=["actT%d%d" % (par, grp), "w2b%d" % wi], writes=["ps%d" % pk])
                            P.op("dve", lambda: nc.vector.scalar_tensor_tensor(out=yacc[:, tl, n * 512:(n + 1) * 512], in0=psb[pk][:, :], scalar=gwtm[:, tl, e:e + 1],
                                                                               in1=yacc[:, tl, n * 512:(n + 1) * 512], op0=ALU.mult, op1=ALU.add),
                                 reads=["yacc", "ps%d" % pk, "gwtm"], writes=["yacc"])

                for ps_ in range(NPASS):
                    tb = ps_ * PT
                    s_ = tb // SEQ
                    P.op("sp", lambda: nc.sync.dma_start(out=u2T[:], in_=u2T_d[:, tb:tb + PT].rearrange("(k p) t -> p k t", p=128)), reads=["u2T_d"], writes=["u2T6"], dma="u2T6")
                    P.op("sp", lambda: nc.sync.dma_start(out=gwT6[:], in_=gw_d[:, tb:tb + PT]), reads=["gw_d"], writes=["gwT6"], dma="gwT6")
                    P.op("sp", lambda: nc.sync.dma_start(out=gwtm[:], in_=gwtm_d[tb:tb + PT, :].rearrange("(t p) e -> p t e", p=128)), reads=["gwtm_d"], writes=["gwtm"], dma="gwtm")
                    if tb % SEQ == 0:
                        P.op("sp", lambda: nc.sync.dma_start(out=g2b[:], in_=bcast_rows(mod_d[s_, 5 * D:6 * D], D)), reads=["mod_d"], writes=["g2b"], dma="g2b")
                    for tl in range(PT // 128):
                        for n in range(2):
                            pk = 5 + (n % 2)
                            P.op("pe", lambda: nc.tensor.matmul(psb[pk][:, :], lhsT=gwT6[:, tl * 128:(tl + 1) * 128], rhs=b2s[:, n * 512:(n + 1) * 512], start=True, stop=True),
                                 reads=["gwT6", "b2s"], writes=["ps%d" % pk])
                            P.op("act", lambda: nc.scalar.copy(out=yacc[:, tl, n * 512:(n + 1) * 512], in_=psb[pk][:, :]), reads=["ps%d" % pk], writes=["yacc"])
                    jb = ps_ * NE * 2
                    h_phase(ps_, 0, jb)
                    w_loads(0)
                    for e in range(NE):
                        if e + 1 < NE:
                            w_loads(e + 1)
                        if e + 1 < NE:
                            h_phase(ps_, e + 1, jb + (e + 1) * 2)
                        w_phase(e)
                    for tl in range(PT // 128):
                        r0 = tb + tl * 128
                        xb_, xk = x16[tl % 2], "x16%d" % (tl % 2)
                        P.op("sp", lambda: nc.sync.dma_start(out=xb_[:], in_=x1_d[r0:r0 + 128, :]), reads=["x1_d"], writes=[xk], dma=xk)
                        P.op("dve", lambda: nc.vector.tensor_tensor(out=yacc[:, tl, :], in0=yacc[:, tl, :], in1=g2b[:], op=ALU.mult), reads=["yacc", "g2b"], writes=["yacc"])
                        P.op("dve", lambda: nc.vector.tensor_tensor(out=xb_[:], in0=yacc[:, tl, :], in1=xb_[:], op=ALU.add), reads=["yacc", xk], writes=[xk])
                        P.op("sp", lambda: nc.sync.dma_start(out=out_d[r0:r0 + 128, :], in_=xb_[:]), reads=[xk], writes=["out"], dma=xk)
                P.barrier()

        if "copyout" in stages:
            with nc.sbuf_tensor("cpy", [128, D], F32) as cpy:
                P.op("sp", lambda: nc.sync.dma_start(out=cpy[:], in_=x_d[0:128, :]), writes=["cpy"], dma="cpy")
                P.op("sp", lambda: nc.sync.dma_start(out=out_d[0:128, :], in_=cpy[:]), reads=["cpy"], writes=["out"], dma="cpy")
                P.drain("sp", outs_to_drain)
        for name in dbg:
            if name in ("qkv", "z", "dt", "gl", "xbcT"):
                pass
        P.drain("sp", outs_to_drain)
    return nc, P, dbg_outs


def make_consts():
    ident = np.eye(128, dtype=np.float32)
    rev = ident[::-1].copy()
    tri = np.triu(np.ones((128, 128), np.float32))
    negtri = np.where(np.arange(128)[None, :] >= np.arange(128)[:, None], 0.0, NEG).astype(np.float32)
    i = np.arange(2304)
    d = i - 127
    dd = np.maximum(d, 1).astype(np.float32)
    large = 16 + (np.log(dd / 16.0) / math.log(1024 / 16.0) * 16).astype(np.int32)
    large = np.minimum(large, 31)
    bucket = np.where(d < 16, np.maximum(d, 0), large)
    oh = np.zeros((33, 2304), np.float32)
    oh[bucket, i] = 1.0
    oh[:, d < 0] = 0.0
    oh[32, d < 0] = 1.0
    sel = np.zeros((128, 3, 16, 8), np.float32)
    for t in range(16):
        qb = t // 2
        for n in range(8):
            if n >= qb:
                sel[:, 0, t, n] = -1e30
            else:
                sel[:, 1, t, n] = 1.0
            if n == qb:
                sel[:, 2, t, n] = 1.0
    kind = (np.arange(SEQ)[None, :] // 256 == np.arange(8)[:, None]).astype(np.float32)
    return dict(c_ident=ident, c_rev=rev, c_tri=tri, c_negtri=negtri, c_bucket=oh, c_selmask=sel, c_kind=kind)


def make_in_maps(inputs):
    f = lambda a: np.ascontiguousarray(np.asarray(a, dtype=np.float32))
    consts = make_consts()
    x = f(inputs["x"]).reshape(NCORES, T, D)
    c = f(inputs["c"]).reshape(NCORES, NSEQ, D)
    rel = np.concatenate([f(inputs["rel_bias_table"]), np.full((1, H), NEG, np.float32)], 0)
    b1 = f(inputs["expert_b1"])[0]
    shared = dict(
        ada_w=f(inputs["ada_w"])[0], ada_b=f(inputs["ada_b"])[0], norm1_g=f(inputs["norm1_g"])[0], w_in=f(inputs["w_in"])[0],
        q_norm_g=f(inputs["q_norm_g"])[0], k_norm_g=f(inputs["k_norm_g"])[0], rel_tab=rel,
        conv_wT=f(f(inputs["conv_w"])[0].T), conv_b=f(f(inputs["conv_b"])[0].reshape(24, 128).T), dt_bias=f(inputs["dt_bias"])[0],
        a_log=f(inputs["a_log"])[0], d_skip=f(inputs["d_skip"])[0], ssm_norm_g=f(inputs["ssm_norm_g"])[0],
        w_attn=f(inputs["w_attn_branch"])[0], w_ssm=f(inputs["w_ssm_branch"])[0], gate_bias=f(inputs["gate_bias"])[0],
        w_out=f(inputs["w_out"])[0], norm2_g=f(inputs["norm2_g"])[0], router_w=f(inputs["router_w"])[0],
        router_b=f(inputs["router_b"])[0], w1=f(inputs["expert_w1"])[0],
        b1T=f(b1.reshape(NE, 16, 128).transpose(2, 0, 1)), w2=f(inputs["expert_w2"])[0], b2=f(inputs["expert_b2"])[0],
    )
    shared.update(consts)
    maps = []
    for i in range(NCORES):
        m = dict(shared)
        m["x"] = x[i]
        m["cT"] = f(c[i].T)
        maps.append(m)
    return maps


def kernel(**inputs):
    nc, P, _ = build_program()
    in_maps = make_in_maps(inputs)
    res = run_bass_kernel_spmd(nc, in_maps, core_ids=list(range(NCORES)))
    out = np.stack([np.asarray(r["out"], dtype=np.float32) for r in res.results], 0)
    return out.reshape(16, SEQ, D)
```

```python
import contextlib
import os
import math
import numpy as np
import concourse.bass as bass
import concourse.mybir as mybir
from concourse.bass_utils import run_bass_kernel_spmd

F32 = mybir.dt.float32
BF16 = mybir.dt.bfloat16
AF = mybir.ActivationFunctionType
ALU = mybir.AluOpType
AX = mybir.AxisListType

NCORES = 8
D = 1024
SEQ = 2048
NSEQ = 2
T = NSEQ * SEQ
NT = T // 128
H = 16
HD = 64
SSM_INNER = 2048
SSM_H = 32
SSM_G = 4
SSM_N = 128
CONV_DIM = 3072
NE = 32
FF = 1024
EPS = 1e-6
IN_PROJ = 10272
OFF_Q, OFF_K, OFF_V, OFF_Z, OFF_XBC, OFF_DT, OFF_GATE = 0, 1024, 2048, 3072, 5120, 8192, 8224
NEG = -30000.0


class Prog:
    def __init__(self, nc, es):
        self.nc, self.es = nc, es
        self.eng = {"pe": nc.tensor, "act": nc.scalar, "dve": nc.vector, "pool": nc.gpsimd, "sp": nc.sync}
        self.sems, self.val = {}, {}
        self.seen = {e: {} for e in self.eng}
        self.lastw, self.readers = {}, {}
        self.nins = 0
        self.free = []
        self.free_sw = []
        self.swkeys = set()
        self.nsem = 0

    def sem(self, key, sw=False):
        if key not in self.sems:
            fl = self.free_sw if sw else self.free
            if sw:
                self.swkeys.add(key)
            if fl:
                h, v = fl.pop()
                self.sems[key] = h
                self.val[key] = v
                for e in self.seen:
                    self.seen[e][key] = v
            else:
                self.nsem += 1
                self.sems[key] = self.es.enter_context(self.nc.semaphore("s%d" % self.nsem))
                self.val[key] = 0
        return self.sems[key]

    def sb(self, name, shape, dt=F32):
        return self.es.enter_context(self.nc.sbuf_tensor(name, list(shape), dt))

    def ps(self, name, shape, dt=F32):
        return self.es.enter_context(self.nc.psum_tensor(name, list(shape), dt))

    def op(self, eng, fn, reads=(), writes=(), dma=None):
        deps = {}
        for b in reads:
            for k, v in self.lastw.get(b, {}).items():
                deps[k] = max(deps.get(k, 0), v)
            if b.startswith("ps"):
                for k, v in self.readers.get(b, {}).items():
                    if k != eng:
                        deps[k] = max(deps.get(k, 0), v)
        for b in writes:
            for k, v in self.lastw.get(b, {}).items():
                deps[k] = max(deps.get(k, 0), v)
            for k, v in self.readers.get(b, {}).items():
                deps[k] = max(deps.get(k, 0), v)
        e = self.eng[eng]
        for k, v in deps.items():
            if dma is None and k == eng and eng == "pe":
                continue
            if self.seen[eng].get(k, 0) >= v:
                continue
            e.wait_ge(self.sems[k], v)
            self.seen[eng][k] = v
        ins = fn()
        if dma is None:
            key, inc = eng, 1
        else:
            key, inc = "dma:" + dma, 16
        s = self.sem(key, sw=(dma is not None and eng == "pool"))
        self.val[key] += inc
        ins.then_inc(s, inc)
        v = self.val[key]
        for b in writes:
            if dma is None:
                self.lastw[b] = {key: v}
            else:
                d_ = {k_: v_ for k_, v_ in self.lastw.get(b, {}).items() if k_.startswith("dma:")}
                d_[key] = v
                self.lastw[b] = d_
            self.readers[b] = {}
        for b in reads:
            self.readers.setdefault(b, {})[key] = v
        self.nins += 1
        return ins

    def barrier(self):
        for eng, e in self.eng.items():
            for k, v in self.val.items():
                if v > 0 and self.seen[eng].get(k, 0) < v:
                    e.wait_ge(self.sems[k], v)
                    self.seen[eng][k] = v
        for k in [k for k in self.sems if k.startswith("dma:")]:
            (self.free_sw if k in self.swkeys else self.free).append((self.sems.pop(k), self.val.pop(k)))
            self.swkeys.discard(k)
            for eng in self.seen:
                self.seen[eng].pop(k, None)
        self.lastw, self.readers = {}, {}

    def drain(self, eng, bufs):
        e = self.eng[eng]
        for b in bufs:
            for k, v in self.lastw.get(b, {}).items():
                if self.seen[eng].get(k, 0) < v:
                    e.wait_ge(self.sems[k], v)
                    self.seen[eng][k] = v


def bcast_rows(ap1d, n):
    return bass.AP(ap1d.tensor, ap1d.offset, [[0, 128], [1, n]])


def build_program(stages=("s0", "s1", "s3", "s4", "s5", "s6"), dbg=()):
    nc = bass.Bass("TRN2", target_bir_lowering=False)
    es = contextlib.ExitStack()

    def din(name, shape, dt=F32):
        return nc.dram_tensor(name, list(shape), dt, kind="ExternalInput").ap()

    def dscr(name, shape, dt=F32):
        if name in dbg:
            outs_to_drain.append(name)
            return nc.dram_tensor(name, list(shape), dt, kind="ExternalOutput").ap()
        return nc.dram_tensor(name, list(shape), dt, kind="Internal").ap()

    def dout(name, shape, dt=F32):
        return nc.dram_tensor(name, list(shape), dt, kind="ExternalOutput").ap()

    outs_to_drain = ["out"]
    x_d = din("x", [T, D])
    cT_d = din("cT", [D, NSEQ])
    ada_w = din("ada_w", [D, 6 * D])
    ada_b = din("ada_b", [6 * D])
    norm1_g = din("norm1_g", [D])
    w_in = din("w_in", [D, IN_PROJ])
    q_norm_g = din("q_norm_g", [HD])
    k_norm_g = din("k_norm_g", [HD])
    rel_tab = din("rel_tab", [33, H])
    conv_wT = din("conv_wT", [CONV_DIM, 4])
    conv_b = din("conv_b", [128, 24])
    dt_bias = din("dt_bias", [SSM_H])
    a_log = din("a_log", [SSM_H])
    d_skip = din("d_skip", [SSM_H])
    ssm_norm_g = din("ssm_norm_g", [SSM_INNER])
    w_attn = din("w_attn", [D, D])
    w_ssm = din("w_ssm", [SSM_INNER, D])
    gate_bias = din("gate_bias", [2 * D])
    w_out = din("w_out", [D, D])
    norm2_g = din("norm2_g", [D])
    router_w = din("router_w", [D, NE])
    router_b = din("router_b", [NE])
    w1 = din("w1", [NE, D, 2 * FF])
    b1T = din("b1T", [128, NE, 16])
    w2 = din("w2", [NE, FF, D])
    b2 = din("b2", [NE, D])
    c_ident = din("c_ident", [128, 128])
    c_rev = din("c_rev", [128, 128])
    c_tri = din("c_tri", [128, 128])
    c_negtri = din("c_negtri", [128, 128])
    c_bucket = din("c_bucket", [33, 2304])
    c_kind = din("c_kind", [8, SEQ])
    c_selmask = din("c_selmask", [128, 3, 16, 8])
    out_d = dout("out", [T, D])

    P = Prog(nc, es)
    with es:
        mod_d = dscr("mod_d", [NSEQ, 6 * D])
        qkv_d = dscr("qkv_d", [T, 3 * D])
        z_d = dscr("z_d", [T, SSM_INNER])
        dt_d = dscr("dt_d", [T, SSM_H])
        gl_d = dscr("gl_d", [T, 2 * D])
        xbcT_d = dscr("xbcT_d", [CONV_DIM, T])
        expf_d = dscr("expf_d", [H, 2304], BF16)
        wnat_d = dscr("wnat_d", [H, 128, SEQ], BF16)
        attnT_d = dscr("attnT_d", [D, T], BF16)
        ssmT_d = dscr("ssmT_d", [SSM_INNER, T], BF16)
        x1_d = dscr("x1_d", [T, D])
        u2T_d = dscr("u2T_d", [D, T], BF16)
        gw_d = dscr("gw_d", [NE, T])
        gwtm_d = dscr("gwtm_d", [T, NE])

        ident_f = P.sb("ident_f", [128, 128])
        rev_f = P.sb("rev_f", [128, 128])
        ident_b = P.sb("ident_b", [128, 128], BF16)
        rev_b = P.sb("rev_b", [128, 128], BF16)
        ones_b = P.sb("ones_b", [128, 128], BF16)
        ones_f = P.sb("ones_f", [128, 128])
        P.op("sp", lambda: nc.sync.dma_start(out=ident_f[:], in_=c_ident[:, :]), writes=["ident_f"], dma="ident_f")
        P.op("sp", lambda: nc.sync.dma_start(out=rev_f[:], in_=c_rev[:, :]), writes=["rev_f"], dma="rev_f")
        P.op("pool", lambda: nc.gpsimd.dma_start(out=ident_b[:], in_=c_ident[:, :]), writes=["ident_b"], dma="ident_b")
        P.op("pool", lambda: nc.gpsimd.dma_start(out=rev_b[:], in_=c_rev[:, :]), writes=["rev_b"], dma="rev_b")
        P.op("dve", lambda: nc.vector.memset(ones_b[:], 1.0), writes=["ones_b"])
        P.op("dve", lambda: nc.vector.memset(ones_f[:], 1.0), writes=["ones_f"])

        psb = [P.ps("psb%d" % i, [128, 512]) for i in range(8)]

        dbg_outs = {}

        def tap(name, shape, dt=F32):
            dbg_outs[name] = dout("dbg_" + name, shape, dt)
            outs_to_drain.append("dbg_" + name)
            return dbg_outs[name]

        if "s0" in stages:
            with contextlib.ExitStack() as st:
                sb = lambda n, s, d=F32: st.enter_context(nc.sbuf_tensor(n, list(s), d))
                cact = sb("cact", [128, 8, NSEQ])
                adab = sb("adab", [NSEQ, 6 * D])
                modsb = sb("modsb", [NSEQ, 6 * D])
                wb = [sb("s0w%d" % i, [128, 8, 512]) for i in range(2)]
                P.op("sp", lambda: nc.sync.dma_start(out=cact[:], in_=cT_d.rearrange("(k p) b -> p k b", p=128)),
                     writes=["cact"], dma="cact")
                P.op("sp", lambda: nc.sync.dma_start(out=adab[:], in_=bass.AP(ada_b.tensor, 0, [[0, NSEQ], [1, 6 * D]])),
                     writes=["adab"], dma="adab")
                P.op("act", lambda: nc.scalar.activation(out=cact[:], in_=cact[:], func=AF.Silu), reads=["cact"], writes=["cact"])
                for j in range(12):
                    w = wb[j % 2]
                    wk = "s0w%d" % (j % 2)
                    P.op("sp", lambda: nc.sync.dma_start(out=w[:], in_=ada_w[:, j * 512:(j + 1) * 512].rearrange("(k p) n -> p k n", p=128)),
                         writes=[wk], dma=wk)
                    pk = "ps%d" % (j % 2)
                    for k in range(8):
                        P.op("pe", lambda: nc.tensor.matmul(psb[j % 2][0:NSEQ, :], lhsT=cact[:, k, :], rhs=w[:, k, :], start=(k == 0), stop=(k == 7)),
                             reads=["cact", wk], writes=[pk])
                    P.op("dve", lambda: nc.vector.tensor_tensor(out=modsb[:, j * 512:(j + 1) * 512], in0=psb[j % 2][0:NSEQ, :],
                                                                in1=adab[:, j * 512:(j + 1) * 512], op=ALU.add),
                         reads=[pk, "adab"], writes=["modsb"])
                P.op("sp", lambda: nc.sync.dma_start(out=mod_d[:, :], in_=modsb[:]), reads=["modsb"], writes=["mod_d"], dma="modsb")
                if "mod" in dbg:
                    t_ = tap("mod", [NSEQ, 6 * D])
                    P.op("sp", lambda: nc.sync.dma_start(out=t_[:, :], in_=modsb[:]), reads=["modsb"], writes=["dbg_mod"], dma="modsb")
                tab = sb("tab", [33, H])
                oh = sb("oh", [33, 2304])
                ef = sb("ef", [H, 2304], BF16)
                P.op("sp", lambda: nc.sync.dma_start(out=tab[:], in_=rel_tab[:, :]), writes=["tab"], dma="tab")
                P.op("sp", lambda: nc.sync.dma_start(out=oh[:], in_=c_bucket[:, :]), writes=["oh"], dma="oh")
                for j in range(5):
                    n = 512 if j < 4 else 256
                    pk = "ps%d" % (2 + j % 2)
                    P.op("pe", lambda: nc.tensor.matmul(psb[2 + j % 2][0:H, 0:n], lhsT=tab[:], rhs=oh[:, j * 512:j * 512 + n], start=True, stop=True),
                         reads=["tab", "oh"], writes=[pk])
                    P.op("act", lambda: nc.scalar.activation(out=ef[:, j * 512:j * 512 + n], in_=psb[2 + j % 2][0:H, 0:n], func=AF.Exp),
                         reads=[pk], writes=["ef"])
                P.op("sp", lambda: nc.sync.dma_start(out=expf_d[:, :], in_=ef[:]), reads=["ef"], writes=["expf_d"], dma="ef")
                wrv = [sb("wrv%d" % i, [128, SEQ], BF16) for i in range(2)]
                wnt = [sb("wnt%d" % i, [128, SEQ], BF16) for i in range(2)]
                for h in range(H):
                    wr_, wrk = wrv[h % 2], "wrv%d" % (h % 2)
                    wn_, wnk = wnt[h % 2], "wnt%d" % (h % 2)
                    P.op("sp", lambda: nc.sync.dma_start(out=wr_[:], in_=bass.AP(expf_d.tensor, h * 2304, [[1, 128], [1, SEQ]])), reads=["expf_d"], writes=[wrk], dma=wrk)
                    for c in range(4):
                        pk = "ps%d" % (4 + c)
                        P.op("pe", lambda: nc.tensor.matmul(psb[4 + c][:, :], lhsT=rev_b[:], rhs=wr_[:, c * 512:(c + 1) * 512], start=True, stop=True), reads=["rev_b", wrk], writes=[pk])
                        if c % 2 == 0:
                            P.op("act", lambda: nc.scalar.copy(out=wn_[:, c * 512:(c + 1) * 512], in_=psb[4 + c][:, :]), reads=[pk], writes=[wnk])
                        else:
                            P.op("dve", lambda: nc.vector.tensor_copy(out=wn_[:, c * 512:(c + 1) * 512], in_=psb[4 + c][:, :]), reads=[pk], writes=[wnk])
                    P.op("sp", lambda: nc.sync.dma_start(out=wnat_d[h, :, :], in_=wn_[:]), reads=[wnk], writes=["wnat_d"], dma=wnk)
                P.barrier()

        GT = 1024
        NG = T // GT
        if "s1" in stages:
            with contextlib.ExitStack() as st:
                sb = lambda n, s, d=F32: st.enter_context(nc.sbuf_tensor(n, list(s), d))
                g1bc = sb("g1bc", [128, D])
                A1 = sb("A1", [128, D])
                sh1 = sb("sh1", [128, D])
                xt = [sb("xt%d" % i, [128, D]) for i in range(2)]
                ut = [sb("ut%d" % i, [128, D]) for i in range(2)]
                sq = sb("sq", [128, D])
                ss = sb("ss", [128, 2])
                uT = sb("uT", [128, 8, GT], BF16)
                wbuf = [sb("wbuf%d" % i, [128, 8, 512], BF16) for i in range(2)]
                stg = [sb("stg%d" % i, [128, 512]) for i in range(2)]
                raw = [sb("raw%d" % i, [128, GT + 3]) for i in range(2)]
                cacc = [sb("cacc%d" % i, [128, GT]) for i in range(2)]
                carry = sb("carry", [128, 24, 3])
                cw = sb("cw", [128, 24, 4])
                cb = sb("cb", [128, 24])
                P.op("sp", lambda: nc.sync.dma_start(out=g1bc[:], in_=bcast_rows(norm1_g, D)), writes=["g1bc"], dma="g1bc")
                P.op("sp", lambda: nc.sync.dma_start(out=cw[:], in_=conv_wT.rearrange("(m p) k -> p m k", p=128)), writes=["cw"], dma="cw")
                P.op("sp", lambda: nc.sync.dma_start(out=cb[:], in_=conv_b[:, :]), writes=["cb"], dma="cb")
                chunks = []
                for j in range(6):
                    chunks.append((OFF_Q + 512 * j, 512, "tm", qkv_d, 512 * j))
                for j in range(4):
                    chunks.append((OFF_Z + 512 * j, 512, "tm", z_d, 512 * j))
                for j in range(6):
                    chunks.append((OFF_XBC + 512 * j, 512, "fm", None, j))
                chunks.append((OFF_DT, 32, "tm", dt_d, 0))
                for j in range(4):
                    chunks.append((OFF_GATE + 512 * j, 512, "tm", gl_d, 512 * j))
                wcount = 0
                scount = 0
                rcount = 0
                for g in range(NG):
                    s = (g * GT) // SEQ
                    if (g * GT) % SEQ == 0:
                        P.op("sp", lambda: nc.sync.dma_start(out=A1[:], in_=bcast_rows(mod_d[s, D:2 * D], D)), reads=["mod_d"], writes=["A1"], dma="A1")
                        P.op("sp", lambda: nc.sync.dma_start(out=sh1[:], in_=bcast_rows(mod_d[s, 0:D], D)), reads=["mod_d"], writes=["sh1"], dma="sh1")
                        P.op("dve", lambda: nc.vector.scalar_tensor_tensor(out=A1[:], in0=A1[:], scalar=1.0, in1=g1bc[:], op0=ALU.add, op1=ALU.mult),
                             reads=["A1", "g1bc"], writes=["A1"])
                        P.op("dve", lambda: nc.vector.memset(carry[:], 0.0), writes=["carry"])
                    for tt in range(GT // 128):
                        t = g * (GT // 128) + tt
                        xb_, xk = xt[t % 2], "xt%d" % (t % 2)
                        ub_, uk = ut[t % 2], "ut%d" % (t % 2)
                        P.op("sp", lambda: nc.sync.dma_start(out=xb_[:], in_=x_d[t * 128:(t + 1) * 128, :]), writes=[xk], dma=xk)
                        P.op("act", lambda: nc.scalar.activation(out=sq[:], in_=xb_[:], func=AF.Square, scale=1.0 / 32.0, accum_out=ss[:, 0:1]),
                             reads=[xk], writes=["sq", "ss"])
                        P.op("dve", lambda: nc.vector.tensor_scalar_add(out=ss[:, 1:2], in0=ss[:, 0:1], scalar1=EPS), reads=["ss"], writes=["ss"])
                        P.op("act", lambda: nc.scalar.sqrt(out=ss[:, 1:2], in_=ss[:, 1:2]), reads=["ss"], writes=["ss"])
                        P.op("dve", lambda: nc.vector.reciprocal(out=ss[:, 1:2], in_=ss[:, 1:2]), reads=["ss"], writes=["ss"])
                        P.op("dve", lambda: nc.vector.scalar_tensor_tensor(out=ub_[:], in0=xb_[:], scalar=ss[:, 1:2], in1=A1[:], op0=ALU.mult, op1=ALU.mult),
                             reads=[xk, "ss", "A1"], writes=[uk])
                        P.op("dve", lambda: nc.vector.tensor_tensor(out=ub_[:], in0=ub_[:], in1=sh1[:], op=ALU.add), reads=[uk, "sh1"], writes=[uk])
                        if "u" in dbg and t < 2:
                            if "u" not in dbg_outs:
                                tap("u", [256, D])
                            P.op("sp", lambda: nc.sync.dma_start(out=dbg_outs["u"][t * 128:(t + 1) * 128, :], in_=ub_[:]), reads=[uk], writes=["dbg_u"], dma=uk)
                        for half in range(2):
                            pb, pk = psb[half], "ps%d" % half
                            for kk in range(4):
                                k = half * 4 + kk
                                P.op("pe", lambda: nc.tensor.matmul(pb[:, kk * 128:(kk + 1) * 128], lhsT=ub_[:, k * 128:(k + 1) * 128], rhs=ident_f[:],
                                                                    start=True, stop=True), reads=[uk, "ident_f"], writes=[pk])
                            P.op("act", lambda: nc.scalar.copy(out=uT[:, half * 4:half * 4 + 4, tt * 128:(tt + 1) * 128],
                                                               in_=pb[:].rearrange("p (k t) -> p k t", k=4)), reads=[pk], writes=["uT"])
                    for (c0, ncol, kind, dest, dcol) in chunks:
                        wb_, wk = wbuf[wcount % 2], "wbuf%d" % (wcount % 2)
                        wcount += 1
                        P.op("pool", lambda: nc.gpsimd.dma_start(out=wb_[:, :, 0:ncol], in_=w_in[:, c0:c0 + ncol].rearrange("(k p) n -> p k n", p=128)),
                             writes=[wk], dma=wk)
                        if kind == "tm":
                            for tt in range(GT // 128):
                                t = g * (GT // 128) + tt
                                pi = 2 + (scount % 4)
                                pb, pk = psb[pi], "ps%d" % pi
                                sg, sk = stg[scount % 2], "stg%d" % (scount % 2)
                                scount += 1
                                for k in range(8):
                                    P.op("pe", lambda: nc.tensor.matmul(pb[:, 0:ncol], lhsT=uT[:, k, tt * 128:(tt + 1) * 128], rhs=wb_[:, k, 0:ncol],
                                                                        start=(k == 0), stop=(k == 7)), reads=["uT", wk], writes=[pk])
                                ev = "act" if scount % 2 else "dve"
                                if ev == "act":
                                    P.op("act", lambda: nc.scalar.copy(out=sg[:, 0:ncol], in_=pb[:, 0:ncol]), reads=[pk], writes=[sk])
                                else:
                                    P.op("dve", lambda: nc.vector.tensor_copy(out=sg[:, 0:ncol], in_=pb[:, 0:ncol]), reads=[pk], writes=[sk])
                                P.op("sp", lambda: nc.sync.dma_start(out=dest[t * 128:(t + 1) * 128, dcol:dcol + ncol], in_=sg[:, 0:ncol]),
                                     reads=[sk], writes=[dest.tensor.name], dma=sk)
                        else:
                            for mm in range(4):
                                m = dcol * 4 + mm
                                rw, rk = raw[rcount % 2], "raw%d" % (rcount % 2)
                                ca, ck = cacc[rcount % 2], "cacc%d" % (rcount % 2)
                                rcount += 1
                                P.op("dve", lambda: nc.vector.tensor_copy(out=rw[:, 0:3], in_=carry[:, m, :]), reads=["carry"], writes=[rk])
                                for hf in range(GT // 512):
                                    pi = 6 + (hf % 2)
                                    pb, pk = psb[pi], "ps%d" % pi
                                    for k in range(8):
                                        P.op("pe", lambda: nc.tensor.matmul(pb[:, :], lhsT=wb_[:, k, mm * 128:(mm + 1) * 128], rhs=uT[:, k, hf * 512:(hf + 1) * 512],
                                                                            start=(k == 0), stop=(k == 7)), reads=["uT", wk], writes=[pk])
                                    P.op("act", lambda: nc.scalar.copy(out=rw[:, 3 + hf * 512:3 + (hf + 1) * 512], in_=pb[:, :]), reads=[pk], writes=[rk])
                                P.op("dve", lambda: nc.vector.tensor_copy(out=carry[:, m, :], in_=rw[:, GT:GT + 3]), reads=[rk], writes=["carry"])
                                P.op("dve", lambda: nc.vector.tensor_scalar(out=ca[:], in0=rw[:, 3:GT + 3], scalar1=cw[:, m, 3:4], scalar2=cb[:, m:m + 1],
                                                                            op0=ALU.mult, op1=ALU.add), reads=[rk, "cw", "cb"], writes=[ck])
                                for tap_ in range(3):
                                    P.op("dve", lambda: nc.vector.scalar_tensor_tensor(out=ca[:], in0=rw[:, tap_:GT + tap_], scalar=cw[:, m, tap_:tap_ + 1], in1=ca[:],
                                                                                       op0=ALU.mult, op1=ALU.add), reads=[rk, ck, "cw"], writes=[ck])
                                P.op("act", lambda: nc.scalar.activation(out=ca[:], in_=ca[:], func=AF.Silu), reads=[ck], writes=[ck])
                                P.op("sp", lambda: nc.sync.dma_start(out=xbcT_d[m * 128:(m + 1) * 128, g * GT:(g + 1) * GT], in_=ca[:]),
                                     reads=[ck], writes=["xbcT_d"], dma=ck)
                P.barrier()

        if "s3" in stages:
            with contextlib.ExitStack() as st:
                sb = lambda n, s, d=F32: st.enter_context(nc.sbuf_tensor(n, list(s), d))
                gq = sb("gq", [128, HD]); gk = sb("gk", [128, HD])
                selm = sb("selm", [128, 3, 16, 8])
                qf = [sb("qf%d" % i, [128, 16, HD]) for i in range(2)]
                kf = [sb("kf%d" % i, [128, 16, HD]) for i in range(2)]
                vf = [sb("vf%d" % i, [128, 16, HD]) for i in range(2)]
                sqt = sb("sqt", [128, 16, HD])
                nrm = sb("nrm", [128, 2, 16])
                qTg = sb("qTg", [64, SEQ], BF16)
                kmT = sb("kmT", [64, 8]); kmb = sb("kmb", [64, 8], BF16)
                gate = sb("gate", [128, 16, 8]); cmpb = sb("cmpb", [128, 16, 8, 8]); rank = sb("rank", [128, 16, 8])
                qaug = sb("qaug", [128, 16, 72], BF16); kb16 = sb("kb16", [128, 16, HD], BF16)
                qTa = [sb("qTa%d" % i, [72, SEQ], BF16) for i in range(2)]
                kTa = [sb("kTa%d" % i, [72, SEQ], BF16) for i in range(2)]
                vaug = [sb("vaug%d" % i, [128, 16, 128], BF16) for i in range(2)]
                Wt = [sb("Wt%d" % i, [128, SEQ], BF16) for i in range(2)]
                pS = [sb("pS%d" % i, [128, 512], BF16) for i in range(3)]
                pW = [sb("pW%d" % i, [128, 512], BF16) for i in range(3)]
                rec = sb("rec", [128, SEQ])
                aT = [sb("aT%d" % i, [64, SEQ], BF16) for i in range(2)]
                P.op("sp", lambda: nc.sync.dma_start(out=gq[:], in_=bcast_rows(q_norm_g, HD)), writes=["gq"], dma="gq")
                P.op("sp", lambda: nc.sync.dma_start(out=gk[:], in_=bcast_rows(k_norm_g, HD)), writes=["gk"], dma="gk")
                P.op("sp", lambda: nc.sync.dma_start(out=selm[:], in_=c_selmask[:, :, :, :]), writes=["selm"], dma="selm")
                for i in range(2):
                    P.op("dve", lambda: nc.vector.memset(vaug[i][:], 1.0), writes=["vaug%d" % i])
                    P.op("pool", lambda: nc.gpsimd.dma_start(out=kTa[i][64:72, :], in_=c_kind[:, :]), writes=["kTa%d" % i], dma="kTa%d" % i)

                def prologue(idx):
                    s_, h = divmod(idx, H)
                    b2_ = idx % 2
                    r0 = s_ * SEQ
                    q_, k_, v_ = qf[b2_], kf[b2_], vf[b2_]
                    qk_, kk_, vk_ = "qf%d" % b2_, "kf%d" % b2_, "vf%d" % b2_
                    qTak, kTak, vak = "qTa%d" % b2_, "kTa%d" % b2_, "vaug%d" % b2_
                    for (buf, nm, off) in [(qf, "qf", OFF_Q), (kf, "kf", OFF_K), (vf, "vf", OFF_V)]:
                        P.op("sp", lambda: nc.sync.dma_start(out=buf[b2_][:], in_=qkv_d[r0:r0 + SEQ, off + h * HD:off + (h + 1) * HD].rearrange("(t p) d -> p t d", p=128)),
                             reads=["qkv_d"], writes=["%s%d" % (nm, b2_)], dma="%s%d" % (nm, b2_))
                    P.op("sp", lambda: nc.sync.dma_start(out=Wt[b2_][:], in_=wnat_d[h, :, :]), reads=["wnat_d"], writes=["Wt%d" % b2_], dma="Wt%d" % b2_)
                    P.op("act", lambda: nc.scalar.copy(out=vaug[b2_][:, :, 0:64], in_=v_[:]), reads=[vk_], writes=[vak])
                    for j, (t_, tk_, g_, gk_) in enumerate([(q_, qk_, gq, "gq"), (k_, kk_, gk, "gk")]):
                        P.op("act", lambda: nc.scalar.activation(out=sqt[:], in_=t_[:], func=AF.Square, scale=0.125), reads=[tk_], writes=["sqt"])
                        P.op("dve", lambda: nc.vector.reduce_sum(out=nrm[:, j, :], in_=sqt[:], axis=AX.X), reads=["sqt"], writes=["nrm"])
                        P.op("dve", lambda: nc.vector.tensor_scalar_add(out=nrm[:, j, :], in0=nrm[:, j, :], scalar1=EPS), reads=["nrm"], writes=["nrm"])
                        P.op("act", lambda: nc.scalar.activation(out=nrm[:, j, :], in_=nrm[:, j, :], func=AF.Ln), reads=["nrm"], writes=["nrm"])
                        P.op("act", lambda: nc.scalar.activation(out=nrm[:, j, :], in_=nrm[:, j, :], func=AF.Exp, scale=-0.5), reads=["nrm"], writes=["nrm"])
                        P.op("dve", lambda: nc.vector.tensor_tensor(out=t_[:], in0=t_[:], in1=nrm[:, j, :].unsqueeze(2).to_broadcast([128, 16, HD]), op=ALU.mult),
                             reads=[tk_, "nrm"], writes=[tk_])
                        dst_, dk_ = (qaug[:, :, 0:64], "qaug") if j == 0 else (kb16[:], "kb16")
                        P.op("dve", lambda: nc.vector.tensor_tensor(out=dst_, in0=t_[:], in1=g_[:].unsqueeze(1).to_broadcast([128, 16, HD]), op=ALU.mult),
                             reads=[tk_, gk_], writes=[dk_])
                    yield
                    for i in range(4):
                        for tt in range(4):
                            P.op("pe", lambda: nc.tensor.matmul(psb[7][0:64, tt * 128:(tt + 1) * 128], lhsT=kb16[:, 4 * i + tt, :], rhs=ident_b[:], start=True, stop=True),
                                 reads=["kb16", "ident_b"], writes=["ps7"])
                        P.op("dve", lambda: nc.vector.reduce_sum(out=kmT[:, 2 * i:2 * i + 2], in_=psb[7][0:64, :].rearrange("p (b k) -> p b k", b=2), axis=AX.X),
                             reads=["ps7"], writes=["kmT"])
                        P.op("dve", lambda: nc.vector.tensor_copy(out=kTa[b2_][0:64, i * 512:(i + 1) * 512], in_=psb[7][0:64, :]), reads=["ps7"], writes=[kTak])
                    P.op("dve", lambda: nc.vector.tensor_scalar_mul(out=kmb[:], in0=kmT[:], scalar1=1.0 / 256.0), reads=["kmT"], writes=["kmb"])
                    for i in range(4):
                        for tt in range(4):
                            P.op("pe", lambda: nc.tensor.matmul(psb[7][0:64, tt * 128:(tt + 1) * 128], lhsT=qaug[:, 4 * i + tt, 0:64], rhs=ident_b[:], start=True, stop=True),
                                 reads=["qaug", "ident_b"], writes=["ps7"])
                        P.op("act", lambda: nc.scalar.copy(out=qTg[:, i * 512:(i + 1) * 512], in_=psb[7][0:64, :]), reads=["ps7"], writes=["qTg"])
                    yield
                    for t in range(16):
                        P.op("pe", lambda: nc.tensor.matmul(psb[7][:, t * 8:(t + 1) * 8], lhsT=qTg[:, t * 128:(t + 1) * 128], rhs=kmb[:], start=True, stop=True),
                             reads=["qTg", "kmb"], writes=["ps7"])
                    P.op("dve", lambda: nc.vector.tensor_tensor(out=gate[:], in0=psb[7][:, 0:128].rearrange("p (t n) -> p t n", t=16), in1=selm[:, 0, :, :], op=ALU.add),
                         reads=["ps7", "selm"], writes=["gate"])
                    P.op("dve", lambda: nc.vector.tensor_tensor(out=cmpb[:], in0=gate[:].unsqueeze(2).to_broadcast([128, 16, 8, 8]),
                                                                in1=gate[:].unsqueeze(3).to_broadcast([128, 16, 8, 8]), op=ALU.is_gt), reads=["gate"], writes=["cmpb"])
                    P.op("dve", lambda: nc.vector.reduce_sum(out=rank[:], in_=cmpb[:], axis=AX.X), reads=["cmpb"], writes=["rank"])
                    P.op("dve", lambda: nc.vector.tensor_single_scalar(out=rank[:], in_=rank[:], scalar=3.0, op=ALU.is_lt), reads=["rank"], writes=["rank"])
                    P.op("dve", lambda: nc.vector.tensor_tensor(out=rank[:], in0=rank[:], in1=selm[:, 1, :, :], op=ALU.mult), reads=["rank", "selm"], writes=["rank"])
                    P.op("dve", lambda: nc.vector.tensor_tensor(out=rank[:], in0=rank[:], in1=selm[:, 2, :, :], op=ALU.add), reads=["rank", "selm"], writes=["rank"])
                    P.op("dve", lambda: nc.vector.tensor_scalar(out=qaug[:, :, 64:72], in0=rank[:], scalar1=-NEG, scalar2=NEG, op0=ALU.mult, op1=ALU.add),
                         reads=["rank"], writes=["qaug"])
                    for i in range(4):
                        for tt in range(4):
                            P.op("pe", lambda: nc.tensor.matmul(psb[7][0:72, tt * 128:(tt + 1) * 128], lhsT=qaug[:, 4 * i + tt, :], rhs=ident_b[:], start=True, stop=True),
                                 reads=["qaug", "ident_b"], writes=["ps7"])
                        P.op("act", lambda: nc.scalar.copy(out=qTa[b2_][:, i * 512:(i + 1) * 512], in_=psb[7][0:72, :]), reads=["ps7"], writes=[qTak])

                cnt3 = [0]

                def main(idx, inject):
                    s_, h = divmod(idx, H)
                    b2_ = idx % 2
                    r0 = s_ * SEQ
                    qTak, kTak, vak, wk_ = "qTa%d" % b2_, "kTa%d" % b2_, "vaug%d" % b2_, "Wt%d" % b2_
                    its = []
                    for kt in range(16):
                        k0 = kt * 128
                        for c in range(k0 // 512, 4):
                            its.append((kt, k0, c, max(k0, 512 * c), 512 * (c + 1)))

                    def emit_s(j):
                        kt, k0, c, q_lo, q_hi = its[j]
                        sbk = 4 + (base + j) % 3
                        P.op("pe", lambda: nc.tensor.matmul(psb[sbk][:, 0:q_hi - q_lo], lhsT=kTa[b2_][:, k0:k0 + 128], rhs=qTa[b2_][:, q_lo:q_hi], start=True, stop=True),
                             reads=[kTak, qTak], writes=["ps%d" % sbk])

                    base = cnt3[0]
                    cnt3[0] += len(its)
                    LOOK = 2
                    for j in range(min(LOOK, len(its))):
                        emit_s(j)
                    for j in range(len(its)):
                        kt, k0, c, q_lo, q_hi = its[j]
                        n = q_hi - q_lo
                        cn = base + j
                        sbk = 4 + cn % 3
                        ps_, pw_ = pS[cn % 3], pW[cn % 3]
                        psk, pwk = "pS%d" % (cn % 3), "pW%d" % (cn % 3)
                        if j + LOOK < len(its):
                            emit_s(j + LOOK)
                        P.op("act", lambda: nc.scalar.activation(out=ps_[:, 0:n], in_=psb[sbk][:, 0:n], func=AF.Exp, scale=0.125), reads=["ps%d" % sbk], writes=[psk])
                        P.op("dve", lambda: nc.vector.tensor_tensor(out=pw_[:, 0:n], in0=ps_[:, 0:n], in1=Wt[b2_][:, q_lo - k0:q_hi - k0], op=ALU.mult),
                             reads=[psk, wk_], writes=[pwk])
                        P.op("pe", lambda: nc.tensor.matmul(psb[c][:, q_lo - 512 * c:q_hi - 512 * c], lhsT=vaug[b2_][:, kt, :], rhs=pw_[:, 0:n],
                                                            start=(kt == 0), stop=(kt == 4 * c + 3), skip_group_check=True), reads=[vak, pwk], writes=["ps%d" % c])
                        if j in (3, 14, 25):
                            inject()
                    for c in range(4):
                        P.op("act", lambda: nc.scalar.activation(out=rec[64:128, c * 512:(c + 1) * 512], in_=psb[c][64:128, :], func=AF.Ln), reads=["ps%d" % c], writes=["rec"])
                        P.op("act", lambda: nc.scalar.activation(out=rec[64:128, c * 512:(c + 1) * 512], in_=rec[64:128, c * 512:(c + 1) * 512], func=AF.Exp, scale=-1.0), reads=["rec"], writes=["rec"])
                        P.op("dve", lambda: nc.vector.tensor_tensor(out=aT[b2_][:, c * 512:(c + 1) * 512], in0=psb[c][0:64, :], in1=rec[64:128, c * 512:(c + 1) * 512], op=ALU.mult),
                             reads=["ps%d" % c, "rec"], writes=["aT%d" % b2_])
                    P.op("sp", lambda: nc.sync.dma_start(out=attnT_d[h * HD:(h + 1) * HD, r0:r0 + SEQ], in_=aT[b2_][:]), reads=["aT%d" % b2_], writes=["attnT_d"], dma="aT%d" % b2_)

                NHD = NSEQ * H
                for _ in prologue(0):
                    pass
                for idx in range(NHD):
                    gen = prologue(idx + 1) if idx + 1 < NHD else iter(())
                    main(idx, lambda g_=gen: next(g_, None))
                    for _ in gen:
                        pass
                P.barrier()

        if "s4" in stages:
            with contextlib.ExitStack() as st:
                sb = lambda n, s, d=F32: st.enter_context(nc.sbuf_tensor(n, list(s), d))
                tri = sb("tri", [128, 128]); negtri = sb("negtri", [128, 128])
                dtb = sb("dtb", [128, SSM_H]); abc = sb("abc", [128, SSM_H]); dsk = sb("dsk", [128, SSM_H])
                sng = sb("sng", [128, SSM_INNER])
                xsT = [sb("xsT%d" % i, [128, 16, 128]) for i in range(2)]
                BTb = [sb("BTb%d" % i, [128, 4, 128], BF16) for i in range(2)]
                CTb = [sb("CTb%d" % i, [128, 4, 128], BF16) for i in range(2)]
                zt = [sb("zt%d" % i, [128, SSM_INNER]) for i in range(2)]
                dtr = [sb("dtr%d" % i, [128, SSM_H]) for i in range(2)]
                sm = sb("sm", [128, 13, SSM_H])
                Rb = sb("Rb", [128, SSM_H, 128])
                xs = sb("xs", [128, SSM_INNER])
                xdt = sb("xdt", [128, SSM_INNER], BF16); xdte = sb("xdte", [128, SSM_INNER], BF16)
                Btm = sb("Btm", [128, 4, 128], BF16)
                Dm = [sb("Dm%d" % i, [128, 4, 128]) for i in range(2)]
                MT = [sb("MT%d" % i, [128, 4, 128], BF16) for i in range(2)]
                Hf = sb("Hf", [128, SSM_INNER]); Hb = sb("Hb", [128, SSM_INNER], BF16)
                ysb = sb("ysb", [128, SSM_INNER]); tmp = sb("tmp4", [128, SSM_INNER])
                nr4 = sb("nr4", [128, 8])
                ssb = sb("ssb", [128, SSM_INNER], BF16)
                sT = [sb("sT%d" % i, [128, 16, 512], BF16) for i in range(2)]
                P.op("sp", lambda: nc.sync.dma_start(out=tri[:], in_=c_tri[:, :]), writes=["tri"], dma="tri")
                P.op("sp", lambda: nc.sync.dma_start(out=negtri[:], in_=c_negtri[:, :]), writes=["negtri"], dma="negtri")
                P.op("sp", lambda: nc.sync.dma_start(out=dtb[:], in_=bcast_rows(dt_bias, SSM_H)), writes=["dtb"], dma="dtb")
                P.op("sp", lambda: nc.sync.dma_start(out=abc[:], in_=bcast_rows(a_log, SSM_H)), writes=["abc"], dma="abc")
                P.op("sp", lambda: nc.sync.dma_start(out=dsk[:], in_=bcast_rows(d_skip, SSM_H)), writes=["dsk"], dma="dsk")
                P.op("sp", lambda: nc.sync.dma_start(out=sng[:], in_=bcast_rows(ssm_norm_g, SSM_INNER)), writes=["sng"], dma="sng")
                P.op("act", lambda: nc.scalar.activation(out=abc[:], in_=abc[:], func=AF.Exp), reads=["abc"], writes=["abc"])
                P.op("dve", lambda: nc.vector.tensor_scalar_mul(out=abc[:], in0=abc[:], scalar1=-1.0), reads=["abc"], writes=["abc"])
                V_ = lambda i: sm[:, i, :]
                bc3 = lambda ap, n, w: ap.unsqueeze(2).to_broadcast([128, n, w])
                for s_ in range(NSEQ):
                    P.op("dve", lambda: nc.vector.memset(Hf[:], 0.0), writes=["Hf"])
                    P.op("dve", lambda: nc.vector.memset(Hb[:], 0.0), writes=["Hb"])
                    for c in range(16):
                        cc = s_ * 16 + c
                        b2_ = cc % 2
                        t0 = cc * 128
                        P.op("sp", lambda: nc.sync.dma_start(out=xsT[b2_][:], in_=xbcT_d[0:2048, t0:t0 + 128].rearrange("(m p) t -> p m t", p=128)),
                             reads=["xbcT_d"], writes=["xsT%d" % b2_], dma="xsT%d" % b2_)
                        P.op("pool", lambda: nc.gpsimd.dma_start(out=BTb[b2_][:], in_=xbcT_d[2048:2560, t0:t0 + 128].rearrange("(m p) t -> p m t", p=128)),
                             reads=["xbcT_d"], writes=["BTb%d" % b2_], dma="BTb%d" % b2_)
                        P.op("pool", lambda: nc.gpsimd.dma_start(out=CTb[b2_][:], in_=xbcT_d[2560:3072, t0:t0 + 128].rearrange("(m p) t -> p m t", p=128)),
                             reads=["xbcT_d"], writes=["CTb%d" % b2_], dma="CTb%d" % b2_)
                        P.op("sp", lambda: nc.sync.dma_start(out=zt[b2_][:], in_=z_d[t0:t0 + 128, :]), reads=["z_d"], writes=["zt%d" % b2_], dma="zt%d" % b2_)
                        P.op("sp", lambda: nc.sync.dma_start(out=dtr[b2_][:], in_=dt_d[t0:t0 + 128, :]), reads=["dt_d"], writes=["dtr%d" % b2_], dma="dtr%d" % b2_)
                        xk, bk, ck, zk, dk = "xsT%d" % b2_, "BTb%d" % b2_, "CTb%d" % b2_, "zt%d" % b2_, "dtr%d" % b2_
                        P.op("dve", lambda: nc.vector.tensor_tensor(out=V_(0), in0=dtr[b2_][:], in1=dtb[:], op=ALU.add), reads=[dk, "dtb"], writes=["sm0"])
                        P.op("act", lambda: nc.scalar.activation(out=V_(1), in_=V_(0), func=AF.Abs), reads=["sm0"], writes=["sm1"])
                        P.op("act", lambda: nc.scalar.activation(out=V_(2), in_=V_(1), func=AF.Exp, scale=-1.0), reads=["sm1"], writes=["sm2"])
                        P.op("act", lambda: nc.scalar.activation(out=V_(2), in_=V_(2), func=AF.Ln, bias=1.0), reads=["sm2"], writes=["sm2"])
                        P.op("dve", lambda: nc.vector.scalar_tensor_tensor(out=V_(3), in0=V_(0), scalar=0.0, in1=V_(2), op0=ALU.max, op1=ALU.add), reads=["sm0", "sm2"], writes=["sm3"])
                        P.op("dve", lambda: nc.vector.tensor_tensor(out=V_(4), in0=V_(3), in1=abc[:], op=ALU.mult), reads=["sm3", "abc"], writes=["sm4"])
                        P.op("pe", lambda: nc.tensor.matmul(psb[0][:, 0:32], lhsT=tri[:], rhs=V_(4), start=True, stop=True), reads=["tri", "sm4"], writes=["ps0"])
                        P.op("pe", lambda: nc.tensor.matmul(psb[0][:, 32:64], lhsT=ones_f[:], rhs=V_(4), start=True, stop=True), reads=["ones_f", "sm4"], writes=["ps0"])
                        P.op("act", lambda: nc.scalar.copy(out=sm[:, 5:7, :], in_=psb[0][:, 0:64].rearrange("p (a h) -> p a h", a=2)), reads=["ps0"], writes=["sm5", "sm6"])
                        P.op("dve", lambda: nc.vector.tensor_tensor(out=V_(7), in0=V_(6), in1=V_(5), op=ALU.subtract), reads=["sm5", "sm6"], writes=["sm7"])
                        P.op("act", lambda: nc.scalar.activation(out=sm[:, 10:13, :], in_=sm[:, 5:8, :], func=AF.Exp), reads=["sm5", "sm6", "sm7"], writes=["sm10", "sm11", "sm12"])
                        EACS, CD, DTE = 10, 11, 12
                        P.op("dve", lambda: nc.vector.tensor_tensor(out=Rb[:], in0=tri[:].unsqueeze(1).to_broadcast([128, SSM_H, 128]), in1=bc3(V_(4), SSM_H, 128), op=ALU.mult),
                             reads=["tri", "sm4"], writes=["Rb"])
                        for m in range(16):
                            P.op("pe", lambda: nc.tensor.matmul(psb[3 + m // 4][:, (m % 4) * 128:(m % 4 + 1) * 128], lhsT=xsT[b2_][:, m, :], rhs=ident_f[:], start=True, stop=True),
                                 reads=[xk, "ident_f"], writes=["ps%d" % (3 + m // 4)])
                        for i in range(4):
                            P.op("act", lambda: nc.scalar.copy(out=xs[:, i * 512:(i + 1) * 512], in_=psb[3 + i][:, :]), reads=["ps%d" % (3 + i)], writes=["xs"])
                        xs3 = xs[:].rearrange("p (h d) -> p h d", h=SSM_H)
                        P.op("dve", lambda: nc.vector.tensor_tensor(out=xdt[:].rearrange("p (h d) -> p h d", h=SSM_H), in0=xs3, in1=bc3(V_(3), SSM_H, 64), op=ALU.mult),
                             reads=["xs", "sm3"], writes=["xdt"])
                        P.op("dve", lambda: nc.vector.tensor_tensor(out=xdte[:].rearrange("p (h d) -> p h d", h=SSM_H), in0=xdt[:].rearrange("p (h d) -> p h d", h=SSM_H),
                                                                    in1=bc3(V_(DTE), SSM_H, 64), op=ALU.mult), reads=["xdt", "sm12"], writes=["xdte"])
                        for g in range(4):
                            P.op("pe", lambda: nc.tensor.matmul(psb[7][:, g * 128:(g + 1) * 128], lhsT=BTb[b2_][:, g, :], rhs=ident_b[:], start=True, stop=True),
                                 reads=[bk, "ident_b"], writes=["ps7"])
                        P.op("act", lambda: nc.scalar.copy(out=Btm[:], in_=psb[7][:, :].rearrange("p (g n) -> p g n", g=4)), reads=["ps7"], writes=["Btm"])
                        for g in range(4):
                            P.op("pe", lambda: nc.tensor.matmul(psb[2][:, g * 128:(g + 1) * 128], lhsT=BTb[b2_][:, g, :], rhs=CTb[b2_][:, g, :], start=True, stop=True),
                                 reads=[bk, ck], writes=["ps2"])
                        def emit_ones(j):
                            P.op("pe", lambda: nc.tensor.matmul(psb[j % 2][:, :], lhsT=ones_f[:], rhs=Rb[:, 4 * j:4 * j + 4, :], start=True, stop=True),
                                 reads=["ones_f", "Rb"], writes=["ps%d" % (j % 2)])
                        emit_ones(0)
                        for g in range(4):
                            P.op("pe", lambda: nc.tensor.matmul(psb[4][:, :], lhsT=CTb[b2_][:, g, :], rhs=Hb[:, g * 512:(g + 1) * 512], start=True, stop=True),
                                 reads=[ck, "Hb"], writes=["ps4"])
                            P.op("pe", lambda: nc.tensor.matmul(psb[5][:, :], lhsT=Btm[:, g, :], rhs=xdte[:, g * 512:(g + 1) * 512], start=True, stop=True),
                                 reads=["Btm", "xdte"], writes=["ps5"])
                            for j2 in range(2):
                                j = g * 2 + j2
                                pb = j % 2
                                dm, dmk = Dm[j % 2], "Dm%d" % (j % 2)
                                mt, mtk = MT[j % 2], "MT%d" % (j % 2)
                                P.op("dve", lambda: nc.vector.tensor_tensor(out=dm[:], in0=psb[pb][:, :].rearrange("p (h l) -> p h l", h=4), in1=bc3(sm[:, 5, 4 * j:4 * j + 4], 4, 128), op=ALU.subtract),
                                     reads=["ps%d" % pb, "sm5"], writes=[dmk])
                                if j + 1 < 8:
                                    emit_ones(j + 1)
                                P.op("dve", lambda: nc.vector.tensor_tensor(out=dm[:], in0=dm[:], in1=negtri[:].unsqueeze(1).to_broadcast([128, 4, 128]), op=ALU.add),
                                     reads=[dmk, "negtri"], writes=[dmk])
                                P.op("act", lambda: nc.scalar.activation(out=dm[:], in_=dm[:], func=AF.Exp), reads=[dmk], writes=[dmk])
                                P.op("dve", lambda: nc.vector.tensor_tensor(out=mt[:], in0=dm[:], in1=psb[2][:, g * 128:(g + 1) * 128].unsqueeze(1).to_broadcast([128, 4, 128]), op=ALU.mult),
                                     reads=[dmk, "ps2"], writes=[mtk])
                                for hh in range(4):
                                    hd_ = 4 * j + hh
                                    P.op("pe", lambda: nc.tensor.matmul(psb[3][:, (hd_ % 8) * 64:(hd_ % 8 + 1) * 64], lhsT=mt[:, hh, :], rhs=xdt[:, hd_ * 64:(hd_ + 1) * 64], start=True, stop=True),
                                         reads=[mtk, "xdt"], writes=["ps3"])
                            ysl = ysb[:, g * 512:(g + 1) * 512].rearrange("p (h d) -> p h d", h=8)
                            P.op("dve", lambda: nc.vector.tensor_tensor(out=ysl, in0=psb[4][:, :].rearrange("p (h d) -> p h d", h=8), in1=bc3(sm[:, EACS, 8 * g:8 * g + 8], 8, 64), op=ALU.mult),
                                 reads=["ps4", "sm10"], writes=["ysb"])
                            P.op("dve", lambda: nc.vector.tensor_tensor(out=ysb[:, g * 512:(g + 1) * 512], in0=ysb[:, g * 512:(g + 1) * 512], in1=psb[3][:, :], op=ALU.add),
                                 reads=["ysb", "ps3"], writes=["ysb"])
                            hsl = Hf[:, g * 512:(g + 1) * 512]
                            P.op("dve", lambda: nc.vector.tensor_tensor(out=hsl.rearrange("p (h d) -> p h d", h=8), in0=hsl.rearrange("p (h d) -> p h d", h=8),
                                                                        in1=bc3(sm[:, CD, 8 * g:8 * g + 8], 8, 64), op=ALU.mult), reads=["Hf", "sm11"], writes=["Hf"])
                            P.op("dve", lambda: nc.vector.tensor_tensor(out=hsl, in0=hsl, in1=psb[5][:, :], op=ALU.add), reads=["Hf", "ps5"], writes=["Hf"])
                        P.op("act", lambda: nc.scalar.copy(out=Hb[:], in_=Hf[:]), reads=["Hf"], writes=["Hb"])
                        P.op("dve", lambda: nc.vector.tensor_tensor(out=tmp[:].rearrange("p (h d) -> p h d", h=SSM_H), in0=xs3, in1=bc3(dsk[:], SSM_H, 64), op=ALU.mult),
                             reads=["xs", "dsk"], writes=["tmp4"])
                        P.op("dve", lambda: nc.vector.tensor_tensor(out=ysb[:], in0=ysb[:], in1=tmp[:], op=ALU.add), reads=["ysb", "tmp4"], writes=["ysb"])
                        P.op("act", lambda: nc.scalar.activation(out=tmp[:], in_=zt[b2_][:], func=AF.Silu), reads=[zk], writes=["tmp4"])
                        P.op("dve", lambda: nc.vector.tensor_tensor(out=ysb[:], in0=ysb[:], in1=tmp[:], op=ALU.mult), reads=["ysb", "tmp4"], writes=["ysb"])
                        for g in range(4):
                            P.op("act", lambda: nc.scalar.activation(out=tmp[:, g * 512:(g + 1) * 512], in_=ysb[:, g * 512:(g + 1) * 512], func=AF.Square,
                                                                     scale=float(512 ** -0.5), accum_out=nr4[:, g:g + 1]), reads=["ysb"], writes=["tmp4", "nr4"])
                        P.op("dve", lambda: nc.vector.tensor_scalar_add(out=nr4[:, 4:8], in0=nr4[:, 0:4], scalar1=EPS), reads=["nr4"], writes=["nr4"])
                        P.op("act", lambda: nc.scalar.sqrt(out=nr4[:, 4:8], in_=nr4[:, 4:8]), reads=["nr4"], writes=["nr4"])
                        P.op("dve", lambda: nc.vector.reciprocal(out=nr4[:, 4:8], in_=nr4[:, 4:8]), reads=["nr4"], writes=["nr4"])
                        P.op("dve", lambda: nc.vector.tensor_tensor(out=ysb[:].rearrange("p (g d) -> p g d", g=4), in0=ysb[:].rearrange("p (g d) -> p g d", g=4),
                                                                    in1=bc3(nr4[:, 4:8], 4, 512), op=ALU.mult), reads=["ysb", "nr4"], writes=["ysb"])
                        P.op("dve", lambda: nc.vector.tensor_tensor(out=ssb[:], in0=ysb[:], in1=sng[:], op=ALU.mult), reads=["ysb", "sng"], writes=["ssb"])
                        so, sok = sT[(cc // 4) % 2], "sT%d" % ((cc // 4) % 2)
                        for m in range(16):
                            P.op("pe", lambda: nc.tensor.matmul(psb[3 + m // 4][:, (m % 4) * 128:(m % 4 + 1) * 128], lhsT=ssb[:, m * 128:(m + 1) * 128], rhs=ident_b[:], start=True, stop=True),
                                 reads=["ssb", "ident_b"], writes=["ps%d" % (3 + m // 4)])
                        for i in range(4):
                            P.op("act", lambda: nc.scalar.copy(out=so[:, 4 * i:4 * i + 4, (cc % 4) * 128:(cc % 4 + 1) * 128], in_=psb[3 + i][:, :].rearrange("p (m t) -> p m t", m=4)),
                                 reads=["ps%d" % (3 + i)], writes=[sok])
                        if cc % 4 == 3:
                            tb = (cc // 4) * 512
                            P.op("sp", lambda: nc.sync.dma_start(out=ssmT_d[:, tb:tb + 512].rearrange("(m p) t -> p m t", p=128), in_=so[:]), reads=[sok], writes=["ssmT_d"], dma=sok)
                P.barrier()

        if "s5" in stages:
            with contextlib.ExitStack() as st:
                sb = lambda n, s, d=F32: st.enter_context(nc.sbuf_tensor(n, list(s), d))
                Wa = sb("Wa", [128, 8, D], BF16); Ws = sb("Ws", [128, 16, D], BF16); Wo = sb("Wo", [128, 8, D], BF16)
                rw = sb("rw", [128, 8, NE]); rbb = sb("rbb", [128, NE])
                gbb = sb("gbb", [128, 2 * D]); g2bc = sb("g2bc", [128, D])
                g1b = [sb("g1b%d" % i, [128, D]) for i in range(2)]; A2 = [sb("A2%d" % i, [128, D]) for i in range(2)]; sh2 = [sb("sh2%d" % i, [128, D]) for i in range(2)]
                aTt = [sb("aTt%d" % i, [128, 8, 128], BF16) for i in range(2)]
                sTt = [sb("sTt%d" % i, [128, 16, 128], BF16) for i in range(2)]
                glt = [sb("glt%d" % i, [128, 2 * D]) for i in range(2)]
                xt5 = [sb("xt5%d" % i, [128, D]) for i in range(2)]
                mrg = sb("mrg", [128, D]); tm5 = sb("tm5", [128, D]); mrb = [sb("mrb%d" % i, [128, D], BF16) for i in range(2)]
                mT = sb("mT", [128, 8, 128], BF16)
                x1t = [sb("x1t%d" % i, [128, D]) for i in range(2)]
                u2 = sb("u2", [128, D]); u2Tf = sb("u2Tf", [128, 8, 128])
                u2Tb = [sb("u2Tb%d" % i, [128, 8, 512], BF16) for i in range(2)]
                ss5 = sb("ss5", [128, 2]); sq5 = sb("sq5", [128, D])
                lg = sb("lg", [128, NE]); m8 = sb("m8", [128, 8]); ex = sb("ex", [128, NE]); msk = sb("msk", [128, NE])
                sm5 = sb("sm5_", [128, 4]); gwt = [sb("gwt%d" % i, [128, NE]) for i in range(2)]
                gwT = [sb("gwT%d" % i, [NE, 128]) for i in range(2)]
                for (wsb, wnm, wdr, nk) in [(Wa, "Wa", w_attn, 8), (Ws, "Ws", w_ssm, 16), (Wo, "Wo", w_out, 8)]:
                    for k in range(0, nk, 8):
                        for n in range(2):
                            P.op("pool", lambda: nc.gpsimd.dma_start(out=wsb[:, k:k + 8, n * 512:(n + 1) * 512],
                                                                     in_=wdr[k * 128:(k + 8) * 128, n * 512:(n + 1) * 512].rearrange("(k p) n -> p k n", p=128)),
                                 reads=["wchain"], writes=[wnm, "wchain"], dma="%s_%d_%d" % (wnm, k, n))
                P.op("sp", lambda: nc.sync.dma_start(out=rw[:], in_=router_w.rearrange("(k p) n -> p k n", p=128)), writes=["rw"], dma="rw")
                P.op("sp", lambda: nc.sync.dma_start(out=rbb[:], in_=bcast_rows(router_b, NE)), writes=["rbb"], dma="rbb")
                P.op("sp", lambda: nc.sync.dma_start(out=gbb[:], in_=bcast_rows(gate_bias, 2 * D)), writes=["gbb"], dma="gbb")
                P.op("sp", lambda: nc.sync.dma_start(out=g2bc[:], in_=bcast_rows(norm2_g, D)), writes=["g2bc"], dma="g2bc")
                def st_a1(t):
                        s_ = t // 16
                        b2_ = t % 2
                        t0 = t * 128
                        ak, sk, gk_, xk = "aTt%d" % b2_, "sTt%d" % b2_, "glt%d" % b2_, "xt5%d" % b2_
                        x1_, x1k = x1t[b2_], "x1t%d" % b2_
                        gw_, gwk = gwt[b2_], "gwt%d" % b2_
                        ub_, ubk = u2Tb[(t // 4) % 2], "u2Tb%d" % ((t // 4) % 2)
                        if t % 16 == 0:
                            P.op("sp", lambda: nc.sync.dma_start(out=g1b[s_][:], in_=bcast_rows(mod_d[s_, 2 * D:3 * D], D)), reads=["mod_d"], writes=["g1b%d" % s_], dma="g1b%d" % s_)
                            P.op("sp", lambda: nc.sync.dma_start(out=sh2[s_][:], in_=bcast_rows(mod_d[s_, 3 * D:4 * D], D)), reads=["mod_d"], writes=["sh2%d" % s_], dma="sh2%d" % s_)
                            P.op("sp", lambda: nc.sync.dma_start(out=A2[s_][:], in_=bcast_rows(mod_d[s_, 4 * D:5 * D], D)), reads=["mod_d"], writes=["A2%d" % s_], dma="A2%d" % s_)
                            P.op("dve", lambda: nc.vector.scalar_tensor_tensor(out=A2[s_][:], in0=A2[s_][:], scalar=1.0, in1=g2bc[:], op0=ALU.add, op1=ALU.mult),
                                 reads=["A2%d" % s_, "g2bc"], writes=["A2%d" % s_])
                        ak, sk, gk_, xk = "aTt%d" % b2_, "sTt%d" % b2_, "glt%d" % b2_, "xt5%d" % b2_
                        P.op("sp", lambda: nc.sync.dma_start(out=aTt[b2_][:], in_=attnT_d[:, t0:t0 + 128].rearrange("(k p) t -> p k t", p=128)), reads=["attnT_d"], writes=[ak], dma=ak)
                        P.op("sp", lambda: nc.sync.dma_start(out=sTt[b2_][:], in_=ssmT_d[:, t0:t0 + 128].rearrange("(k p) t -> p k t", p=128)), reads=["ssmT_d"], writes=[sk], dma=sk)
                        P.op("sp", lambda: nc.sync.dma_start(out=glt[b2_][:], in_=gl_d[t0:t0 + 128, :]), reads=["gl_d"], writes=[gk_], dma=gk_)
                        P.op("sp", lambda: nc.sync.dma_start(out=xt5[b2_][:], in_=x_d[t0:t0 + 128, :]), writes=[xk], dma=xk)
                        P.op("dve", lambda: nc.vector.tensor_tensor(out=glt[b2_][:], in0=glt[b2_][:], in1=gbb[:], op=ALU.add), reads=[gk_, "gbb"], writes=[gk_])
                        P.op("act", lambda: nc.scalar.activation(out=glt[b2_][:], in_=glt[b2_][:], func=AF.Sigmoid), reads=[gk_], writes=[gk_])
                        for n in range(2):
                            for k in range(8):
                                P.op("pe", lambda: nc.tensor.matmul(psb[n][:, :], lhsT=aTt[b2_][:, k, :], rhs=Wa[:, k, n * 512:(n + 1) * 512], start=(k == 0), stop=(k == 7)),
                                     reads=[ak, "Wa"], writes=["ps%d" % n])
                            P.op("dve", lambda: nc.vector.tensor_tensor(out=mrg[:, n * 512:(n + 1) * 512], in0=psb[n][:, :], in1=glt[b2_][:, n * 512:(n + 1) * 512], op=ALU.mult),
                                 reads=["ps%d" % n, gk_], writes=["mrg"])
                            for k in range(16):
                                P.op("pe", lambda: nc.tensor.matmul(psb[2 + n][:, :], lhsT=sTt[b2_][:, k, :], rhs=Ws[:, k, n * 512:(n + 1) * 512], start=(k == 0), stop=(k == 15)),
                                     reads=[sk, "Ws"], writes=["ps%d" % (2 + n)])
                            P.op("dve", lambda: nc.vector.tensor_tensor(out=tm5[:, n * 512:(n + 1) * 512], in0=psb[2 + n][:, :], in1=glt[b2_][:, D + n * 512:D + (n + 1) * 512], op=ALU.mult),
                                 reads=["ps%d" % (2 + n), gk_], writes=["tm5"])
                        P.op("dve", lambda: nc.vector.tensor_tensor(out=mrb[b2_][:], in0=mrg[:], in1=tm5[:], op=ALU.add), reads=["mrg", "tm5"], writes=["mrb%d" % b2_])

                def st_a2(t):
                        s_ = t // 16
                        b2_ = t % 2
                        t0 = t * 128
                        ak, sk, gk_, xk = "aTt%d" % b2_, "sTt%d" % b2_, "glt%d" % b2_, "xt5%d" % b2_
                        x1_, x1k = x1t[b2_], "x1t%d" % b2_
                        gw_, gwk = gwt[b2_], "gwt%d" % b2_
                        ub_, ubk = u2Tb[(t // 4) % 2], "u2Tb%d" % ((t // 4) % 2)
                        for half in range(2):
                            for kk in range(4):
                                k = half * 4 + kk
                                P.op("pe", lambda: nc.tensor.matmul(psb[4][:, kk * 128:(kk + 1) * 128], lhsT=mrb[b2_][:, k * 128:(k + 1) * 128], rhs=ident_b[:], start=True, stop=True),
                                     reads=["mrb%d" % b2_, "ident_b"], writes=["ps4"])
                            P.op("act", lambda: nc.scalar.copy(out=mT[:, half * 4:half * 4 + 4, :], in_=psb[4][:, :].rearrange("p (k t) -> p k t", k=4)),
                                 reads=["ps4"], writes=["mT"])
                        x1_, x1k = x1t[b2_], "x1t%d" % b2_
                        for n in range(2):
                            for k in range(8):
                                P.op("pe", lambda: nc.tensor.matmul(psb[6 + n][:, :], lhsT=mT[:, k, :], rhs=Wo[:, k, n * 512:(n + 1) * 512], start=(k == 0), stop=(k == 7)),
                                     reads=["mT", "Wo"], writes=["ps%d" % (6 + n)])
                            P.op("dve", lambda: nc.vector.tensor_tensor(out=x1_[:, n * 512:(n + 1) * 512], in0=psb[6 + n][:, :], in1=g1b[s_][:, n * 512:(n + 1) * 512], op=ALU.mult),
                                 reads=["ps%d" % (6 + n), "g1b%d" % s_], writes=[x1k])
                        P.op("dve", lambda: nc.vector.tensor_tensor(out=x1_[:], in0=x1_[:], in1=xt5[b2_][:], op=ALU.add), reads=[x1k, xk], writes=[x1k])
                        P.op("sp", lambda: nc.sync.dma_start(out=x1_d[t0:t0 + 128, :], in_=x1_[:]), reads=[x1k], writes=["x1_d"], dma=x1k)

                def st_b(t):
                        s_ = t // 16
                        b2_ = t % 2
                        t0 = t * 128
                        ak, sk, gk_, xk = "aTt%d" % b2_, "sTt%d" % b2_, "glt%d" % b2_, "xt5%d" % b2_
                        x1_, x1k = x1t[b2_], "x1t%d" % b2_
                        gw_, gwk = gwt[b2_], "gwt%d" % b2_
                        ub_, ubk = u2Tb[(t // 4) % 2], "u2Tb%d" % ((t // 4) % 2)
                        P.op("act", lambda: nc.scalar.activation(out=sq5[:], in_=x1_[:], func=AF.Square, scale=1.0 / 32.0, accum_out=ss5[:, 0:1]), reads=[x1k], writes=["sq5", "ss5"])
                        P.op("dve", lambda: nc.vector.tensor_scalar_add(out=ss5[:, 1:2], in0=ss5[:, 0:1], scalar1=EPS), reads=["ss5"], writes=["ss5"])
                        P.op("act", lambda: nc.scalar.sqrt(out=ss5[:, 1:2], in_=ss5[:, 1:2]), reads=["ss5"], writes=["ss5"])
                        P.op("dve", lambda: nc.vector.reciprocal(out=ss5[:, 1:2], in_=ss5[:, 1:2]), reads=["ss5"], writes=["ss5"])
                        P.op("dve", lambda: nc.vector.scalar_tensor_tensor(out=u2[:], in0=x1_[:], scalar=ss5[:, 1:2], in1=A2[s_][:], op0=ALU.mult, op1=ALU.mult),
                             reads=[x1k, "ss5", "A2%d" % s_], writes=["u2"])
                        P.op("dve", lambda: nc.vector.tensor_tensor(out=u2[:], in0=u2[:], in1=sh2[s_][:], op=ALU.add), reads=["u2", "sh2%d" % s_], writes=["u2"])
                        ub_, ubk = u2Tb[(t // 4) % 2], "u2Tb%d" % ((t // 4) % 2)
                        for half in range(2):
                            for kk in range(4):
                                k = half * 4 + kk
                                P.op("pe", lambda: nc.tensor.matmul(psb[5][:, kk * 128:(kk + 1) * 128], lhsT=u2[:, k * 128:(k + 1) * 128], rhs=ident_f[:], start=True, stop=True),
                                     reads=["u2", "ident_f"], writes=["ps5"])
                            P.op("act", lambda: nc.scalar.copy(out=u2Tf[:, half * 4:half * 4 + 4, :], in_=psb[5][:, :].rearrange("p (k t) -> p k t", k=4)),
                                 reads=["ps5"], writes=["u2Tf"])
                            P.op("dve", lambda: nc.vector.tensor_copy(out=ub_[:, half * 4:half * 4 + 4, (t % 4) * 128:(t % 4 + 1) * 128], in_=psb[5][:, :].rearrange("p (k t) -> p k t", k=4)),
                                 reads=["ps5"], writes=[ubk])
                        if t % 4 == 3:
                            tb = (t // 4) * 512
                            P.op("sp", lambda: nc.sync.dma_start(out=u2T_d[:, tb:tb + 512].rearrange("(k p) t -> p k t", p=128), in_=ub_[:]), reads=[ubk], writes=["u2T_d"], dma=ubk)
                        for k in range(8):
                            P.op("pe", lambda: nc.tensor.matmul(psb[5][:, 0:NE], lhsT=u2Tf[:, k, :], rhs=rw[:, k, :], start=(k == 0), stop=(k == 7)), reads=["u2Tf", "rw"], writes=["ps5"])
                        P.op("dve", lambda: nc.vector.tensor_tensor(out=lg[:], in0=psb[5][:, 0:NE], in1=rbb[:], op=ALU.add), reads=["ps5", "rbb"], writes=["lg"])
                        P.op("dve", lambda: nc.vector.max(out=m8[:], in_=lg[:]), reads=["lg"], writes=["m8"])
                        P.op("dve", lambda: nc.vector.tensor_scalar(out=msk[:], in0=lg[:], scalar1=m8[:, 3:4], scalar2=None, op0=ALU.is_ge), reads=["lg", "m8"], writes=["msk"])
                        P.op("dve", lambda: nc.vector.tensor_scalar_mul(out=sm5[:, 0:1], in0=m8[:, 0:1], scalar1=-1.0), reads=["m8"], writes=["sm5_"])
                        P.op("act", lambda: nc.scalar.activation(out=ex[:], in_=lg[:], func=AF.Exp, bias=sm5[:, 0:1], scale=1.0), reads=["lg", "sm5_"], writes=["ex"])
                        P.op("dve", lambda: nc.vector.tensor_tensor(out=ex[:], in0=ex[:], in1=msk[:], op=ALU.mult), reads=["ex", "msk"], writes=["ex"])
                        P.op("dve", lambda: nc.vector.reduce_sum(out=sm5[:, 1:2], in_=ex[:], axis=AX.X), reads=["ex"], writes=["sm5_"])
                        P.op("dve", lambda: nc.vector.reciprocal(out=sm5[:, 2:3], in_=sm5[:, 1:2]), reads=["sm5_"], writes=["sm5_"])
                        gw_, gwk = gwt[b2_], "gwt%d" % b2_
                        P.op("dve", lambda: nc.vector.tensor_scalar(out=gw_[:], in0=ex[:], scalar1=sm5[:, 2:3], scalar2=None, op0=ALU.mult), reads=["ex", "sm5_"], writes=[gwk])
                        P.op("sp", lambda: nc.sync.dma_start(out=gwtm_d[t0:t0 + 128, :], in_=gw_[:]), reads=[gwk], writes=["gwtm_d"], dma=gwk)
                        P.op("pe", lambda: nc.tensor.matmul(psb[5][0:NE, 128:256], lhsT=gw_[:], rhs=ident_f[:], start=True, stop=True), reads=[gwk, "ident_f"], writes=["ps5"])
                        P.op("act", lambda: nc.scalar.copy(out=gwT[b2_][:], in_=psb[5][0:NE, 128:256]), reads=["ps5"], writes=["gwT%d" % b2_])
                        P.op("sp", lambda: nc.sync.dma_start(out=gw_d[:, t0:t0 + 128], in_=gwT[b2_][:]), reads=["gwT%d" % b2_], writes=["gw_d"], dma="gwT%d" % b2_)

                for i in range(NT + 2):
                    if i < NT:
                        st_a1(i)
                    if 0 <= i - 1 < NT:
                        st_a2(i - 1)
                    if 0 <= i - 2 < NT:
                        st_b(i - 2)
                P.barrier()

        if "s6" in stages:
            PT = 1024
            with contextlib.ExitStack() as st:
                sb = lambda n, s, d=F32: st.enter_context(nc.sbuf_tensor(n, list(s), d))
                u2T = sb("u2T6", [128, 8, PT], BF16)
                gwT6 = sb("gwT6", [NE, PT])
                b1s = sb("b1s", [128, NE, 16]); b2s = sb("b2s", [NE, D]); g2b = sb("g2b", [128, D])
                yacc = sb("yacc", [128, PT // 128, D])
                w1a = [sb("w1a%d" % i, [128, 8, 512], BF16) for i in range(3)]
                w1l = [sb("w1l%d" % i, [128, 8, 512], BF16) for i in range(3)]
                w2b = [sb("w2b%d" % i, [128, 8, 512], BF16) for i in range(4)]
                gwtm = sb("gwtm", [128, PT // 128, NE])
                ebuf = {nm: [sb("%s%d" % (nm, i), [128, 512]) for i in range(2)] for nm in ("gg", "ll", "t1", "t2")}
                actT = [[sb("actT%d%d" % (a, b), [128, 8, 512], BF16) for b in range(2)] for a in range(2)]
                x16 = [sb("x16%d" % i, [128, D]) for i in range(2)]
                P.op("sp", lambda: nc.sync.dma_start(out=b1s[:], in_=b1T[:, :, :]), writes=["b1s"], dma="b1s")
                P.op("sp", lambda: nc.sync.dma_start(out=b2s[:], in_=b2[:, :]), writes=["b2s"], dma="b2s")
                b17 = sb("b17", [128, NE, 8])
                P.op("dve", lambda: nc.vector.tensor_scalar_add(out=b17[:], in0=b1s[:, :, 8:16], scalar1=7.0), reads=["b1s"], writes=["b17"])
                NPASS = T // PT
                chunks = [(ps_, e, hh) for ps_ in range(NPASS) for e in range(NE) for hh in range(2)]
                state = {"w1next": 0, "ecnt": 0}

                def emit_w1(upto):
                    while state["w1next"] <= min(upto, len(chunks) - 1):
                        j = state["w1next"]
                        _, e, hh = chunks[j]
                        for (wl_, nm, c0) in [(w1a, "w1a", hh * 512), (w1l, "w1l", FF + hh * 512)]:
                            wk = "%s%d" % (nm, j % 3)
                            P.op("pool", lambda: nc.gpsimd.dma_start(out=wl_[j % 3][:], in_=w1[e, :, c0:c0 + 512].rearrange("(k p) n -> p k n", p=128)),
                                 writes=[wk], dma=wk)
                        state["w1next"] += 1

                def h_phase(ps_, e, jbase):
                    par = e % 2
                    for hh in range(2):
                        j = jbase + hh
                        emit_w1(j + 2)
                        wa_, wak = w1a[j % 3], "w1a%d" % (j % 3)
                        wl2_, wlk = w1l[j % 3], "w1l%d" % (j % 3)
                        for ii in range(4):
                            i = hh * 4 + ii
                            for grp in range(2):
                                n_ = state["ecnt"]; state["ecnt"] += 1
                                pa, pb = (n_ % 2) * 2, (n_ % 2) * 2 + 1
                                for k in range(8):
                                    P.op("pe", lambda: nc.tensor.matmul(psb[pa][:, :], lhsT=wa_[:, k, ii * 128:(ii + 1) * 128], rhs=u2T[:, k, grp * 512:(grp + 1) * 512], start=(k == 0), stop=(k == 7)),
                                         reads=[wak, "u2T6"], writes=["ps%d" % pa])
                                for k in range(8):
                                    P.op("pe", lambda: nc.tensor.matmul(psb[pb][:, :], lhsT=wl2_[:, k, ii * 128:(ii + 1) * 128], rhs=u2T[:, k, grp * 512:(grp + 1) * 512], start=(k == 0), stop=(k == 7)),
                                         reads=[wlk, "u2T6"], writes=["ps%d" % pb])
                                B = {nm: (ebuf[nm][n_ % 2], "%s%d" % (nm, n_ % 2)) for nm in ebuf}
                                P.op("dve", lambda: nc.vector.tensor_scalar(out=B["gg"][0][:], in0=psb[pa][:, :], scalar1=b1s[:, e, i:i + 1], scalar2=7.0, op0=ALU.add, op1=ALU.min),
                                     reads=["ps%d" % pa, "b1s"], writes=[B["gg"][1]])
                                P.op("act", lambda: nc.scalar.activation(out=B["t1"][0][:], in_=B["gg"][0][:], func=AF.Gelu_apprx_sigmoid), reads=[B["gg"][1]], writes=[B["t1"][1]])
                                P.op("act", lambda: nc.scalar.activation(out=B["ll"][0][:], in_=psb[pb][:, :], func=AF.Relu, bias=b17[:, e, i:i + 1], scale=1.0),
                                     reads=["ps%d" % pb, "b17"], writes=[B["ll"][1]])
                                P.op("dve", lambda: nc.vector.tensor_scalar(out=B["t2"][0][:], in0=B["ll"][0][:], scalar1=14.0, scalar2=-6.0, op0=ALU.min, op1=ALU.add),
                                     reads=[B["ll"][1]], writes=[B["t2"][1]])
                                P.op("dve", lambda: nc.vector.tensor_tensor(out=actT[par][grp][:, i, :], in0=B["t1"][0][:], in1=B["t2"][0][:], op=ALU.mult),
                                     reads=[B["t1"][1], B["t2"][1]], writes=["actT%d%d" % (par, grp)])

                def w_loads(e):
                    for n in range(2):
                        wi = (e % 2) * 2 + n
                        P.op("pool", lambda: nc.gpsimd.dma_start(out=w2b[wi][:], in_=w2[e, :, n * 512:(n + 1) * 512].rearrange("(k p) n -> p k n", p=128)),
                             writes=["w2b%d" % wi], dma="w2b%d" % wi)

                def w_phase(e):
                    par = e % 2
                    for n in range(2):
                        for tl in range(PT // 128):
                            grp, off = tl // 4, (tl % 4) * 128
                            pk = 5 + (tl % 2)
                            wi = (e % 2) * 2 + n
                            for k in range(8):
                                P.op("pe", lambda: nc.tensor.matmul(psb[pk][:, :], lhsT=actT[par][grp][:, k, off:off + 128], rhs=w2b[wi][:, k, :], start=(k == 0), stop=(k == 7)),
                                     reads=["actT%d%d" % (par, grp), "w2b%d" % wi], writes=["ps%d" % pk])
                            P.op("dve", lambda: nc.vector.scalar_tensor_tensor(out=yacc[:, tl, n * 512:(n + 1) * 512], in0=psb[pk][:, :], scalar=gwtm[:, tl, e:e + 1],
                                                                               in1=yacc[:, tl, n * 512:(n + 1) * 512], op0=ALU.mult, op1=ALU.add),
                                 reads=["yacc", "ps%d" % pk, "gwtm"], writes=["yacc"])

                for ps_ in range(NPASS):
                    tb = ps_ * PT
                    s_ = tb // SEQ
                    P.op("sp", lambda: nc.sync.dma_start(out=u2T[:], in_=u2T_d[:, tb:tb + PT].rearrange("(k p) t -> p k t", p=128)), reads=["u2T_d"], writes=["u2T6"], dma="u2T6")
                    P.op("sp", lambda: nc.sync.dma_start(out=gwT6[:], in_=gw_d[:, tb:tb + PT]), reads=["gw_d"], writes=["gwT6"], dma="gwT6")
                    P.op("sp", lambda: nc.sync.dma_start(out=gwtm[:], in_=gwtm_d[tb:tb + PT, :].rearrange("(t p) e -> p t e", p=128)), reads=["gwtm_d"], writes=["gwtm"], dma="gwtm")
                    if tb % SEQ == 0:
                        P.op("sp", lambda: nc.sync.dma_start(out=g2b[:], in_=bcast_rows(mod_d[s_, 5 * D:6 * D], D)), reads=["mod_d"], writes=["g2b"], dma="g2b")
                    for tl in range(PT // 128):
                        for n in range(2):
                            pk = 5 + (n % 2)
                            P.op("pe", lambda: nc.tensor.matmul(psb[pk][:, :], lhsT=gwT6[:, tl * 128:(tl + 1) * 128], rhs=b2s[:, n * 512:(n + 1) * 512], start=True, stop=True),
                                 reads=["gwT6", "b2s"], writes=["ps%d" % pk])
                            P.op("act", lambda: nc.scalar.copy(out=yacc[:, tl, n * 512:(n + 1) * 512], in_=psb[pk][:, :]), reads=["ps%d" % pk], writes=["yacc"])
                    jb = ps_ * NE * 2
                    h_phase(ps_, 0, jb)
                    w_loads(0)
                    for e in range(NE):
                        if e + 1 < NE:
                            w_loads(e + 1)
                        if e + 1 < NE:
                            h_phase(ps_, e + 1, jb + (e + 1) * 2)
                        w_phase(e)
                    for tl in range(PT // 128):
                        r0 = tb + tl * 128
                        xb_, xk = x16[tl % 2], "x16%d" % (tl % 2)
                        P.op("sp", lambda: nc.sync.dma_start(out=xb_[:], in_=x1_d[r0:r0 + 128, :]), reads=["x1_d"], writes=[xk], dma=xk)
                        P.op("dve", lambda: nc.vector.tensor_tensor(out=yacc[:, tl, :], in0=yacc[:, tl, :], in1=g2b[:], op=ALU.mult), reads=["yacc", "g2b"], writes=["yacc"])
                        P.op("dve", lambda: nc.vector.tensor_tensor(out=xb_[:], in0=yacc[:, tl, :], in1=xb_[:], op=ALU.add), reads=["yacc", xk], writes=[xk])
                        P.op("sp", lambda: nc.sync.dma_start(out=out_d[r0:r0 + 128, :], in_=xb_[:]), reads=[xk], writes=["out"], dma=xk)
                P.barrier()

        if "copyout" in stages:
            with nc.sbuf_tensor("cpy", [128, D], F32) as cpy:
                P.op("sp", lambda: nc.sync.dma_start(out=cpy[:], in_=x_d[0:128, :]), writes=["cpy"], dma="cpy")
                P.op("sp", lambda: nc.sync.dma_start(out=out_d[0:128, :], in_=cpy[:]), reads=["cpy"], writes=["out"], dma="cpy")
                P.drain("sp", outs_to_drain)
        for name in dbg:
            if name in ("qkv", "z", "dt", "gl", "xbcT"):
                pass
        P.drain("sp", outs_to_drain)
    return nc, P, dbg_outs


def make_consts():
    ident = np.eye(128, dtype=np.float32)
    rev = ident[::-1].copy()
    tri = np.triu(np.ones((128, 128), np.float32))
    negtri = np.where(np.arange(128)[None, :] >= np.arange(128)[:, None], 0.0, NEG).astype(np.float32)
    i = np.arange(2304)
    d = i - 127
    dd = np.maximum(d, 1).astype(np.float32)
    large = 16 + (np.log(dd / 16.0) / math.log(1024 / 16.0) * 16).astype(np.int32)
    large = np.minimum(large, 31)
    bucket = np.where(d < 16, np.maximum(d, 0), large)
    oh = np.zeros((33, 2304), np.float32)
    oh[bucket, i] = 1.0
    oh[:, d < 0] = 0.0
    oh[32, d < 0] = 1.0
    sel = np.zeros((128, 3, 16, 8), np.float32)
    for t in range(16):
        qb = t // 2
        for n in range(8):
            if n >= qb:
                sel[:, 0, t, n] = -1e30
            else:
                sel[:, 1, t, n] = 1.0
            if n == qb:
                sel[:, 2, t, n] = 1.0
    kind = (np.arange(SEQ)[None, :] // 256 == np.arange(8)[:, None]).astype(np.float32)
    return dict(c_ident=ident, c_rev=rev, c_tri=tri, c_negtri=negtri, c_bucket=oh, c_selmask=sel, c_kind=kind)


def make_in_maps(inputs):
    f = lambda a: np.ascontiguousarray(np.asarray(a, dtype=np.float32))
    consts = make_consts()
    x = f(inputs["x"]).reshape(NCORES, T, D)
    c = f(inputs["c"]).reshape(NCORES, NSEQ, D)
    rel = np.concatenate([f(inputs["rel_bias_table"]), np.full((1, H), NEG, np.float32)], 0)
    b1 = f(inputs["expert_b1"])[0]
    shared = dict(
        ada_w=f(inputs["ada_w"])[0], ada_b=f(inputs["ada_b"])[0], norm1_g=f(inputs["norm1_g"])[0], w_in=f(inputs["w_in"])[0],
        q_norm_g=f(inputs["q_norm_g"])[0], k_norm_g=f(inputs["k_norm_g"])[0], rel_tab=rel,
        conv_wT=f(f(inputs["conv_w"])[0].T), conv_b=f(f(inputs["conv_b"])[0].reshape(24, 128).T), dt_bias=f(inputs["dt_bias"])[0],
        a_log=f(inputs["a_log"])[0], d_skip=f(inputs["d_skip"])[0], ssm_norm_g=f(inputs["ssm_norm_g"])[0],
        w_attn=f(inputs["w_attn_branch"])[0], w_ssm=f(inputs["w_ssm_branch"])[0], gate_bias=f(inputs["gate_bias"])[0],
        w_out=f(inputs["w_out"])[0], norm2_g=f(inputs["norm2_g"])[0], router_w=f(inputs["router_w"])[0],
        router_b=f(inputs["router_b"])[0], w1=f(inputs["expert_w1"])[0],
        b1T=f(b1.reshape(NE, 16, 128).transpose(2, 0, 1)), w2=f(inputs["expert_w2"])[0], b2=f(inputs["expert_b2"])[0],
    )
    shared.update(consts)
    maps = []
    for i in range(NCORES):
        m = dict(shared)
        m["x"] = x[i]
        m["cT"] = f(c[i].T)
        maps.append(m)
    return maps


def kernel(**inputs):
    nc, P, _ = build_program()
    in_maps = make_in_maps(inputs)
    res = run_bass_kernel_spmd(nc, in_maps, core_ids=list(range(NCORES)))
    out = np.stack([np.asarray(r["out"], dtype=np.float32) for r in res.results], 0)
    return out.reshape(16, SEQ, D)
```
